# Optimizing a Trainium2 kernel written in Bass

```python
import math
import jax
import jax.numpy as jnp
from jax import lax
import numpy as np

D_MODEL = 1024
BATCH = 8
SEQ = 2048
DEPTH = 4

HEAD_DIM = 64
H_SB = 4
H_DIFF = 4
DIFF_QK_DIM = HEAD_DIM // 2
DIFF_V_DIM = HEAD_DIM
H_DSA = 4
IDX_HEADS = 8
IDX_DIM = 64
DSA_TOPK_MAX = 256
H_MOBA = 4
MOBA_BLOCK = 256
MOBA_TOPK = 3
N_BRANCH = 4
N_SOFTMAX_HEADS = H_DIFF + H_DSA + H_MOBA
N_BUCKETS = 32
MAX_DISTANCE = 128
Q_BLOCK = 128
D_FF = ((8 * D_MODEL + 3 * 256 - 1) // (3 * 256)) * 256
NORM_EPS = 1e-6
N_IN = (3 * H_SB * HEAD_DIM + H_DIFF * (4 * DIFF_QK_DIM + DIFF_V_DIM) + 3 * H_DSA * HEAD_DIM + IDX_HEADS * IDX_DIM + IDX_DIM + IDX_HEADS + 3 * H_MOBA * HEAD_DIM + N_BRANCH * D_MODEL)

kernel_name = 'hybrid_gated_sparse_trunk'


def _rms_norm(x, g):
    xf = x.astype(jnp.float32)
    y = xf * lax.rsqrt(jnp.mean(xf * xf, axis=-1, keepdims=True) + NORM_EPS)
    return (y * g.astype(jnp.float32)).astype(x.dtype)


def _split_sizes():
    hd = HEAD_DIM
    return ((H_SB * hd,) * 3
            + (H_DIFF * DIFF_QK_DIM,) * 4 + (H_DIFF * DIFF_V_DIM,)
            + (H_DSA * hd,) * 3 + (IDX_HEADS * IDX_DIM, IDX_DIM, IDX_HEADS)
            + (H_MOBA * hd,) * 3
            + (N_BRANCH * D_MODEL,))


def _split_columns(z):
    points, acc = [], 0
    for sz in _split_sizes()[:-1]:
        acc += sz
        points.append(acc)
    return jnp.split(z, points, axis=-1)


def _seq_blocks(t):
    b, s = t.shape[:2]
    t = t.reshape((b, s // Q_BLOCK, Q_BLOCK) + t.shape[2:])
    return jnp.moveaxis(t, 1, 0)


def _merge_blocks(t):
    t = jnp.moveaxis(t, 0, 1)
    return t.reshape((t.shape[0], t.shape[1] * t.shape[2]) + t.shape[3:])


def _t5_bucket(dist):
    max_exact = N_BUCKETS // 2
    d = jnp.maximum(dist, 0)
    log_ratio = jnp.log(jnp.maximum(d, 1).astype(jnp.float32) / max_exact) / math.log(MAX_DISTANCE / max_exact)
    large = jnp.minimum(max_exact + (log_ratio * (N_BUCKETS - max_exact)).astype(jnp.int32), N_BUCKETS - 1)
    return jnp.where(d < max_exact, d, large)


def _t5_bias(table, dist):
    return jnp.moveaxis(table.astype(jnp.float32)[_t5_bucket(dist)], -1, 0)


def _stick_breaking_attention(q, k, v):
    s = q.shape[1]
    pos = jnp.arange(s, dtype=jnp.int32)
    scale = HEAD_DIM ** -0.5

    def block(args):
        qb, qpos = args
        z = jnp.einsum('bqhd,bkhd->bhqk', qb, k).astype(jnp.float32) * scale
        strict = pos[None, :] < qpos[:, None]
        sp = jnp.where(strict, jax.nn.softplus(z), 0.0)
        tail = lax.cumsum(sp, axis=3, reverse=True) - sp
        a = jnp.where(strict, jnp.exp(jax.nn.log_sigmoid(z) - tail), 0.0)
        return jnp.einsum('bhqk,bkhd->bqhd', a.astype(v.dtype), v)

    return _merge_blocks(lax.map(block, (_seq_blocks(q), pos.reshape(-1, Q_BLOCK))))


def _differential_attention(q1, q2, k1, k2, v, lam, bias_table):
    s = q1.shape[1]
    pos = jnp.arange(s, dtype=jnp.int32)
    scale = DIFF_QK_DIM ** -0.5

    def block(args):
        qb1, qb2, qpos = args
        causal = pos[None, :] <= qpos[:, None]
        bias = _t5_bias(bias_table, qpos[:, None] - pos[None, :])

        def probs(qb, kk):
            lg = jnp.einsum('bqhd,bkhd->bhqk', qb, kk).astype(jnp.float32) * scale + bias
            return jax.nn.softmax(jnp.where(causal, lg, -jnp.inf), axis=-1)

        w = probs(qb1, k1) - lam * probs(qb2, k2)
        return jnp.einsum('bhqk,bkhd->bqhd', w.astype(v.dtype), v)

    return _merge_blocks(lax.map(block, (_seq_blocks(q1), _seq_blocks(q2), pos.reshape(-1, Q_BLOCK))))


def _dsa_attention(q, k, v, q_idx, k_idx, w_idx, bias_table):
    s = q.shape[1]
    pos = jnp.arange(s, dtype=jnp.int32)
    topk = min(DSA_TOPK_MAX, s // 4)
    scale = HEAD_DIM ** -0.5
    gather = jax.vmap(lambda t, i: t[i])

    def block(args):
        qb, qib, wb, qpos = args
        rel = jax.nn.relu(jnp.einsum('bqhd,bkd->bqhk', qib, k_idx).astype(jnp.float32) * IDX_DIM ** -0.5)
        score = jnp.einsum('bqh,bqhk->bqk', wb.astype(jnp.float32), rel)
        score = jnp.where((pos[None, :] <= qpos[:, None])[None], score, -jnp.inf)
        top_score, sel = lax.top_k(score, topk)
        valid = (top_score > -jnp.inf)[:, None]
        kg = gather(k, sel)
        vg = gather(v, sel)
        bias = jnp.moveaxis(bias_table.astype(jnp.float32)[_t5_bucket(qpos[None, :, None] - sel)], -1, 1)
        lg = jnp.einsum('bqhd,bqkhd->bhqk', qb, kg).astype(jnp.float32) * scale + bias
        p = jax.nn.softmax(jnp.where(valid, lg, -jnp.inf), axis=-1)
        return jnp.einsum('bhqk,bqkhd->bqhd', p.astype(v.dtype), vg)

    xs = (_seq_blocks(q), _seq_blocks(q_idx), _seq_blocks(w_idx), pos.reshape(-1, Q_BLOCK))
    return _merge_blocks(lax.map(block, xs))


def _moba_attention(q, k, v, bias_table):
    b, s, h, d = q.shape
    pos = jnp.arange(s, dtype=jnp.int32)
    nblk = -(-s // MOBA_BLOCK)
    pad = nblk * MOBA_BLOCK - s

    def to_blocks(t):
        t = jnp.pad(jnp.swapaxes(t, 1, 2), ((0, 0), (0, 0), (0, pad), (0, 0)))
        return t.reshape(b, h, nblk, MOBA_BLOCK, d)

    kb, vb = to_blocks(k), to_blocks(v)
    kmean = jnp.mean(kb, axis=3)
    topb = min(MOBA_TOPK, nblk - 1)
    head_ids = jnp.arange(h)
    in_blk = jnp.arange(MOBA_BLOCK, dtype=jnp.int32)
    scale = d ** -0.5
    gather = jax.vmap(jax.vmap(lambda t, i: t[i]))
    table_t = bias_table.astype(jnp.float32).T

    def block(args):
        qb, qpos = args
        own = qpos[0] // MOBA_BLOCK
        k_own = lax.dynamic_index_in_dim(kb, own, axis=2, keepdims=False)
        v_own = lax.dynamic_index_in_dim(vb, own, axis=2, keepdims=False)
        own_pos = own * MOBA_BLOCK + in_blk
        lg_own = (jnp.einsum('bqhd,bhkd->bhqk', qb, k_own).astype(jnp.float32) * scale
                  + _t5_bias(bias_table, qpos[:, None] - own_pos[None, :]))
        lg_own = jnp.where(own_pos[None, :] <= qpos[:, None], lg_own, -jnp.inf)
        if topb == 0:
            p = jax.nn.softmax(lg_own, axis=-1)
            return jnp.einsum('bhqk,bhkd->bqhd', p.astype(v.dtype), v_own)
        gate = jnp.einsum('bqhd,bhnd->bhqn', qb, kmean).astype(jnp.float32)
        gate = jnp.where(jnp.arange(nblk) < own, gate, -jnp.inf)
        g_score, sel = lax.top_k(gate, topb)
        valid = (g_score > -jnp.inf)[..., None]
        kg = gather(kb, sel)
        vg = gather(vb, sel)
        past_pos = sel[..., None] * MOBA_BLOCK + in_blk
        bias_past = table_t[head_ids[None, :, None, None, None], _t5_bucket(qpos[None, None, :, None, None] - past_pos)]
        lg_past = jnp.einsum('bqhd,bhqnkd->bhqnk', qb, kg).astype(jnp.float32) * scale + bias_past
        lg_past = jnp.where(valid, lg_past, -jnp.inf)
        nq = qb.shape[1]
        p = jax.nn.softmax(jnp.concatenate([lg_past.reshape(b, h, nq, topb * MOBA_BLOCK), lg_own], axis=-1), axis=-1)
        p_past = p[..., :topb * MOBA_BLOCK].reshape(b, h, nq, topb, MOBA_BLOCK).astype(v.dtype)
        p_own = p[..., topb * MOBA_BLOCK:].astype(v.dtype)
        return (jnp.einsum('bhqnk,bhqnkd->bqhd', p_past, vg)
                + jnp.einsum('bhqk,bhkd->bqhd', p_own, v_own))

    return _merge_blocks(lax.map(block, (_seq_blocks(q), pos.reshape(-1, Q_BLOCK))))


def _token_mixers(h, w_in, w_br_sb, w_br_diff, w_br_dsa, w_br_moba, w_out, lam, lam_init, diff_subln_g, rel_bias):
    b, s, _ = h.shape
    (q_sb, k_sb, v_sb, q1, q2, k1, k2, v_df, q_ds, k_ds, v_ds, qi_ds, ki_ds, wi_ds,
     q_mb, k_mb, v_mb, gate_logits) = _split_columns(h @ w_in)

    def heads(t, n):
        return t.reshape(b, s, n, -1)

    def flat(t):
        return t.reshape(b, s, -1)

    bias_df = rel_bias[:, :H_DIFF]
    bias_ds = rel_bias[:, H_DIFF:H_DIFF + H_DSA]
    bias_mb = rel_bias[:, H_DIFF + H_DSA:]
    o_sb = _stick_breaking_attention(heads(q_sb, H_SB), heads(k_sb, H_SB), heads(v_sb, H_SB))
    o_df = _differential_attention(heads(q1, H_DIFF), heads(q2, H_DIFF), heads(k1, H_DIFF), heads(k2, H_DIFF),
                                   heads(v_df, H_DIFF), lam, bias_df)
    o_df = _rms_norm(o_df, diff_subln_g) * (1.0 - lam_init)
    o_ds = _dsa_attention(heads(q_ds, H_DSA), heads(k_ds, H_DSA), heads(v_ds, H_DSA),
                          heads(qi_ds, IDX_HEADS), ki_ds, wi_ds * IDX_HEADS ** -0.5, bias_ds)
    o_mb = _moba_attention(heads(q_mb, H_MOBA), heads(k_mb, H_MOBA), heads(v_mb, H_MOBA), bias_mb)
    g = jax.nn.sigmoid(gate_logits.astype(jnp.float32)).astype(h.dtype).reshape(b, s, N_BRANCH, D_MODEL)
    merged = (g[:, :, 0] * (flat(o_sb) @ w_br_sb)
              + g[:, :, 1] * (flat(o_df) @ w_br_diff)
              + g[:, :, 2] * (flat(o_ds) @ w_br_dsa)
              + g[:, :, 3] * (flat(o_mb) @ w_br_moba))
    return merged @ w_out


def _swiglu(h, w_ffn_in, w_ffn_out):
    gate, up = jnp.split(h @ w_ffn_in, 2, axis=-1)
    return (jax.nn.silu(gate) * up) @ w_ffn_out


def setup_inputs(seed: int = 0) -> dict:
    key = jax.random.key(seed)
    ks = jax.random.split(key, 19)
    f32 = jnp.float32

    def nrm(k, shape, fan_in):
        return jax.random.normal(k, shape, f32) * fan_in ** -0.5

    def gain(k, shape):
        return 1.0 + 0.05 * jax.random.normal(k, shape, f32)

    br_in = HEAD_DIM * 4
    return {
        'x': jax.random.normal(ks[0], (BATCH, SEQ, D_MODEL), f32),
        'w_in': nrm(ks[1], (DEPTH, D_MODEL, N_IN), D_MODEL),
        'w_br_sb': nrm(ks[2], (DEPTH, H_SB * HEAD_DIM, D_MODEL), br_in),
        'w_br_diff': nrm(ks[3], (DEPTH, H_DIFF * DIFF_V_DIM, D_MODEL), br_in),
        'w_br_dsa': nrm(ks[4], (DEPTH, H_DSA * HEAD_DIM, D_MODEL), br_in),
        'w_br_moba': nrm(ks[5], (DEPTH, H_MOBA * HEAD_DIM, D_MODEL), br_in),
        'w_out': nrm(ks[6], (DEPTH, D_MODEL, D_MODEL), D_MODEL),
        'lambda_q1': 0.1 * jax.random.normal(ks[7], (DEPTH, DIFF_QK_DIM), f32),
        'lambda_k1': 0.1 * jax.random.normal(ks[8], (DEPTH, DIFF_QK_DIM), f32),
        'lambda_q2': 0.1 * jax.random.normal(ks[9], (DEPTH, DIFF_QK_DIM), f32),
        'lambda_k2': 0.1 * jax.random.normal(ks[10], (DEPTH, DIFF_QK_DIM), f32),
        'diff_subln_g': gain(ks[11], (DEPTH, DIFF_V_DIM)),
        'rel_bias': 0.1 * jax.random.normal(ks[12], (N_BUCKETS, N_SOFTMAX_HEADS), f32),
        'w_ffn_in': nrm(ks[13], (DEPTH, D_MODEL, 2 * D_FF), D_MODEL),
        'w_ffn_out': nrm(ks[14], (DEPTH, D_FF, D_MODEL), D_FF),
        'g_pre_mix': gain(ks[15], (DEPTH, D_MODEL)),
        'g_post_mix': gain(ks[16], (DEPTH, D_MODEL)),
        'g_pre_ffn': gain(ks[17], (DEPTH, D_MODEL)),
        'g_post_ffn': gain(ks[18], (DEPTH, D_MODEL)),
    }


def reference(x, w_in, w_br_sb, w_br_diff, w_br_dsa, w_br_moba, w_out, lambda_q1, lambda_k1, lambda_q2, lambda_k2,
              diff_subln_g, rel_bias, w_ffn_in, w_ffn_out, g_pre_mix, g_post_mix, g_pre_ffn, g_post_ffn):
    f32 = jnp.float32
    for l in range(DEPTH):
        lam_init = 0.8 - 0.6 * math.exp(-0.3 * l)
        lam = (jnp.exp(jnp.sum(lambda_q1[l].astype(f32) * lambda_k1[l].astype(f32)))
               - jnp.exp(jnp.sum(lambda_q2[l].astype(f32) * lambda_k2[l].astype(f32))) + lam_init)
        h = _rms_norm(x, g_pre_mix[l])
        y = _token_mixers(h, w_in[l], w_br_sb[l], w_br_diff[l], w_br_dsa[l], w_br_moba[l], w_out[l],
                          lam, lam_init, diff_subln_g[l], rel_bias)
        x = x + _rms_norm(y, g_post_mix[l])
        h = _rms_norm(x, g_pre_ffn[l])
        x = x + _rms_norm(_swiglu(h, w_ffn_in[l], w_ffn_out[l]), g_post_ffn[l])
    return x
```

```python
import math
from contextlib import ExitStack
import numpy as np
import concourse.bass as bass
import concourse.mybir as mybir
from concourse.bass_utils import run_bass_kernel_spmd

F32 = mybir.dt.float32
F32R = mybir.dt.float32r
BF16 = mybir.dt.bfloat16
ALU = mybir.AluOpType
AF = mybir.ActivationFunctionType
AX = mybir.AxisListType
NS = 8
ENGS = ['pe', 'act', 'dve', 'pool', 'sp']

D = 1024
S = 2048
DEPTH = 4
NIN = 7752
DFF = 2816
EPS = 1e-6
NEG = -30000.0


class Prog:
    def __init__(self, nc):
        self.nc = nc
        self.ops = []
        self.lastw = {}
        self.readers = {}
        self.fence = {}

    def add(self, eng, fn, r=(), w=(), nofence=False):
        i = len(self.ops)
        deps = set()
        for k in r:
            if k in self.lastw:
                deps.add(self.lastw[k])
        for k in w:
            if k in self.lastw:
                deps.add(self.lastw[k])
            deps.update(self.readers.get(k, ()))
        if eng in self.fence and not nofence:
            deps.update(self.fence.pop(eng))
        red = {}
        out = set()
        for d in deps:
            e = self.ops[d][0]
            if e == 'sp':
                out.add(d)
            else:
                red[e] = max(red.get(e, -1), d)
        out.update(red.values())
        for k in r:
            self.readers.setdefault(k, []).append(i)
        for k in w:
            self.lastw[k] = i
            self.readers[k] = []
        self.ops.append((eng, fn, out))
        return i

    def do_fence(self):
        last = {}
        for i, (e, _, _) in enumerate(self.ops):
            if e == 'sp':
                last.setdefault(e, []).append(i)
                last[e] = last[e][-NS:]
            else:
                last[e] = [i]
        allidx = set(i for v in last.values() for i in v)
        for e in ENGS:
            self.fence[e] = set(allidx)

    def emit(self, stack):
        nc = self.nc
        ops = self.ops
        need = [False] * len(ops)
        for (e, fn, deps) in ops:
            for d in deps:
                if ops[d][0] == 'pe' and e == 'pe':
                    continue
                need[d] = True
        for i, (e, _, _) in enumerate(ops):
            if e == 'sp':
                need[i] = True
        sig = {}
        cnt = {}
        ndma = 0
        extra = {}
        for i, (e, fn, deps) in enumerate(ops):
            if not need[i]:
                continue
            if e == 'sp':
                s = ('sp', ndma % NS)
                ndma += 1
                c = cnt.get(s, 0)
                if c > 0:
                    extra[i] = (s, c)
                cnt[s] = c + 16
                sig[i] = (s, c + 16)
            else:
                s = (e, 0)
                cnt[s] = cnt.get(s, 0) + 1
                sig[i] = (s, cnt[s])
        sems = {}
        for s in cnt:
            sems[s] = stack.enter_context(nc.semaphore("sem_%s_%d" % s))
        idxs = {e: [] for e in ENGS}
        for i, (e, _, _) in enumerate(ops):
            idxs[e].append(i)

        def mk(e):
            def body(eng):
                waited = {}
                for i in idxs[e]:
                    _, fn, deps = ops[i]
                    ws = {}
                    for d in deps:
                        if ops[d][0] == 'pe' and e == 'pe':
                            continue
                        s, c = sig[d]
                        if waited.get(s, 0) < c:
                            ws[s] = max(ws.get(s, 0), c)
                    if i in extra:
                        s, c = extra[i]
                        if waited.get(s, 0) < c:
                            ws[s] = max(ws.get(s, 0), c)
                    for s, c in ws.items():
                        eng.wait_ge(sems[s], c)
                        waited[s] = c
                    ins = fn(eng)
                    if i in sig:
                        ins.then_inc(sems[sig[i][0]], 16 if e == 'sp' else 1)
                if e == 'sp':
                    for s, c in cnt.items():
                        if s[0] == 'sp' and waited.get(s, 0) < c:
                            eng.wait_ge(sems[s], c)
            return body

        with nc.Block() as block:
            block.tensor(mk('pe'))
            block.scalar(mk('act'))
            block.vector(mk('dve'))
            block.gpsimd(mk('pool'))
            block.sync(mk('sp'))


def _t5_bucket_np(d):
    d = np.maximum(d, 0)
    lr = np.log(np.maximum(d, 1).astype(np.float32) / np.float32(16)) / np.float32(math.log(128 / 16))
    large = np.minimum(16 + (lr * 16).astype(np.int32), 31)
    return np.where(d < 16, d, large)


def build_program(layers, dbg=None):
    nc = bass.Bass("TRN2", target_bir_lowering=False)
    dr = {}

    def din(name, shape):
        dr[name] = nc.dram_tensor(name, list(shape), F32, kind="ExternalInput").ap()
        return dr[name]

    xT_d = din("xT", [D, S])
    w_in = din("w_in", [DEPTH, D, NIN])
    w_br = din("w_br", [DEPTH, 4, 256, D])
    w_out = din("w_out", [DEPTH, D, D])
    w_f1 = din("w_ffn_in", [DEPTH, D, 2 * DFF])
    w_f2 = din("w_ffn_out", [DEPTH, DFF, D])
    gains_d = din("gains", [128, 4 * DEPTH * 8])
    strips_d = din("strips", [12, 128, 640])
    c31_d = din("c31", [128, 12])
    lamv_d = din("lamv", [128, DEPTH * 4 * 32])
    subg_d = din("subg", [128, DEPTH])
    ident_d = din("ident", [128, 128])
    negT_d = din("negT", [128, 128])
    sbmask_d = din("sbmask", [128, 128])
    cmaskq_d = din("cmaskq", [128, 128])
    pastmask_d = din("pastmask", [128, 8 * 32])
    sel_d = din("sel", [32, 32 * 128])
    headmask_d = din("headmask", [128, 8])
    pow2_d = din("pow2tab", [128, 24])
    outT_d = nc.dram_tensor("outT", [D, S], F32, kind="ExternalOutput").ap()
    xpark = nc.dram_tensor("xpark", [128, 8 * S], F32, kind="Internal").ap()
    dbg_out = {}
    if dbg:
        for n in dbg:
            dbg_out[n] = nc.dram_tensor("dbg_" + n, [D, S], F32, kind="ExternalOutput").ap()

    st = ExitStack()
    NW = 51384
    big = st.enter_context(nc.sbuf_tensor("SB", [128, NW], F32))
    r_sp = [st.enter_context(nc.sbuf_tensor("r_sp%d" % i, [128, 512], F32R)) for i in range(2)]
    r_c = st.enter_context(nc.sbuf_tensor("r_c", [128, 512], F32R))
    r_negT = st.enter_context(nc.sbuf_tensor("r_negT", [128, 128], F32R))
    r_negOnes = st.enter_context(nc.sbuf_tensor("r_negOnes", [128, 128], F32R))
    thr_t = st.enter_context(nc.sbuf_tensor("thrAll", [128, 16], F32))
    thrAll = thr_t[:, :]
    psb = [st.enter_context(nc.psum_tensor("ps%d" % i, [128, 512], F32)) for i in range(7)]
    psT = st.enter_context(nc.psum_tensor("psT", [128, 1024], BF16))

    def view(off, shape, dt):
        assert off % 4 == 0
        n = int(np.prod(shape[1:]))
        if dt == F32:
            assert off // 4 + n <= NW, (off, shape)
            ap = big[:shape[0], off // 4: off // 4 + n]
        else:
            assert n % 2 == 0 and off // 4 + n // 2 <= NW, (off, shape)
            ap = big[:shape[0], off // 4: off // 4 + n // 2].bitcast(BF16)
        if len(shape) == 3:
            ap = ap.rearrange("p (a b) -> p a b", a=shape[1])
        elif len(shape) == 4:
            ap = ap.rearrange("p (a b c) -> p a b c", a=shape[1], b=shape[2])
        return ap

    A0, B0, C0, D0, E0 = 0, 65536, 98304, 131072, 163840
    xT = view(A0, [128, 8, S], F32)
    hT = view(B0, [128, 8, S], BF16)
    oT = view(C0, [128, 8, S], BF16)
    mT = view(D0, [128, 8, S], BF16)
    eo = [E0]

    def ealloc(shape, dt):
        n = int(np.prod(shape[1:])) * (4 if dt == F32 else 2)
        n = (n + 31) // 32 * 32
        v = view(eo[0], shape, dt)
        eo[0] += n
        return v

    wst = [ealloc([128, 8, 256], F32) for _ in range(2)]
    wbf = [ealloc([128, 8, 256], BF16) for _ in range(3)]
    identb = ealloc([128, 128], BF16)
    onesF = ealloc([128, 128], F32)
    negTr = r_negT[:, :]
    negOnesr = r_negOnes[:, :]
    sbmaskb = ealloc([128, 128], BF16)
    gains = ealloc([128, 4 * DEPTH * 8], F32)
    c31 = ealloc([128, 12], F32)
    subg = ealloc([128, DEPTH], F32)
    headmask = ealloc([128, 8], F32)
    pow2tab = ealloc([128, 24], F32)
    stripb = ealloc([128, 4, 640], BF16)
    rstd = ealloc([128, 512], F32)
    sqb = [ealloc([128, 512], F32) for _ in range(2)]
    lnt = sqb[0]
    SPb = [r_sp[0][:, :], r_sp[1][:, :]]
    Cb = r_c[:, :]
    assert eo[0] <= NW * 4, eo[0]

    P = Prog(nc)
    cnt = {'ps': 0, 'acc': 0, 'wst': 0, 'wbf': 0, 'ev': 0}
    pending = []
    acc_pending = {5: 0, 6: 0}

    def flush_pending():
        while pending:
            pending.pop(0)()

    def flush_one():
        if pending:
            pending.pop(0)()

    def reg_fin(accb, fn):
        acc_pending[accb] += 1

        def w_():
            fn()
            acc_pending[accb] -= 1
        pending.append(w_)

    live = set()

    def nb():
        for _ in range(5):
            cnt['ps'] += 1
            b = cnt['ps'] % 5
            if b not in live:
                live.add(b)
                return b
        raise AssertionError("no free PSUM bank")

    _orig_add = P.add

    def _add2(eng, fn, r=(), w=(), nofence=False):
        if eng != 'pe':
            for k in w:
                if isinstance(k, tuple) and len(k) == 2 and k[0] == 'ps' and k[1] in live:
                    live.discard(k[1])
        return _orig_add(eng, fn, r=r, w=w, nofence=nofence)
    P.add = _add2

    def nacc():
        cnt['acc'] += 1
        b = 5 + cnt['acc'] % 2
        if acc_pending[b] > 0:
            flush_pending()
        return b

    def PS(b):
        return 'psT' if b == 'T' else ('ps', b)

    def qs(qc, c0=0):
        return slice(512 * qc + c0, 512 * (qc + 1))

    def gcol(gt, l, c):
        i = (gt * DEPTH + l) * 8 + c
        return gains[:, i:i + 1]

    def dma(out, in_, r=(), w=(), nofence=False):
        P.add('sp', lambda e: e.dma_start(out=out, in_=in_), r=r, w=w, nofence=nofence)

    def mm(out, lhsT, rhs, start, stop, r=(), w=(), sgc=False, nofence=False):
        P.add('pe', lambda e: e.matmul(out, lhsT=lhsT, rhs=rhs, start=start, stop=stop, skip_group_check=sgc), r=r, w=w, nofence=nofence)

    def evac(dst, src, dk, b, scale=None, eng=None):
        cnt['ev'] += 1
        if eng is None:
            eng = 'act' if cnt['ev'] % 2 == 0 else 'dve'
        if eng == 'act':
            if scale is None:
                P.add('act', lambda e: e.copy(out=dst, in_=src), w=[PS(b)] + list(dk))
            else:
                P.add('act', lambda e: e.mul(out=dst, in_=src, mul=scale), w=[PS(b)] + list(dk))
        else:
            if scale is None:
                P.add('dve', lambda e: e.tensor_copy(out=dst, in_=src), w=[PS(b)] + list(dk))
            else:
                P.add('dve', lambda e: e.tensor_scalar(out=dst, in0=src, scalar1=scale, scalar2=None, op0=ALU.mult), w=[PS(b)] + list(dk))

    def load_w(src, nk, ncols, eng='act'):
        s = cnt['wst'] % 2
        cnt['wst'] += 1
        b = cnt['wbf'] % 3
        cnt['wbf'] += 1
        stg = wst[s][:, 0:nk, 0:ncols]
        dma(stg, src.rearrange("(c p) n -> p c n", p=128), w=[('wst', s)], nofence=True)
        dst = wbf[b][:, 0:nk, 0:ncols]
        if eng == 'act':
            P.add('act', lambda e: e.copy(out=dst, in_=stg), r=[('wst', s)], w=[('wbf', b)], nofence=True)
        else:
            P.add(eng, lambda e: e.tensor_copy(out=dst, in_=stg), r=[('wst', s)], w=[('wbf', b)], nofence=True)
        return wbf[b], ('wbf', b)

    def load_const_f32(dst, src, key):
        dma(dst, src, w=[key])

    def load_const_bf16(dst, src, key, shape):
        s = cnt['wst'] % 2
        cnt['wst'] += 1
        n = int(np.prod(shape[1:]))
        stg = wst[s].rearrange("p a b -> p (a b)")[:shape[0], 0:n]
        if len(shape) == 3:
            stg = stg.rearrange("p (a b) -> p a b", a=shape[1])
        dma(stg, src, w=[('wst', s)])
        P.add('pool', lambda e: e.tensor_copy(out=dst, in_=stg), r=[('wst', s)], w=[key])

    load_const_bf16(identb, ident_d, 'identb', [128, 128])
    load_const_bf16(sbmaskb, sbmask_d, 'sbmaskb', [128, 128])
    load_const_f32(gains, gains_d, 'gains')
    load_const_f32(c31, c31_d, 'c31')
    load_const_f32(subg, subg_d, 'subg')
    load_const_f32(headmask, headmask_d, 'headmask')
    load_const_f32(pow2tab, pow2_d, 'pow2tab')
    P.add('dve', lambda e: e.memset(onesF, 1.0), w=['onesF'])
    _s = cnt['wst'] % 2
    cnt['wst'] += 1
    _stg = wst[_s].rearrange("p a b -> p (a b)")[:, 0:128]
    dma(_stg, negT_d, w=[('wst', _s)])
    P.add('dve', lambda e: e.tensor_copy(out=negTr, in_=_stg), r=[('wst', _s)], w=['negTr'])
    P.add('dve', lambda e: e.tensor_scalar(out=negOnesr, in0=onesF, scalar1=-1.0, scalar2=None, op0=ALU.mult), r=['onesF'], w=['negOnesr'])
    for c in range(8):
        dma(xT[:, c, :], xT_d[c * 128:(c + 1) * 128, :], w=[('x', c, q) for q in range(4)])

    def rmsnorm_to_h(l, gt):
        for qc in range(4):
            b = nb()
            for c in range(8):
                sq = sqb[c % 2]
                xs = xT[:, c, qs(qc)]
                P.add('act', lambda e, sq=sq, xs=xs: e.activation(out=sq, in_=xs, func=AF.Square), r=[('x', c, qc)], w=[('sq', c % 2)])
                mm(psb[b][:, :], onesF, sq, c == 0, c == 7, r=[('sq', c % 2), 'onesF'], w=[PS(b)])
            P.add('act', lambda e, b=b: e.activation(out=lnt, in_=psb[b][:, :], func=AF.Ln, scale=1.0 / D, bias=EPS), w=[PS(b), ('sq', 0)])
            P.add('act', lambda e: e.activation(out=rstd, in_=lnt, func=AF.Exp, scale=-0.5), r=[('sq', 0)], w=['rstd'])
            for c in range(8):
                xs = xT[:, c, qs(qc)]
                hs = hT[:, c, qs(qc)]
                g = gcol(gt, l, c)
                P.add('dve', lambda e, xs=xs, hs=hs, g=g: e.scalar_tensor_tensor(out=hs, in0=xs, scalar=g, in1=rstd, op0=ALU.mult, op1=ALU.mult),
                      r=[('x', c, qc), 'rstd', 'gains'], w=[('h', c, qc)])

    def projT(l, col0, ncols_total, dst_fn, scale=None):
        for t0 in range(0, ncols_total, 256):
            ncl = min(256, ncols_total - t0)
            w, wk = load_w(w_in[l, :, col0 + t0: col0 + t0 + ncl], 8, ncl)
            for jj in range(0, ncl, 128):
                m = min(128, ncl - jj)
                for qc in range(4):
                    b = nb()
                    for c in range(8):
                        mm(psb[b][0:m, :], w[:, c, jj:jj + m], hT[:, c, qs(qc)], c == 0, c == 7, r=[wk, ('h', c, qc)], w=[PS(b)], nofence=True)
                    for (dst, dk, rows) in dst_fn((t0 + jj) // 128, qc):
                        evac(dst, psb[b][rows, :], dk, b, scale)

    def projTok(l, col0, ncols, dst_fn):
        w, wk = load_w(w_in[l, :, col0: col0 + ncols], 8, ncols)
        for tt in range(16):
            b = nb()
            for c in range(8):
                mm(psb[b][:, 0:ncols], hT[:, c, tt * 128:(tt + 1) * 128], w[:, c, 0:ncols], c == 0, c == 7, r=[wk, ('h', c, tt // 4)], w=[PS(b)], nofence=True)
            dst, dk, src = dst_fn(tt, psb[b])
            evac(dst, src, dk, b)

    def attn_soft_qc(*a, **k):
        holder = []
        for _ in attn_soft_qc_g(*a, holder=holder, **k):
            pass
        return holder[0]

    def attn_soft_qc_g(qc, kfn, qfn, kkeys, qkeys, strip, c31col, vfn, vkey, Pbuf, extra_fn=None, extra_keys=(), vkeyfn=None, holder=None):
        accb = nacc()
        holder.append(accb)
        last = 4 * qc + 3
        info = {}

        def stA(kb):
            ks = kb * 128
            c0 = max(0, ks - 512 * qc)
            n = 512 - c0
            near = kb >= 4 * qc - 1
            b1 = nb()
            mms = [(psb[b1][:, c0:512], kfn(kb), qfn(qc, c0), list(kkeys) + list(qkeys))]
            if near:
                r0 = 512 * qc + c0 - ks
                mms.append((psb[b1][:, c0:512], identb, strip[:, r0:r0 + n], ['identb', 'strip']))
            if extra_fn is not None:
                mms += extra_fn(b1, qc, kb, c0)
            for i, (o_, lt, rh, rk) in enumerate(mms):
                mm(o_, lt, rh, i == 0, i == len(mms) - 1, r=rk + list(extra_keys), w=[PS(b1)], sgc=True)
            info[kb] = (b1, c0, near)

        def stB(kb):
            b1, c0, near = info[kb]
            pi = kb % len(Pbuf)
            Pt = Pbuf[pi]
            if near:
                P.add('act', lambda e, Pt=Pt, b1=b1, c0=c0: e.activation(out=Pt[:, c0:512], in_=psb[b1][:, c0:512], func=AF.Exp),
                      w=[PS(b1), ('P', pi)])
            else:
                P.add('act', lambda e, Pt=Pt, b1=b1, c0=c0: e.activation(out=Pt[:, c0:512], in_=psb[b1][:, c0:512], func=AF.Exp, bias=c31col),
                      r=['c31'], w=[PS(b1), ('P', pi)])

        def stC(kb):
            b1, c0, near = info[kb]
            pi = kb % len(Pbuf)
            Pt = Pbuf[pi]
            mm(psb[accb][0:65, c0:512], vfn(kb), Pt[:, c0:512], kb == 0, kb == last, r=[('P', pi), vkeyfn(kb)], w=[PS(accb)], sgc=True)

        stA(0)
        if last >= 1:
            stA(1)
        for kb in range(0, last + 1):
            if kb + 2 <= last:
                stA(kb + 2)
            stB(kb)
            if kb >= 1:
                stC(kb - 1)
            flush_one()
            yield
        stC(last)

    e64h = [None]

    def setup_e64(alloc_fn, bcs):
        e64 = alloc_fn([128, 128], F32)
        e64h[0] = e64
        P.add('dve', lambda e: e.memset(e64, 0.0), w=['e64'])
        P.add('dve', lambda e: e.memset(e64[64:65, :], 1.0), w=['e64'])
        P.add('dve', lambda e: e.memset(bcs, 0.0), w=['bcs', 'rrec'])

    def soft_norm_stages(accb, rrec, bcs):
        def s1():
            P.add('dve', lambda e: e.reciprocal(out=rrec[64:65, :], in_=psb[accb][64:65, :]), w=[PS(accb), 'rrec'])

        def s2():
            bb = nb()
            mm(psb[bb][:, :], e64h[0], bcs[:, :], True, True, r=['rrec', 'bcs', 'e64'], w=[PS(bb)])
            P.add('act', lambda e: e.copy(out=bcs[0:64, :], in_=psb[bb][0:64, :]), w=[PS(bb), 'bcs'])
        return [s1, s2]

    def interleave(items):
        st_ = [[g, max(1, est), 0] for g, est in items]
        while st_:
            st_.sort(key=lambda t: t[2] / t[1])
            t = st_[0]
            try:
                next(t[0])
                t[2] += 1
            except StopIteration:
                st_.remove(t)

    def run_gen(g):
        for _ in g:
            pass

    def score_tile_g(qt, scbuf, skey, qiT, kiT, wi, rl, itc, qimb, every=2):
        L = (qt + 1) * 128
        step = 0
        for ih in range(8):
            ich, ir0 = ih // 2, (ih % 2) * 64
            for kc in range((L + 511) // 512):
                n = min(512, L - kc * 512)
                if kc == 0:
                    qi2 = itc[1] % 2
                    itc[1] += 1
                    qim_ = qimb[qi2]
                    P.add('dve', lambda e, qim_=qim_, ich=ich, ih=ih: e.tensor_scalar(out=qim_, in0=qiT[:, ich, qt * 128:(qt + 1) * 128], scalar1=headmask[:, 4 + ih % 2:5 + ih % 2], scalar2=None, op0=ALU.mult),
                          r=[('qi', ich, qt // 4), 'headmask'], w=[('qim', qi2)])
                b = nb()
                mm(psb[b][:, 0:n], qim_, kiT[:, kc * 512:kc * 512 + n], True, True,
                   r=[('qim', qi2), ('ki', kc)], w=[PS(b)])
                i2 = itc[0] % 2
                itc[0] += 1
                rt = rl[i2]
                P.add('act', lambda e, rt=rt, b=b, n=n: e.activation(out=rt[:, 0:n], in_=psb[b][:, 0:n], func=AF.Relu), w=[PS(b), ('rl', i2)])
                sc = scbuf[:, kc * 512:kc * 512 + n]
                wcol = wi[:, qt, ih:ih + 1]
                if ih == 0:
                    P.add('dve', lambda e, sc=sc, rt=rt, n=n, wcol=wcol: e.tensor_scalar(out=sc, in0=rt[:, 0:n], scalar1=wcol, scalar2=None, op0=ALU.mult),
                          r=[('rl', i2), ('wi', qt)], w=[(skey, kc)])
                else:
                    P.add('dve', lambda e, sc=sc, rt=rt, n=n, wcol=wcol: e.scalar_tensor_tensor(out=sc, in0=rt[:, 0:n], scalar=wcol, in1=sc, op0=ALU.mult, op1=ALU.add),
                          r=[('rl', i2), ('wi', qt)], w=[(skey, kc)])
                step += 1
                if step % every == 0:
                    yield
        yield

    def score_steps(qt, every=2):
        L = (qt + 1) * 128
        return (8 * ((L + 511) // 512)) // every + 1

    def load_strips(h0):
        for j in range(4):
            load_const_bf16(stripb[:, j, :], strips_d[h0 + j], 'strip', [128, 640])

    for l in layers:
        lam_init = 0.8 - 0.6 * math.exp(-0.3 * l)
        rmsnorm_to_h(l, 0)
        if dbg and 'h' in dbg:
            pass
        for c in range(8):
            dma(xpark[:, c * S:(c + 1) * S], xT[:, c, :], r=[('x', c, q) for q in range(4)])
        P.do_fence()

        o = [A0]

        def aalloc(shape, dt, o=o):
            n = int(np.prod(shape[1:])) * (4 if dt == F32 else 2)
            n = (n + 31) // 32 * 32
            v = view(o[0], shape, dt)
            o[0] += n
            assert o[0] <= B0
            return v

        qT = aalloc([128, 2, S], BF16)
        kT = aalloc([128, 2, S], BF16)
        Vt = aalloc([128, 16, 256], BF16)
        Eb = [aalloc([128, 512], F32) for _ in range(2)]
        Ab = [aalloc([128, 512], BF16) for _ in range(2)]
        projT(l, 0, 256, lambda j, qc: [(qT[:, j, qs(qc)], [('q', j, qc)], slice(0, 128))], scale=0.125)
        projT(l, 256, 256, lambda j, qc: [(kT[:, j, qs(qc)], [('k', j, qc)], slice(0, 128))])
        projTok(l, 512, 256, lambda tt, ps: (Vt[:, tt, :], [('V', tt)], ps[:, 0:256]))
        x_qiT = aalloc([128, 4, S], BF16)
        x_kiT = aalloc([128, S], BF16)
        x_wi = aalloc([128, 16, 8], F32)
        x_rl = [aalloc([128, 512], F32) for _ in range(2)]
        x_qimb = [aalloc([128, 128], BF16) for _ in range(2)]
        sbqm = [aalloc([128, 512], BF16) for _ in range(2)]
        o2 = [D0]

        def dalloc(shape, dt, o2=o2):
            n = int(np.prod(shape[1:])) * (4 if dt == F32 else 2)
            n = (n + 31) // 32 * 32
            v = view(o2[0], shape, dt)
            o2[0] += n
            assert o2[0] <= E0
            return v
        x_scb = [dalloc([128, S], F32) for _ in range(2)]
        x_junk = [dalloc([128, S], BF16) for _ in range(2)]
        NIT = 16
        mids = [dalloc([128, NIT + 2], F32) for _ in range(2)]
        hcols = [dalloc([128, NIT + 2], F32) for _ in range(2)]
        cntb = [dalloc([128, NIT + 2], F32) for _ in range(2)]
        rmm = [dalloc([128, 8], F32) for _ in range(2)]
        x_cmaskq = dalloc([128, 128], F32)
        dma(x_cmaskq, cmaskq_d, w=['cmaskq'])
        projT(l, 2304, 512, lambda j, qc: [(x_qiT[:, j, qs(qc)], [('qi', j, qc)], slice(0, 128))])
        projT(l, 2816, 64, lambda j, qc: [(x_kiT[0:64, qs(qc)], [('ki', qc)], slice(0, 64)), (x_kiT[64:128, qs(qc)], [('ki', qc)], slice(0, 64))])
        projTok(l, 2880, 8, lambda tt, ps: (x_wi[:, tt, :], [('wi', tt)], ps[:, 0:8]))

        def gen_index():
            itc = [0, 0]
            P.add('dve', lambda e: e.memset(thrAll[:, 0:2], -1e29), w=['thrAll'])
            yield
            for pr in range(1, 8):
                tiles = [2 * pr, 2 * pr + 1]
                for s_, qt in enumerate(tiles):
                    L = (qt + 1) * 128
                    scbuf = x_scb[s_]
                    yield from score_tile_g(qt, scbuf, ('score', s_), x_qiT, x_kiT, x_wi, x_rl, itc, x_qimb)
                    allsc = [(('score', s_), kc) for kc in range(4)]
                    P.add('dve', lambda e, s_=s_, L=L, scbuf=scbuf: e.tensor_reduce(out=rmm[s_][:, 0:1], in_=scbuf[:, 0:L], axis=AX.X, op=ALU.min), r=allsc, w=[('rmm', s_)])
                    P.add('dve', lambda e, s_=s_, L=L, scbuf=scbuf: e.tensor_reduce(out=rmm[s_][:, 1:2], in_=scbuf[:, 0:L], axis=AX.X, op=ALU.max), r=allsc, w=[('rmm', s_)])
                    dsl = scbuf[:, qt * 128:(qt + 1) * 128]
                    P.add('dve', lambda e, dsl=dsl: e.tensor_tensor(out=dsl, in0=dsl, in1=x_cmaskq, op=ALU.add), r=['cmaskq'], w=allsc)
                for s_ in range(2):
                    P.add('dve', lambda e, s_=s_: e.tensor_tensor(out=rmm[s_][:, 2:3], in0=rmm[s_][:, 1:2], in1=rmm[s_][:, 0:1], op=ALU.subtract), w=[('rmm', s_)])
                    P.add('dve', lambda e, s_=s_: e.tensor_scalar(out=hcols[s_][:, 0:NIT], in0=pow2tab[:, 0:NIT], scalar1=rmm[s_][:, 2:3], scalar2=None, op0=ALU.mult),
                          r=[('rmm', s_), 'pow2tab'], w=[('hc', s_)])
                    P.add('dve', lambda e, s_=s_: e.tensor_tensor(out=mids[s_][:, 0:1], in0=rmm[s_][:, 0:1], in1=hcols[s_][:, 0:1], op=ALU.add),
                          r=[('rmm', s_), ('hc', s_)], w=[('mid', s_)])
                yield
                for i_ in range(NIT):
                    for s_, qt in enumerate(tiles):
                        L = (qt + 1) * 128
                        scr = [(('score', s_), kc) for kc in range(4)]
                        if s_ == 0:
                            P.add('dve', lambda e, s_=s_, L=L, i_=i_: e.tensor_scalar(out=x_junk[s_][:, 0:L], in0=x_scb[s_][:, 0:L], scalar1=mids[s_][:, i_:i_ + 1], scalar2=0.0,
                                                                                   op0=ALU.is_ge, op1=ALU.add, accum_out=cntb[s_][:, i_:i_ + 1]),
                                  r=scr + [('mid', s_)], w=[('junk', s_), ('cnt', s_)])
                        else:
                            P.add('pool', lambda e, s_=s_, i_=i_: e.tensor_scalar(out=rmm[s_][:, 4:5], in0=mids[s_][:, i_:i_ + 1], scalar1=-1.0, scalar2=None, op0=ALU.mult),
                                  r=[('mid', s_)], w=[('nmid', s_)])
                            P.add('act', lambda e, s_=s_, L=L, i_=i_: e.activation(out=x_junk[s_][:, 0:L], in_=x_scb[s_][:, 0:L], func=AF.Sign, bias=rmm[s_][:, 4:5], scale=1.0,
                                                                                accum_out=cntb[s_][:, i_:i_ + 1]),
                                  r=scr + [('nmid', s_)], w=[('junk', s_), ('cnt', s_)])
                    for s_, qt in enumerate(tiles):
                        L = (qt + 1) * 128
                        cth = 255.5 if s_ == 0 else (510.5 - L)
                        sub_ = 0.5 if i_ < NIT - 1 else 1.0
                        P.add('pool', lambda e, s_=s_, i_=i_, cth=cth, sub_=sub_: e.tensor_scalar(out=rmm[s_][:, 3:4], in0=cntb[s_][:, i_:i_ + 1], scalar1=cth, scalar2=sub_,
                                                                                       op0=ALU.is_ge, op1=ALU.subtract), r=[('cnt', s_)], w=[('tmpb', s_)])
                    for s_ in range(2):
                        P.add('pool', lambda e, s_=s_, i_=i_: e.tensor_scalar(out=mids[s_][:, i_ + 1:i_ + 2], in0=rmm[s_][:, 3:4], scalar1=hcols[s_][:, i_:i_ + 1], scalar2=mids[s_][:, i_:i_ + 1],
                                                                           op0=ALU.mult, op1=ALU.add),
                              r=[('tmpb', s_), ('hc', s_)], w=[('mid', s_)])
                    yield
                for s_, qt in enumerate(tiles):
                    P.add('dve', lambda e, s_=s_, qt=qt: e.tensor_copy(out=thrAll[:, qt:qt + 1], in_=mids[s_][:, NIT:NIT + 1]), r=[('mid', s_)], w=['thrAll'])
                yield

        def gen_sb():
            for h in range(4):
                ch, r0 = h // 2, (h % 2) * 64
                for qc in range(4):
                    for j_ in range(4):
                        P.add('dve', lambda e, j_=j_: e.tensor_scalar(out=Cb[:, j_ * 128:(j_ + 1) * 128], in0=onesF, scalar1=0.0, scalar2=None, op0=ALU.mult), r=['onesF'], w=['C'])
                    last = 4 * qc + 3
                    accb = nacc()
                    qmi = (h * 4 + qc) % 2
                    qm_ = sbqm[qmi]
                    P.add('dve', lambda e, qm_=qm_, ch=ch, qc=qc, h=h: e.tensor_scalar(out=qm_, in0=qT[:, ch, qs(qc)], scalar1=headmask[:, 4 + h % 2:5 + h % 2], scalar2=None, op0=ALU.mult),
                          r=[('q', ch, qc), 'headmask'], w=[('sbqm', qmi)])
                    order = list(range(last, -1, -1))
                    n_ = len(order)
                    inf = {}

                    def geo(k, qc=qc, ch=ch, r0=r0, qm_=qm_, qmi=qmi):
                        kb = order[k]
                        ks = kb * 128
                        c0 = max(0, ks - 512 * qc)
                        diag = kb >= 4 * qc
                        lk = kT[:, ch, ks:ks + 128]
                        rq = qm_[:, c0:512]
                        rk = [('k', ch, kb // 4), ('sbqm', qmi)]
                        return kb, c0, diag, lk, rq, rk

                    def sA1(k):
                        kb, c0, diag, lk, rq, rk = geo(k)
                        b1 = nb()
                        inf[k] = b1
                        mm(psb[b1][:, c0:512], lk, rq, True, not diag, r=rk, w=[PS(b1)], sgc=True)
                        if diag:
                            mm(psb[b1][:, c0:c0 + 128], identb, sbmaskb, False, True, r=['identb', 'sbmaskb'], w=[PS(b1)], sgc=True)

                    def sB1(k):
                        kb, c0, diag, lk, rq, rk = geo(k)
                        b1 = inf[k]
                        i2 = k % 2
                        E, SPt = Eb[i2], SPb[i2]
                        P.add('act', lambda e, E=E, b1=b1, c0=c0: e.activation(out=E[:, c0:512], in_=psb[b1][:, c0:512], func=AF.Exp),
                              w=[PS(b1), ('E', i2)])
                        P.add('act', lambda e, E=E, SPt=SPt, c0=c0: e.activation(out=SPt[:, c0:512], in_=E[:, c0:512], func=AF.Ln, bias=1.0, scale=1.0),
                              r=[('E', i2)], w=[('SP', i2)])

                    def sA2(k):
                        kb, c0, diag, lk, rq, rk = geo(k)
                        i2 = k % 2
                        SPt = SPb[i2]
                        b2 = nb()
                        inf[('b2', k)] = b2
                        mm(psb[b2][:, c0:512], lk, rq, True, False, r=rk, w=[PS(b2)], sgc=True)
                        if diag:
                            mm(psb[b2][:, c0:c0 + 128], identb, sbmaskb, False, False, r=['identb', 'sbmaskb'], w=[PS(b2)], sgc=True)
                        mm(psb[b2][:, c0:512], negTr, SPt[:, c0:512], False, k == 0, r=[('SP', i2), 'negTr'], w=[PS(b2)], sgc=True)
                        if k != 0:
                            mm(psb[b2][:, c0:512], negOnesr, Cb[:, c0:512], False, True, r=['C', 'negOnesr'], w=[PS(b2)], sgc=True)

                    def sB2(k):
                        kb, c0, diag, lk, rq, rk = geo(k)
                        i2 = k % 2
                        At = Ab[i2]
                        b2 = inf[('b2', k)]
                        P.add('act', lambda e, At=At, b2=b2, c0=c0: e.activation(out=At[:, c0:512], in_=psb[b2][:, c0:512], func=AF.Exp),
                              w=[PS(b2), ('A', i2)])

                    def sC(k):
                        kb, c0, diag, lk, rq, rk = geo(k)
                        i2 = k % 2
                        SPt = SPb[i2]
                        if k != n_ - 1:
                            P.add('pool', lambda e, SPt=SPt, c0=c0: e.tensor_tensor(out=Cb[:, c0:512], in0=Cb[:, c0:512].bitcast(F32), in1=SPt[:, c0:512].bitcast(F32), op=ALU.add),
                                  r=[('SP', i2)], w=['C'])

                    def sA3(k, h=h):
                        kb, c0, diag, lk, rq, rk = geo(k)
                        i2 = k % 2
                        At = Ab[i2]
                        mm(psb[accb][:, c0:512], Vt[:, kb, (h // 2) * 128:(h // 2 + 1) * 128], At[:, c0:512], k == 0, k == n_ - 1,
                           r=[('A', i2), ('V', kb)], w=[PS(accb)], sgc=True)

                    sA1(0)
                    sB1(0)
                    for k in range(n_):
                        if k + 1 < n_:
                            sA1(k + 1)
                            sB1(k + 1)
                        sA2(k)
                        sB2(k)
                        sC(k)
                        if k >= 1:
                            sA3(k - 1)
                        flush_one()
                        yield
                    sA3(n_ - 1)
                    reg_fin(accb, lambda r0=r0, ch=ch, qc=qc, accb=accb: evac(oT[r0:r0 + 64, ch, qs(qc)], psb[accb][r0:r0 + 64, :], [('o', ch, qc)], accb))

        n_index = sum(score_steps(2 * pr) + score_steps(2 * pr + 1) + NIT + 2 for pr in range(1, 8)) + 1
        interleave([(gen_sb(), 160), (gen_index(), n_index)])
        flush_pending()
        P.do_fence()
        if dbg and 'stop_sb' in dbg:
            break

        o[0] = A0
        q1T = aalloc([128, S], BF16)
        q2T = aalloc([128, S], BF16)
        k1T = aalloc([128, S], BF16)
        k2T = aalloc([128, S], BF16)
        Vaug = aalloc([128, 16, 4, 66], BF16)
        qmb = [aalloc([128, 512], BF16) for _ in range(2)]
        Pb = [aalloc([128, 512], BF16) for _ in range(3)]
        bcs = aalloc([128, 512], F32)
        rrec = bcs
        setup_e64(aalloc, bcs)
        t1 = aalloc([128, 512], F32)
        t2 = aalloc([128, 512], F32)
        od = aalloc([128, 512], F32)
        sq64 = aalloc([128, 512], F32)
        rs64 = aalloc([128, 512], F32)
        ln64 = aalloc([128, 512], F32)
        lamv = aalloc([128, 4 * 32], F32)
        smallf = aalloc([128, 64], F32)
        load_strips(0)
        P.add('dve', lambda e: e.memset(sq64, 0.0), w=['sq64'])
        dma(lamv, lamv_d[:, l * 128:(l + 1) * 128], w=['lamv'])
        P.add('dve', lambda e: e.tensor_tensor(out=smallf[:, 0:32], in0=lamv[:, 0:32], in1=lamv[:, 32:64], op=ALU.mult), r=['lamv'], w=['sm0'])
        P.add('dve', lambda e: e.reduce_sum(out=smallf[:, 32:33], in_=smallf[:, 0:32], axis=AX.X), r=['sm0'], w=['sm1'])
        P.add('dve', lambda e: e.tensor_tensor(out=smallf[:, 0:32], in0=lamv[:, 64:96], in1=lamv[:, 96:128], op=ALU.mult), r=['lamv', 'sm1'], w=['sm0'])
        P.add('dve', lambda e: e.reduce_sum(out=smallf[:, 33:34], in_=smallf[:, 0:32], axis=AX.X), r=['sm0'], w=['sm1'])
        P.add('act', lambda e: e.activation(out=smallf[:, 34:36], in_=smallf[:, 32:34], func=AF.Exp), r=['sm1'], w=['sm2'])
        P.add('dve', lambda e: e.tensor_tensor(out=smallf[:, 36:37], in0=smallf[:, 35:36], in1=smallf[:, 34:35], op=ALU.subtract), r=['sm2'], w=['sm3'])
        P.add('dve', lambda e, lam_init=lam_init: e.tensor_scalar(out=smallf[:, 37:38], in0=smallf[:, 36:37], scalar1=-lam_init, scalar2=None, op0=ALU.add), r=['sm3'], w=['neglam'])
        neglam = smallf[:, 37:38]
        sc32 = 32 ** -0.5
        projT(l, 768, 128, lambda j, qc: [(q1T[:, qs(qc)], [('q1', qc)], slice(0, 128))], scale=sc32)
        projT(l, 896, 128, lambda j, qc: [(q2T[:, qs(qc)], [('q2', qc)], slice(0, 128))], scale=sc32)
        projT(l, 1024, 128, lambda j, qc: [(k1T[:, qs(qc)], [('k1', qc)], slice(0, 128))])
        projT(l, 1152, 128, lambda j, qc: [(k2T[:, qs(qc)], [('k2', qc)], slice(0, 128))])
        P.add('dve', lambda e: e.memset(Vaug[:, :, :, 64:66], 1.0), w=[('V', t) for t in range(16)])
        projTok(l, 1280, 256, lambda tt, ps: (Vaug[:, tt, :, 0:64], [('V', tt)], ps[:, 0:256].rearrange("p (h d) -> p h d", h=4)))
        lnc = math.log(1.0 - lam_init)
        for h in range(4):
            och, r0 = 2 + h // 2, (h % 2) * 64
            for qc in range(4):
                accs = []
                for which, (qq, kk, qn, kn) in enumerate([(q1T, k1T, 'q1', 'k1'), (q2T, k2T, 'q2', 'k2')]):
                    qm = qmb[which]
                    P.add('dve', lambda e, qm=qm, qq=qq, qc=qc, h=h: e.tensor_scalar(out=qm, in0=qq[:, qs(qc)], scalar1=headmask[:, h:h + 1], scalar2=None, op0=ALU.mult),
                          r=[(qn, qc), 'headmask'], w=[('qm', which)])
                    accb = attn_soft_qc(qc, lambda kb, kk=kk: kk[:, kb * 128:(kb + 1) * 128], lambda qc_, c0, qm=qm: qm[:, c0:512],
                                        [(kn, q) for q in range(4)], [('qm', which)], stripb[:, h, :], c31[:, h:h + 1],
                                        lambda kb, h=h: Vaug[:, kb, h, 0:65], None, Pb, vkeyfn=lambda kb: ('V', kb))
                    for st_ in soft_norm_stages(accb, rrec, bcs):
                        reg_fin(accb, st_)

                    def fin_p3(accb=accb, which=which, bcs=bcs):
                        tt_ = t1 if which == 0 else t2
                        P.add('dve', lambda e, tt_=tt_, accb=accb, bcs=bcs: e.tensor_tensor(out=tt_[0:64, :], in0=psb[accb][0:64, :], in1=bcs[0:64, :], op=ALU.mult),
                              r=['bcs'], w=[PS(accb), ('t', which)])
                    reg_fin(accb, fin_p3)

                def fin_u1():
                    P.add('dve', lambda e: e.scalar_tensor_tensor(out=od[0:64, :], in0=t2[0:64, :], scalar=neglam[0:64, :], in1=t1[0:64, :], op0=ALU.mult, op1=ALU.add),
                          r=[('t', 0), ('t', 1), 'neglam'], w=['od'])
                    P.add('act', lambda e: e.activation(out=sq64[0:64, :], in_=od[0:64, :], func=AF.Square), r=['od'], w=['sq64'])

                def fin_u2():
                    bb = nb()
                    mm(psb[bb][:, :], onesF, sq64[:, :], True, True, r=['sq64', 'onesF'], w=[PS(bb)])
                    P.add('act', lambda e, bb=bb: e.activation(out=ln64[0:64, :], in_=psb[bb][0:64, :], func=AF.Ln, scale=1.0 / 64, bias=EPS), w=[PS(bb), 'ln64'])

                def fin_u3(och=och, r0=r0, qc=qc, l=l, lnc=lnc):
                    P.add('act', lambda e, lnc=lnc: e.activation(out=rs64[0:64, :], in_=ln64[0:64, :], func=AF.Exp, scale=-0.5, bias=lnc), r=['ln64'], w=['rs64'])
                    P.add('dve', lambda e, och=och, r0=r0, qc=qc, l=l: e.scalar_tensor_tensor(out=oT[r0:r0 + 64, och, qs(qc)], in0=od[0:64, :], scalar=subg[0:64, l:l + 1], in1=rs64[0:64, :], op0=ALU.mult, op1=ALU.mult),
                          r=['od', 'rs64', 'subg'], w=[('o', och, qc)])
                pending.extend([fin_u1, fin_u2, fin_u3])
        flush_pending()
        P.do_fence()

        o[0] = A0
        o2[0] = D0
        dq = aalloc([128, 2, S], BF16)
        dk_ = aalloc([128, 2, S], BF16)
        Vaug = aalloc([128, 16, 4, 66], BF16)
        kiT = aalloc([128, S], BF16)
        qiT = aalloc([128, 4, S], BF16)
        score = aalloc([128, S], F32)
        wi = aalloc([128, 16, 8], F32)
        rl = [aalloc([128, 512], F32) for _ in range(2)]
        cmaskq = aalloc([128, 128], F32)
        dma(cmaskq, cmaskq_d, w=['cmaskq'])
        Pb = [aalloc([128, 512], BF16) for _ in range(2)]
        bcs = aalloc([128, 512], F32)
        rrec = bcs
        setup_e64(aalloc, bcs)
        qimb = [aalloc([128, 128], BF16) for _ in range(2)]
        dsqm = [aalloc([128, 512], BF16)]
        maskTs = [dalloc([128, 12, 512], BF16), dalloc([128, 16, 512], BF16)]
        nmb = dalloc([128, S], BF16)
        load_strips(4)
        projT(l, 1536, 256, lambda j, qc: [(dq[:, j, qs(qc)], [('q', j, qc)], slice(0, 128))], scale=0.125)
        projT(l, 1792, 256, lambda j, qc: [(dk_[:, j, qs(qc)], [('k', j, qc)], slice(0, 128))])
        P.add('dve', lambda e, Vaug=Vaug: e.memset(Vaug[:, :, :, 64:66], 1.0), w=[('V', t) for t in range(16)])
        projTok(l, 2048, 256, lambda tt, ps: (Vaug[:, tt, :, 0:64], [('V', tt)], ps[:, 0:256].rearrange("p (h d) -> p h d", h=4)))
        projT(l, 2304, 512, lambda j, qc: [(qiT[:, j, qs(qc)], [('qi', j, qc)], slice(0, 128))])
        projT(l, 2816, 64, lambda j, qc: [(kiT[0:64, qs(qc)], [('ki', qc)], slice(0, 64)), (kiT[64:128, qs(qc)], [('ki', qc)], slice(0, 64))])
        projTok(l, 2880, 8, lambda tt, ps: (wi[:, tt, :], [('wi', tt)], ps[:, 0:8]))
        itc2 = [0, 0]

        def gen_mask(qc):
            mT_ = maskTs[qc % 2]
            mkey = ('maskT', qc % 2)
            for qt in range(4 * qc, 4 * qc + 4):
                L = (qt + 1) * 128
                yield from score_tile_g(qt, score, ('score', 0), qiT, kiT, wi, rl, itc2, qimb)
                allsc = [(('score', 0), kc) for kc in range(4)]
                dsl = score[:, qt * 128:(qt + 1) * 128]
                P.add('dve', lambda e, dsl=dsl: e.tensor_tensor(out=dsl, in0=dsl, in1=cmaskq, op=ALU.add), r=['cmaskq'], w=allsc)
                P.add('dve', lambda e, L=L, qt=qt: e.tensor_scalar(out=nmb[:, 0:L], in0=score[:, 0:L], scalar1=thrAll[:, qt:qt + 1], scalar2=NEG, op0=ALU.is_lt, op1=ALU.mult),
                      r=allsc + ['thrAll'], w=['nm'])
                for kb0 in range(0, qt + 1, 4):
                    nkb = min(4, qt + 1 - kb0)
                    for j in range(nkb):
                        P.add('pe', lambda e, j=j, kb0=kb0: e.transpose(out=psT[:, j * 128:(j + 1) * 128], in_=nmb[:, (kb0 + j) * 128:(kb0 + j + 1) * 128], identity=identb),
                              r=['nm', 'identb'], w=['psT'])
                    qo = (qt % 4) * 128
                    evac(mT_[:, kb0:kb0 + nkb, qo:qo + 128], psT[:, 0:nkb * 128].rearrange("p (a b) -> p a b", a=nkb), [mkey], 'T')
                    yield

        def mask_steps(qc):
            return sum(score_steps(qt) + (qt + 4) // 4 for qt in range(4 * qc, 4 * qc + 4))

        def gen_attn(qc):
            mT_ = maskTs[qc % 2]
            mkey = ('maskT', qc % 2)
            for h in range(4):
                och, r0 = 4 + h // 2, (h % 2) * 64
                ch = h // 2
                holder = []
                qm_ = dsqm[0]
                P.add('dve', lambda e, qm_=qm_, ch=ch, qc=qc, h=h: e.tensor_scalar(out=qm_, in0=dq[:, ch, qs(qc)], scalar1=headmask[:, 4 + h % 2:5 + h % 2], scalar2=None, op0=ALU.mult),
                      r=[('q', ch, qc), 'headmask'], w=[('qm', 0)])
                yield from attn_soft_qc_g(qc, lambda kb, ch=ch: dk_[:, ch, kb * 128:(kb + 1) * 128],
                                          lambda qc_, c0, qm_=qm_: qm_[:, c0:512],
                                          [('k', ch, q) for q in range(4)], [('qm', 0)], stripb[:, h, :], c31[:, 4 + h:5 + h],
                                          lambda kb, h=h: Vaug[:, kb, h, 0:65], None, Pb, vkeyfn=lambda kb: ('V', kb),
                                          extra_fn=lambda b1, qc_, kb, c0, mT_=mT_, mkey=mkey: [(psb[b1][:, c0:512], identb, mT_[:, kb, c0:512], ['identb', mkey])],
                                          holder=holder)
                accb = holder[0]

                for st_ in soft_norm_stages(accb, rrec, bcs):
                    reg_fin(accb, st_)

                def fin_sm(accb=accb, och=och, r0=r0, qc=qc, bcs=bcs):
                    P.add('dve', lambda e, accb=accb, och=och, r0=r0, qc=qc, bcs=bcs: e.tensor_tensor(out=oT[r0:r0 + 64, och, qs(qc)], in0=psb[accb][0:64, :], in1=bcs[0:64, :], op=ALU.mult),
                          r=['bcs'], w=[PS(accb), ('o', och, qc)])
                reg_fin(accb, fin_sm)

        run_gen(gen_mask(0))
        for qc in range(4):
            items = [(gen_attn(qc), 4 * (4 * qc + 4))]
            if qc < 3:
                items.append((gen_mask(qc + 1), mask_steps(qc + 1)))
            interleave(items)
        flush_pending()
        P.do_fence()

        o[0] = A0
        o2[0] = D0
        mq = aalloc([128, 2, S], BF16)
        mk_ = aalloc([128, 2, S], BF16)
        Vaug = aalloc([128, 16, 4, 66], BF16)
        ksf = aalloc([128, 2, 8], F32)
        kshi = aalloc([128, 2, 8], BF16)
        kslo = aalloc([128, 2, 8], BF16)
        gm = aalloc([128, 32], F32)
        t8 = aalloc([128, 4, 8], F32)
        thr4 = aalloc([128, 4], F32)
        negm = aalloc([128, 32], BF16)
        negmT = aalloc([128, S], BF16)
        selb = dalloc([128, 32, 128], BF16)
        P.add('dve', lambda e: e.memset(negmT, 0.0), w=[('negmT', q_) for q_ in range(4)])
        P.add('dve', lambda e: e.memset(selb, 0.0), w=['selb'])
        pastmask = dalloc([128, 8 * 32], F32)
        dma(pastmask, pastmask_d, w=['pastmask'])
        Pb = [aalloc([128, 512], BF16) for _ in range(3)]
        bcs = aalloc([128, 512], F32)
        rrec = bcs
        setup_e64(aalloc, bcs)
        mbqm = [aalloc([128, 512], BF16) for _ in range(2)]
        ksm_hi = aalloc([128, 4, 8], BF16)
        ksm_lo = aalloc([128, 4, 8], BF16)
        load_strips(8)
        load_const_bf16(selb[0:32, 0:16, :], sel_d[:, 0:2048].rearrange("p (a b) -> p a b", a=16), 'selb', [32, 16, 128])
        load_const_bf16(selb[0:32, 16:32, :], sel_d[:, 2048:4096].rearrange("p (a b) -> p a b", a=16), 'selb', [32, 16, 128])
        projT(l, 2888, 256, lambda j, qc: [(mq[:, j, qs(qc)], [('q', j, qc)], slice(0, 128))], scale=0.125)
        projT(l, 3144, 256, lambda j, qc: [(mk_[:, j, qs(qc)], [('k', j, qc)], slice(0, 128))])
        P.add('dve', lambda e: e.memset(Vaug[:, :, :, 64:66], 1.0), w=[('V', t) for t in range(16)])
        projTok(l, 3400, 256, lambda tt, ps: (Vaug[:, tt, :, 0:64], [('V', tt)], ps[:, 0:256].rearrange("p (h d) -> p h d", h=4)))
        for ch in range(2):
            P.add('dve', lambda e, ch=ch: e.reduce_sum(out=ksf[:, ch, :], in_=mk_[:, ch, :].rearrange("p (n k) -> p n k", n=8), axis=AX.X),
                  r=[('k', ch, q) for q in range(4)], w=['ksf'])
        P.add('dve', lambda e: e.tensor_copy(out=kshi, in_=ksf), r=['ksf'], w=['kshi'])
        P.add('dve', lambda e: e.tensor_tensor(out=kslo, in0=ksf, in1=kshi, op=ALU.subtract), r=['ksf', 'kshi'], w=['kslo'])
        for h_ in range(4):
            P.add('dve', lambda e, h_=h_: e.tensor_scalar(out=ksm_hi[:, h_, :], in0=kshi[:, h_ // 2, :], scalar1=headmask[:, 4 + h_ % 2:5 + h_ % 2], scalar2=None, op0=ALU.mult), r=['kshi', 'headmask'], w=['ksm'])
            P.add('dve', lambda e, h_=h_: e.tensor_scalar(out=ksm_lo[:, h_, :], in0=kslo[:, h_ // 2, :], scalar1=headmask[:, 4 + h_ % 2:5 + h_ % 2], scalar2=None, op0=ALU.mult), r=['kslo', 'headmask'], w=['ksm'])
        for qt in range(16):
            own = qt // 2
            bpar = [nb(), nb()]
            for h in range(4):
                ch, r0 = h // 2, (h % 2) * 64
                b = bpar[h % 2]
                lq = mq[:, ch, qt * 128:(qt + 1) * 128]
                mm(psb[b][:, h * 8:(h + 1) * 8], lq, ksm_hi[:, h, :], True, False, r=[('q', ch, qt // 4), 'ksm'], w=[PS(b)], sgc=True)
                mm(psb[b][:, h * 8:(h + 1) * 8], lq, ksm_lo[:, h, :], False, True, r=[('q', ch, qt // 4), 'ksm'], w=[PS(b)], sgc=True)
            for h in range(4):
                b = bpar[h % 2]
                P.add('dve', lambda e, b=b, own=own, h=h: e.tensor_tensor(out=gm[:, h * 8:(h + 1) * 8], in0=psb[b][:, h * 8:(h + 1) * 8], in1=pastmask[:, own * 32 + h * 8:own * 32 + (h + 1) * 8], op=ALU.add),
                      r=['pastmask'], w=[PS(b), 'gm'])
            for h in range(4):
                P.add('dve', lambda e, h=h: e.max(out=t8[:, h, :], in_=gm[:, h * 8:(h + 1) * 8]), r=['gm'], w=['t8'])
            P.add('dve', lambda e: e.tensor_scalar(out=thr4, in0=t8[:, :, 2], scalar1=-1e29, scalar2=None, op0=ALU.max), r=['t8'], w=['thr4'])
            for h in range(4):
                P.add('dve', lambda e, h=h: e.tensor_scalar(out=negm[:, h * 8:(h + 1) * 8], in0=gm[:, h * 8:(h + 1) * 8], scalar1=thr4[:, h:h + 1], scalar2=NEG, op0=ALU.is_lt, op1=ALU.mult),
                      r=['gm', 'thr4'], w=['negm'])
            P.add('pe', lambda e: e.transpose(out=psT[0:32, 0:128], in_=negm, identity=identb), r=['negm', 'identb'], w=['psT'])
            evac(negmT[0:32, qt * 128:(qt + 1) * 128], psT[0:32, 0:128], [('negmT', qt // 4)], 'T')
        for h in range(4):
            och, r0 = 6 + h // 2, (h % 2) * 64
            ch = h // 2

            def mb_extra(b1, qc_, kb, c0, h=h):
                nbk = kb // 2
                if nbk < 2 * qc_:
                    return [(psb[b1][:, c0:512], selb[:, h * 8 + nbk, :], negmT[:, qs(qc_, c0)], ['selb', ('negmT', qc_)])]
                if nbk == 2 * qc_:
                    return [(psb[b1][:, 256:512], selb[:, h * 8 + nbk, :], negmT[:, qs(qc_, 256)], ['selb', ('negmT', qc_)])]
                return []
            for qc in range(4):
                qmi = (h * 4 + qc) % 2
                qm_ = mbqm[qmi]
                P.add('dve', lambda e, qm_=qm_, ch=ch, qc=qc, h=h: e.tensor_scalar(out=qm_, in0=mq[:, ch, qs(qc)], scalar1=headmask[:, 4 + h % 2:5 + h % 2], scalar2=None, op0=ALU.mult),
                      r=[('q', ch, qc), 'headmask'], w=[('qm', qmi)])
                accb = attn_soft_qc(qc, lambda kb, ch=ch: mk_[:, ch, kb * 128:(kb + 1) * 128],
                                    lambda qc_, c0, qm_=qm_: qm_[:, c0:512],
                                    [('k', ch, q) for q in range(4)], [('qm', qmi)], stripb[:, h, :], c31[:, 8 + h:9 + h],
                                    lambda kb, h=h: Vaug[:, kb, h, 0:65], None, Pb, vkeyfn=lambda kb: ('V', kb), extra_fn=mb_extra)
                for st_ in soft_norm_stages(accb, rrec, bcs):
                    reg_fin(accb, st_)

                def fin_sm(accb=accb, och=och, r0=r0, qc=qc, bcs=bcs):
                    P.add('dve', lambda e, accb=accb, och=och, r0=r0, qc=qc, bcs=bcs: e.tensor_tensor(out=oT[r0:r0 + 64, och, qs(qc)], in0=psb[accb][0:64, :], in1=bcs[0:64, :], op=ALU.mult),
                          r=['bcs'], w=[PS(accb), ('o', och, qc)])
                reg_fin(accb, fin_sm)
        flush_pending()
        P.do_fence()
        if dbg and 'stop_br' in dbg:
            break

        o[0] = A0
        macc = aalloc([128, 2, 4, 512], F32)
        sgb = [aalloc([128, 512], F32) for _ in range(2)]
        tb = [aalloc([128, 512], F32) for _ in range(2)]
        kk_ = 0
        for jp in range(4):
            for i in range(4):
                wg, wgk = load_w(w_in[l, :, 3656 + i * 1024 + jp * 256: 3656 + i * 1024 + (jp + 1) * 256], 8, 256)
                wb, wbk = load_w(w_br[l, i, :, jp * 256:(jp + 1) * 256], 2, 256)
                for jj in range(2):
                    j = jp * 2 + jj
                    for qc in range(4):
                        bg = nb()
                        for c in range(8):
                            mm(psb[bg][:, :], wg[:, c, jj * 128:(jj + 1) * 128], hT[:, c, qs(qc)], c == 0, c == 7, r=[wgk, ('h', c, qc)], w=[PS(bg)], nofence=True)
                        by = nb()
                        for c2 in range(2):
                            mm(psb[by][:, :], wb[:, c2, jj * 128:(jj + 1) * 128], oT[:, 2 * i + c2, qs(qc)], c2 == 0, c2 == 1, r=[wbk, ('o', 2 * i + c2, qc)], w=[PS(by)])
                        k2 = kk_ % 2
                        kk_ += 1
                        sg, tt_ = sgb[k2], tb[k2]
                        P.add('act', lambda e, sg=sg, bg=bg: e.activation(out=sg, in_=psb[bg][:, :], func=AF.Sigmoid), w=[PS(bg), ('sg', k2)])
                        mslc = macc[:, jj, qc, :]
                        if i == 0:
                            P.add('dve', lambda e, sg=sg, by=by, mslc=mslc: e.tensor_tensor(out=mslc, in0=sg, in1=psb[by][:, :], op=ALU.mult),
                                  r=[('sg', k2)], w=[PS(by), ('macc', jj, qc)])
                        else:
                            P.add('dve', lambda e, sg=sg, by=by, tt_=tt_: e.tensor_tensor(out=tt_, in0=sg, in1=psb[by][:, :], op=ALU.mult),
                                  r=[('sg', k2)], w=[PS(by), ('mt', k2)])
                            if i < 3:
                                P.add('pool' if kk_ % 3 == 0 else 'dve', lambda e, mslc=mslc, tt_=tt_: e.tensor_tensor(out=mslc, in0=mslc, in1=tt_, op=ALU.add),
                                      r=[('mt', k2)], w=[('macc', jj, qc)])
                            else:
                                dst = mT[:, j, qs(qc)]
                                P.add('pool' if kk_ % 3 == 0 else 'dve', lambda e, mslc=mslc, tt_=tt_, dst=dst: e.tensor_tensor(out=dst, in0=mslc, in1=tt_, op=ALU.add),
                                      r=[('mt', k2), ('macc', jj, qc)], w=[('m', j, qc)])
        P.do_fence()

        for c in range(8):
            dma(xT[:, c, :], xpark[:, c * S:(c + 1) * S], w=[('x', c, q) for q in range(4)])
        wout_bf = view(C0, [128, 8, 1024], BF16)
        ptmp = [view(C0 + 16384 + 2048 * i_, [128, 512], F32) for i_ in range(2)]
        yTb = [view(B0 + 16384 * i_, [128, 8, 512], F32) for i_ in range(2)]
        for t in range(4):
            s_ = cnt['wst'] % 2
            cnt['wst'] += 1
            stg = wst[s_]
            dma(stg, w_out[l, :, t * 256:(t + 1) * 256].rearrange("(c p) n -> p c n", p=128), w=[('wst', s_)])
            dstw = wout_bf[:, :, t * 256:(t + 1) * 256]
            P.add('act', lambda e, dstw=dstw, stg=stg: e.copy(out=dstw, in_=stg), r=[('wst', s_)], w=[('wout', t)])

        def post_norm_residual(gt, y, ykey, qc, l=l):
            bs = nb()
            for j in range(8):
                sq = sqb[j % 2]
                ys = y[:, j, :]
                P.add('act', lambda e, sq=sq, ys=ys: e.activation(out=sq, in_=ys, func=AF.Square), r=[ykey(j)], w=[('sq', j % 2)])
                mm(psb[bs][:, :], onesF, sq, j == 0, j == 7, r=[('sq', j % 2), 'onesF'], w=[PS(bs)])
            P.add('act', lambda e, bs=bs: e.activation(out=lnt, in_=psb[bs][:, :], func=AF.Ln, scale=1.0 / D, bias=EPS), w=[PS(bs), ('sq', 0)])
            P.add('act', lambda e: e.activation(out=rstd, in_=lnt, func=AF.Exp, scale=-0.5), r=[('sq', 0)], w=['rstd'])
            for j in range(8):
                pt = ptmp[j % 2]
                ys = y[:, j, :]
                g = gcol(gt, l, j)
                xs = xT[:, j, qs(qc)]
                P.add('dve', lambda e, pt=pt, ys=ys, g=g: e.scalar_tensor_tensor(out=pt, in0=ys, scalar=g, in1=rstd, op0=ALU.mult, op1=ALU.mult),
                      r=[ykey(j), 'rstd', 'gains'], w=[('pt', j % 2)])
                P.add('pool' if j % 4 == 3 else 'dve', lambda e, pt=pt, xs=xs: e.tensor_tensor(out=xs, in0=xs, in1=pt, op=ALU.add), r=[('pt', j % 2)], w=[('x', j, qc)])

        for qc in range(4):
            y = yTb[qc % 2]
            for j in range(8):
                b = nb()
                for c in range(8):
                    mm(psb[b][:, :], wout_bf[:, c, j * 128:(j + 1) * 128], mT[:, c, qs(qc)], c == 0, c == 7, r=[('wout', j // 2), ('m', c, qc)], w=[PS(b)])
                evac(y[:, j, :], psb[b][:, :], [('y', qc % 2, j)], b)
            post_norm_residual(1, y, lambda j, qc=qc: ('y', qc % 2, j), qc)
        P.do_fence()
        if dbg and 'stop_mix' in dbg:
            break

        rmsnorm_to_h(l, 2)
        uT = view(C0, [128, 22, 1024], BF16)
        yF = view(C0 + 45056, [128, 8, 512], F32)
        ptmp = [view(C0 + 61440 + 2048 * i_, [128, 512], F32) for i_ in range(2)]
        kk_ = 0
        for th in range(2):
            for fp in range(11):
                wg, wgk = load_w(w_f1[l, :, fp * 256:(fp + 1) * 256], 8, 256)
                wu, wuk = load_w(w_f1[l, :, DFF + fp * 256: DFF + (fp + 1) * 256], 8, 256)
                for ff in range(2):
                    f = fp * 2 + ff
                    for q2 in range(2):
                        qc = th * 2 + q2
                        bg = nb()
                        for c in range(8):
                            mm(psb[bg][:, :], wg[:, c, ff * 128:(ff + 1) * 128], hT[:, c, qs(qc)], c == 0, c == 7, r=[wgk, ('h', c, qc)], w=[PS(bg)], nofence=True)
                        bu = nb()
                        for c in range(8):
                            mm(psb[bu][:, :], wu[:, c, ff * 128:(ff + 1) * 128], hT[:, c, qs(qc)], c == 0, c == 7, r=[wuk, ('h', c, qc)], w=[PS(bu)], nofence=True)
                        k2 = kk_ % 2
                        kk_ += 1
                        sg = sqb[k2]
                        P.add('act', lambda e, sg=sg, bg=bg: e.activation(out=sg, in_=psb[bg][:, :], func=AF.Silu), w=[PS(bg), ('sq', k2)])
                        us = uT[:, f, q2 * 512:(q2 + 1) * 512]
                        P.add('dve', lambda e, sg=sg, bu=bu, us=us: e.tensor_tensor(out=us, in0=sg, in1=psb[bu][:, :], op=ALU.mult),
                              r=[('sq', k2)], w=[PS(bu), ('u', f, q2)])
            for q2 in range(2):
                qc = th * 2 + q2
                for jp in range(4):
                    banks = [nb(), nb()]
                    for kg in range(3):
                        nk = 8 if kg < 2 else 6
                        w2, w2k = load_w(w_f2[l, kg * 1024: kg * 1024 + nk * 128, jp * 256:(jp + 1) * 256], nk, 256, eng='dve')
                        for jj in range(2):
                            for fk in range(nk):
                                f = kg * 8 + fk
                                mm(psb[banks[jj]][:, :], w2[:, fk, jj * 128:(jj + 1) * 128], uT[:, f, q2 * 512:(q2 + 1) * 512], f == 0, f == 21,
                                   r=[w2k, ('u', f, q2)], w=[PS(banks[jj])], sgc=True)
                    for jj in range(2):
                        evac(yF[:, jp * 2 + jj, :], psb[banks[jj]][:, :], [('yf', jp * 2 + jj)], banks[jj])
                post_norm_residual(3, yF, lambda j: ('yf', j), qc)
        P.do_fence()
    if dbg and 'thr' in dbg:
        dma(dbg_out['thr'][0:128, 0:16], thrAll, r=['thrAll'])
    if dbg and 'o' in dbg:
        tmp = view(A0, [128, 8, S], F32)
        for c in range(8):
            P.add('dve', lambda e, c=c: e.tensor_copy(out=tmp[:, c, :], in_=oT[:, c, :]), r=[('o', c, q) for q in range(4)], w=[('tmp', c)])
            dma(dbg_out['o'][c * 128:(c + 1) * 128, :], tmp[:, c, :], r=[('tmp', c)])
    elif dbg and 'x' in dbg:
        for c in range(8):
            dma(dbg_out['x'][c * 128:(c + 1) * 128, :], xT[:, c, :], r=[('x', c, q) for q in range(4)])
    else:
        for c in range(8):
            dma(outT_d[c * 128:(c + 1) * 128, :], xT[:, c, :], r=[('x', c, q) for q in range(4)])
    P.emit(st)
    st.close()
    return nc, P


def host_consts(inputs):
    rb = np.asarray(inputs['rel_bias'], np.float32)
    c = {}
    g = np.stack([np.asarray(inputs[k], np.float32) for k in ['g_pre_mix', 'g_post_mix', 'g_pre_ffn', 'g_post_ffn']])
    g = g.reshape(4, DEPTH, 8, 128).transpose(3, 0, 1, 2).reshape(128, 4 * DEPTH * 8)
    c['gains'] = np.ascontiguousarray(g)
    kl = np.arange(128)[:, None]
    r = np.arange(640)[None, :]
    dist = r - kl
    bucket = _t5_bucket_np(dist)
    strips = rb[bucket]
    strips = np.where((dist < 0)[:, :, None], np.float32(NEG), strips)
    c['strips'] = np.ascontiguousarray(strips.transpose(2, 0, 1).astype(np.float32))
    c['c31'] = np.ascontiguousarray(np.broadcast_to(rb[31][None, :], (128, 12)).astype(np.float32))
    lv = np.stack([np.asarray(inputs[k], np.float32) for k in ['lambda_q1', 'lambda_k1', 'lambda_q2', 'lambda_k2']], axis=1)
    c['lamv'] = np.ascontiguousarray(np.broadcast_to(lv.reshape(1, -1), (128, DEPTH * 4 * 32)).astype(np.float32))
    sg = np.asarray(inputs['diff_subln_g'], np.float32)
    c['subg'] = np.ascontiguousarray(np.concatenate([sg.T, sg.T], axis=0))
    c['ident'] = np.eye(128, dtype=np.float32)
    j = np.arange(128)[:, None]
    k = np.arange(128)[None, :]
    c['negT'] = np.where(j >= k, -1.0, 0.0).astype(np.float32)
    c['sbmask'] = np.where(k <= j, NEG, 0.0).astype(np.float32)
    c['cmaskq'] = np.where(k > j, -1e30, 0.0).astype(np.float32)
    pm = np.zeros((128, 8, 4, 8), np.float32)
    for own in range(8):
        for nbk in range(8):
            if not (nbk < own):
                pm[:, own, :, nbk] = -1e30
    c['pastmask'] = pm.reshape(128, 256)
    sel = np.zeros((32, 32, 128), np.float32)
    for i in range(32):
        sel[i, i, :] = 1.0
    c['sel'] = sel.reshape(32, 32 * 128)
    hm = np.zeros((128, 4), np.float32)
    for p in range(128):
        hm[p, p // 32] = 1.0
    hm8 = np.zeros((128, 8), np.float32)
    hm8[:, 0:4] = hm
    hm8[0:64, 4] = 1.0
    hm8[64:128, 5] = 1.0
    c['headmask'] = hm8
    c['pow2tab'] = np.ascontiguousarray(np.broadcast_to((0.5 ** np.arange(1, 25, dtype=np.float64)).astype(np.float32)[None, :], (128, 24)))
    return c


_CACHE = {}


def kernel(**inputs):
    x = np.asarray(inputs['x'], np.float32)
    consts = host_consts(inputs)
    w_br = np.ascontiguousarray(np.stack([np.asarray(inputs[k], np.float32) for k in ['w_br_sb', 'w_br_diff', 'w_br_dsa', 'w_br_moba']], axis=1))
    shared = dict(consts)
    shared['w_in'] = np.ascontiguousarray(np.asarray(inputs['w_in'], np.float32))
    shared['w_br'] = w_br
    shared['w_out'] = np.ascontiguousarray(np.asarray(inputs['w_out'], np.float32))
    shared['w_ffn_in'] = np.ascontiguousarray(np.asarray(inputs['w_ffn_in'], np.float32))
    shared['w_ffn_out'] = np.ascontiguousarray(np.asarray(inputs['w_ffn_out'], np.float32))
    if 'nc' not in _CACHE:
        _CACHE['nc'] = build_program(list(range(DEPTH)))[0]
    nc = _CACHE['nc']
    in_maps = []
    for b in range(8):
        m = dict(shared)
        m['xT'] = np.ascontiguousarray(x[b].T)
        in_maps.append(m)
    res = run_bass_kernel_spmd(nc, in_maps, core_ids=list(range(8)))
    out = np.stack([np.ascontiguousarray(r['outT'].T) for r in res.results], axis=0)
    return out.astype(np.float32)
```

```python
import math
from contextlib import ExitStack
import numpy as np
import concourse.bass as bass
import concourse.mybir as mybir
from concourse.bass_utils import run_bass_kernel_spmd

F32 = mybir.dt.float32
F32R = mybir.dt.float32r
BF16 = mybir.dt.bfloat16
ALU = mybir.AluOpType
AF = mybir.ActivationFunctionType
AX = mybir.AxisListType
NS = 8
ENGS = ['pe', 'act', 'dve', 'pool', 'sp']

D = 1024
S = 2048
DEPTH = 4
NIN = 7752
DFF = 2816
EPS = 1e-6
NEG = -30000.0


class Prog:
    def __init__(self, nc):
        self.nc = nc
        self.ops = []
        self.lastw = {}
        self.readers = {}
        self.fence = {}

    def add(self, eng, fn, r=(), w=(), nofence=False):
        i = len(self.ops)
        deps = set()
        for k in r:
            if k in self.lastw:
                deps.add(self.lastw[k])
        for k in w:
            if k in self.lastw:
                deps.add(self.lastw[k])
            deps.update(self.readers.get(k, ()))
        if eng in self.fence and not nofence:
            deps.update(self.fence.pop(eng))
        red = {}
        out = set()
        for d in deps:
            e = self.ops[d][0]
            if e == 'sp':
                out.add(d)
            else:
                red[e] = max(red.get(e, -1), d)
        out.update(red.values())
        for k in r:
            self.readers.setdefault(k, []).append(i)
        for k in w:
            self.lastw[k] = i
            self.readers[k] = []
        self.ops.append((eng, fn, out))
        return i

    def do_fence(self):
        last = {}
        for i, (e, _, _) in enumerate(self.ops):
            if e == 'sp':
                last.setdefault(e, []).append(i)
                last[e] = last[e][-NS:]
            else:
                last[e] = [i]
        allidx = set(i for v in last.values() for i in v)
        for e in ENGS:
            self.fence[e] = set(allidx)

    def emit(self, stack):
        nc = self.nc
        ops = self.ops
        need = [False] * len(ops)
        for (e, fn, deps) in ops:
            for d in deps:
                if ops[d][0] == 'pe' and e == 'pe':
                    continue
                need[d] = True
        for i, (e, _, _) in enumerate(ops):
            if e == 'sp':
                need[i] = True
        sig = {}
        cnt = {}
        ndma = 0
        extra = {}
        for i, (e, fn, deps) in enumerate(ops):
            if not need[i]:
                continue
            if e == 'sp':
                s = ('sp', ndma % NS)
                ndma += 1
                c = cnt.get(s, 0)
                if c > 0:
                    extra[i] = (s, c)
                cnt[s] = c + 16
                sig[i] = (s, c + 16)
            else:
                s = (e, 0)
                cnt[s] = cnt.get(s, 0) + 1
                sig[i] = (s, cnt[s])
        sems = {}
        for s in cnt:
            sems[s] = stack.enter_context(nc.semaphore("sem_%s_%d" % s))
        idxs = {e: [] for e in ENGS}
        for i, (e, _, _) in enumerate(ops):
            idxs[e].append(i)

        def mk(e):
            def body(eng):
                waited = {}
                for i in idxs[e]:
                    _, fn, deps = ops[i]
                    ws = {}
                    for d in deps:
                        if ops[d][0] == 'pe' and e == 'pe':
                            continue
                        s, c = sig[d]
                        if waited.get(s, 0) < c:
                            ws[s] = max(ws.get(s, 0), c)
                    if i in extra:
                        s, c = extra[i]
                        if waited.get(s, 0) < c:
                            ws[s] = max(ws.get(s, 0), c)
                    for s, c in ws.items():
                        eng.wait_ge(sems[s], c)
                        waited[s] = c
                    ins = fn(eng)
                    if i in sig:
                        ins.then_inc(sems[sig[i][0]], 16 if e == 'sp' else 1)
                if e == 'sp':
                    for s, c in cnt.items():
                        if s[0] == 'sp' and waited.get(s, 0) < c:
                            eng.wait_ge(sems[s], c)
            return body

        with nc.Block() as block:
            block.tensor(mk('pe'))
            block.scalar(mk('act'))
            block.vector(mk('dve'))
            block.gpsimd(mk('pool'))
            block.sync(mk('sp'))


def _t5_bucket_np(d):
    d = np.maximum(d, 0)
    lr = np.log(np.maximum(d, 1).astype(np.float32) / np.float32(16)) / np.float32(math.log(128 / 16))
    large = np.minimum(16 + (lr * 16).astype(np.int32), 31)
    return np.where(d < 16, d, large)


def build_program(layers, dbg=None):
    nc = bass.Bass("TRN2", target_bir_lowering=False)
    dr = {}

    def din(name, shape):
        dr[name] = nc.dram_tensor(name, list(shape), F32, kind="ExternalInput").ap()
        return dr[name]

    xT_d = din("xT", [D, S])
    w_in = din("w_in", [DEPTH, D, NIN])
    w_br = din("w_br", [DEPTH, 4, 256, D])
    w_out = din("w_out", [DEPTH, D, D])
    w_f1 = din("w_ffn_in", [DEPTH, D, 2 * DFF])
    w_f2 = din("w_ffn_out", [DEPTH, DFF, D])
    gains_d = din("gains", [128, 4 * DEPTH * 8])
    strips_d = din("strips", [12, 128, 640])
    c31_d = din("c31", [128, 12])
    lamv_d = din("lamv", [128, DEPTH * 4 * 32])
    subg_d = din("subg", [128, DEPTH])
    ident_d = din("ident", [128, 128])
    negT_d = din("negT", [128, 128])
    sbmask_d = din("sbmask", [128, 128])
    cmaskq_d = din("cmaskq", [128, 128])
    pastmask_d = din("pastmask", [128, 8 * 32])
    sel_d = din("sel", [32, 32 * 128])
    headmask_d = din("headmask", [128, 4])
    pow2_d = din("pow2tab", [128, 24])
    outT_d = nc.dram_tensor("outT", [D, S], F32, kind="ExternalOutput").ap()
    xpark = nc.dram_tensor("xpark", [128, 8 * S], F32, kind="Internal").ap()
    dbg_out = {}
    if dbg:
        for n in dbg:
            dbg_out[n] = nc.dram_tensor("dbg_" + n, [D, S], F32, kind="ExternalOutput").ap()

    st = ExitStack()
    NW = 51384
    big = st.enter_context(nc.sbuf_tensor("SB", [128, NW], F32))
    r_sp = [st.enter_context(nc.sbuf_tensor("r_sp%d" % i, [128, 512], F32R)) for i in range(2)]
    r_c = st.enter_context(nc.sbuf_tensor("r_c", [128, 512], F32R))
    r_negT = st.enter_context(nc.sbuf_tensor("r_negT", [128, 128], F32R))
    r_negOnes = st.enter_context(nc.sbuf_tensor("r_negOnes", [128, 128], F32R))
    thr_t = st.enter_context(nc.sbuf_tensor("thrAll", [128, 16], F32))
    thrAll = thr_t[:, :]
    psb = [st.enter_context(nc.psum_tensor("ps%d" % i, [128, 512], F32)) for i in range(7)]
    psT = st.enter_context(nc.psum_tensor("psT", [128, 1024], BF16))

    def view(off, shape, dt):
        assert off % 4 == 0
        n = int(np.prod(shape[1:]))
        if dt == F32:
            assert off // 4 + n <= NW, (off, shape)
            ap = big[:shape[0], off // 4: off // 4 + n]
        else:
            assert n % 2 == 0 and off // 4 + n // 2 <= NW, (off, shape)
            ap = big[:shape[0], off // 4: off // 4 + n // 2].bitcast(BF16)
        if len(shape) == 3:
            ap = ap.rearrange("p (a b) -> p a b", a=shape[1])
        elif len(shape) == 4:
            ap = ap.rearrange("p (a b c) -> p a b c", a=shape[1], b=shape[2])
        return ap

    A0, B0, C0, D0, E0 = 0, 65536, 98304, 131072, 163840
    xT = view(A0, [128, 8, S], F32)
    hT = view(B0, [128, 8, S], BF16)
    oT = view(C0, [128, 8, S], BF16)
    mT = view(D0, [128, 8, S], BF16)
    eo = [E0]

    def ealloc(shape, dt):
        n = int(np.prod(shape[1:])) * (4 if dt == F32 else 2)
        n = (n + 31) // 32 * 32
        v = view(eo[0], shape, dt)
        eo[0] += n
        return v

    wst = [ealloc([128, 8, 256], F32) for _ in range(2)]
    wbf = [ealloc([128, 8, 256], BF16) for _ in range(3)]
    identb = ealloc([128, 128], BF16)
    onesF = ealloc([128, 128], F32)
    negTr = r_negT[:, :]
    negOnesr = r_negOnes[:, :]
    sbmaskb = ealloc([128, 128], BF16)
    gains = ealloc([128, 4 * DEPTH * 8], F32)
    c31 = ealloc([128, 12], F32)
    subg = ealloc([128, DEPTH], F32)
    headmask = ealloc([128, 4], F32)
    pow2tab = ealloc([128, 24], F32)
    stripb = ealloc([128, 4, 640], BF16)
    rstd = ealloc([128, 512], F32)
    sqb = [ealloc([128, 512], F32) for _ in range(2)]
    lnt = sqb[0]
    SPb = [r_sp[0][:, :], r_sp[1][:, :]]
    Cb = r_c[:, :]
    assert eo[0] <= NW * 4, eo[0]

    P = Prog(nc)
    cnt = {'ps': 0, 'acc': 0, 'wst': 0, 'wbf': 0, 'ev': 0}
    pending = []
    acc_pending = {5: 0, 6: 0}

    def flush_pending():
        while pending:
            pending.pop(0)()

    def flush_one():
        if pending:
            pending.pop(0)()

    def reg_fin(accb, fn):
        acc_pending[accb] += 1

        def w_():
            fn()
            acc_pending[accb] -= 1
        pending.append(w_)

    live = set()

    def nb():
        for _ in range(5):
            cnt['ps'] += 1
            b = cnt['ps'] % 5
            if b not in live:
                live.add(b)
                return b
        raise AssertionError("no free PSUM bank")

    _orig_add = P.add

    def _add2(eng, fn, r=(), w=(), nofence=False):
        if eng != 'pe':
            for k in w:
                if isinstance(k, tuple) and len(k) == 2 and k[0] == 'ps' and k[1] in live:
                    live.discard(k[1])
        return _orig_add(eng, fn, r=r, w=w, nofence=nofence)
    P.add = _add2

    def nacc():
        cnt['acc'] += 1
        b = 5 + cnt['acc'] % 2
        if acc_pending[b] > 0:
            flush_pending()
        return b

    def PS(b):
        return 'psT' if b == 'T' else ('ps', b)

    def qs(qc, c0=0):
        return slice(512 * qc + c0, 512 * (qc + 1))

    def gcol(gt, l, c):
        i = (gt * DEPTH + l) * 8 + c
        return gains[:, i:i + 1]

    def dma(out, in_, r=(), w=(), nofence=False):
        P.add('sp', lambda e: e.dma_start(out=out, in_=in_), r=r, w=w, nofence=nofence)

    def mm(out, lhsT, rhs, start, stop, r=(), w=(), sgc=False, nofence=False):
        P.add('pe', lambda e: e.matmul(out, lhsT=lhsT, rhs=rhs, start=start, stop=stop, skip_group_check=sgc), r=r, w=w, nofence=nofence)

    def evac(dst, src, dk, b, scale=None, eng=None):
        cnt['ev'] += 1
        if eng is None:
            eng = 'act' if cnt['ev'] % 2 == 0 else 'dve'
        if eng == 'act':
            if scale is None:
                P.add('act', lambda e: e.copy(out=dst, in_=src), w=[PS(b)] + list(dk))
            else:
                P.add('act', lambda e: e.mul(out=dst, in_=src, mul=scale), w=[PS(b)] + list(dk))
        else:
            if scale is None:
                P.add('dve', lambda e: e.tensor_copy(out=dst, in_=src), w=[PS(b)] + list(dk))
            else:
                P.add('dve', lambda e: e.tensor_scalar(out=dst, in0=src, scalar1=scale, scalar2=None, op0=ALU.mult), w=[PS(b)] + list(dk))

    def load_w(src, nk, ncols, eng='act'):
        s = cnt['wst'] % 2
        cnt['wst'] += 1
        b = cnt['wbf'] % 3
        cnt['wbf'] += 1
        stg = wst[s][:, 0:nk, 0:ncols]
        dma(stg, src.rearrange("(c p) n -> p c n", p=128), w=[('wst', s)], nofence=True)
        dst = wbf[b][:, 0:nk, 0:ncols]
        if eng == 'act':
            P.add('act', lambda e: e.copy(out=dst, in_=stg), r=[('wst', s)], w=[('wbf', b)], nofence=True)
        else:
            P.add(eng, lambda e: e.tensor_copy(out=dst, in_=stg), r=[('wst', s)], w=[('wbf', b)], nofence=True)
        return wbf[b], ('wbf', b)

    def load_const_f32(dst, src, key):
        dma(dst, src, w=[key])

    def load_const_bf16(dst, src, key, shape):
        s = cnt['wst'] % 2
        cnt['wst'] += 1
        n = int(np.prod(shape[1:]))
        stg = wst[s].rearrange("p a b -> p (a b)")[:shape[0], 0:n]
        if len(shape) == 3:
            stg = stg.rearrange("p (a b) -> p a b", a=shape[1])
        dma(stg, src, w=[('wst', s)])
        P.add('pool', lambda e: e.tensor_copy(out=dst, in_=stg), r=[('wst', s)], w=[key])

    load_const_bf16(identb, ident_d, 'identb', [128, 128])
    load_const_bf16(sbmaskb, sbmask_d, 'sbmaskb', [128, 128])
    load_const_f32(gains, gains_d, 'gains')
    load_const_f32(c31, c31_d, 'c31')
    load_const_f32(subg, subg_d, 'subg')
    load_const_f32(headmask, headmask_d, 'headmask')
    load_const_f32(pow2tab, pow2_d, 'pow2tab')
    P.add('dve', lambda e: e.memset(onesF, 1.0), w=['onesF'])
    _s = cnt['wst'] % 2
    cnt['wst'] += 1
    _stg = wst[_s].rearrange("p a b -> p (a b)")[:, 0:128]
    dma(_stg, negT_d, w=[('wst', _s)])
    P.add('dve', lambda e: e.tensor_copy(out=negTr, in_=_stg), r=[('wst', _s)], w=['negTr'])
    P.add('dve', lambda e: e.tensor_scalar(out=negOnesr, in0=onesF, scalar1=-1.0, scalar2=None, op0=ALU.mult), r=['onesF'], w=['negOnesr'])
    for c in range(8):
        dma(xT[:, c, :], xT_d[c * 128:(c + 1) * 128, :], w=[('x', c, q) for q in range(4)])

    def rmsnorm_to_h(l, gt):
        for qc in range(4):
            b = nb()
            for c in range(8):
                sq = sqb[c % 2]
                xs = xT[:, c, qs(qc)]
                P.add('act', lambda e, sq=sq, xs=xs: e.activation(out=sq, in_=xs, func=AF.Square), r=[('x', c, qc)], w=[('sq', c % 2)])
                mm(psb[b][:, :], onesF, sq, c == 0, c == 7, r=[('sq', c % 2), 'onesF'], w=[PS(b)])
            P.add('act', lambda e, b=b: e.activation(out=lnt, in_=psb[b][:, :], func=AF.Ln, scale=1.0 / D, bias=EPS), w=[PS(b), ('sq', 0)])
            P.add('act', lambda e: e.activation(out=rstd, in_=lnt, func=AF.Exp, scale=-0.5), r=[('sq', 0)], w=['rstd'])
            for c in range(8):
                xs = xT[:, c, qs(qc)]
                hs = hT[:, c, qs(qc)]
                g = gcol(gt, l, c)
                P.add('dve', lambda e, xs=xs, hs=hs, g=g: e.scalar_tensor_tensor(out=hs, in0=xs, scalar=g, in1=rstd, op0=ALU.mult, op1=ALU.mult),
                      r=[('x', c, qc), 'rstd', 'gains'], w=[('h', c, qc)])

    def projT(l, col0, ncols_total, dst_fn, scale=None):
        for t0 in range(0, ncols_total, 256):
            ncl = min(256, ncols_total - t0)
            w, wk = load_w(w_in[l, :, col0 + t0: col0 + t0 + ncl], 8, ncl)
            for jj in range(0, ncl, 128):
                m = min(128, ncl - jj)
                for qc in range(4):
                    b = nb()
                    for c in range(8):
                        mm(psb[b][0:m, :], w[:, c, jj:jj + m], hT[:, c, qs(qc)], c == 0, c == 7, r=[wk, ('h', c, qc)], w=[PS(b)], nofence=True)
                    for (dst, dk, rows) in dst_fn((t0 + jj) // 128, qc):
                        evac(dst, psb[b][rows, :], dk, b, scale)

    def projTok(l, col0, ncols, dst_fn):
        w, wk = load_w(w_in[l, :, col0: col0 + ncols], 8, ncols)
        for tt in range(16):
            b = nb()
            for c in range(8):
                mm(psb[b][:, 0:ncols], hT[:, c, tt * 128:(tt + 1) * 128], w[:, c, 0:ncols], c == 0, c == 7, r=[wk, ('h', c, tt // 4)], w=[PS(b)], nofence=True)
            dst, dk, src = dst_fn(tt, psb[b])
            evac(dst, src, dk, b)

    def attn_soft_qc(*a, **k):
        holder = []
        for _ in attn_soft_qc_g(*a, holder=holder, **k):
            pass
        return holder[0]

    def attn_soft_qc_g(qc, kfn, qfn, kkeys, qkeys, strip, c31col, vfn, vkey, Pbuf, extra_fn=None, extra_keys=(), vkeyfn=None, holder=None):
        accb = nacc()
        holder.append(accb)
        last = 4 * qc + 3
        info = {}

        def stA(kb):
            ks = kb * 128
            c0 = max(0, ks - 512 * qc)
            n = 512 - c0
            near = kb >= 4 * qc - 1
            b1 = nb()
            mms = [(psb[b1][:, c0:512], kfn(kb), qfn(qc, c0), list(kkeys) + list(qkeys))]
            if near:
                r0 = 512 * qc + c0 - ks
                mms.append((psb[b1][:, c0:512], identb, strip[:, r0:r0 + n], ['identb', 'strip']))
            if extra_fn is not None:
                mms += extra_fn(b1, qc, kb, c0)
            for i, (o_, lt, rh, rk) in enumerate(mms):
                mm(o_, lt, rh, i == 0, i == len(mms) - 1, r=rk + list(extra_keys), w=[PS(b1)], sgc=True)
            info[kb] = (b1, c0, near)

        def stB(kb):
            b1, c0, near = info[kb]
            pi = kb % len(Pbuf)
            Pt = Pbuf[pi]
            if near:
                P.add('act', lambda e, Pt=Pt, b1=b1, c0=c0: e.activation(out=Pt[:, c0:512], in_=psb[b1][:, c0:512], func=AF.Exp),
                      w=[PS(b1), ('P', pi)])
            else:
                P.add('act', lambda e, Pt=Pt, b1=b1, c0=c0: e.activation(out=Pt[:, c0:512], in_=psb[b1][:, c0:512], func=AF.Exp, bias=c31col),
                      r=['c31'], w=[PS(b1), ('P', pi)])

        def stC(kb):
            b1, c0, near = info[kb]
            pi = kb % len(Pbuf)
            Pt = Pbuf[pi]
            mm(psb[accb][0:65, c0:512], vfn(kb), Pt[:, c0:512], kb == 0, kb == last, r=[('P', pi), vkeyfn(kb)], w=[PS(accb)], sgc=True)

        stA(0)
        if last >= 1:
            stA(1)
        for kb in range(0, last + 1):
            if kb + 2 <= last:
                stA(kb + 2)
            stB(kb)
            if kb >= 1:
                stC(kb - 1)
            flush_one()
            yield
        stC(last)

    def soft_norm_stages(accb, rrec, bcs):
        def s1():
            P.add('dve', lambda e: e.reciprocal(out=rrec[64:65, :], in_=psb[accb][64:65, :]), w=[PS(accb), 'rrec'])

        def s2():
            bb = nb()
            mm(psb[bb][0:64, :], onesF[64:65, 0:64], rrec[64:65, :], True, True, r=['rrec', 'onesF'], w=[PS(bb)])
            P.add('act', lambda e: e.copy(out=bcs[0:64, :], in_=psb[bb][0:64, :]), w=[PS(bb), 'bcs'])
        return [s1, (lambda: None), (lambda: None), s2]

    def interleave(items):
        st_ = [[g, max(1, est), 0] for g, est in items]
        while st_:
            st_.sort(key=lambda t: t[2] / t[1])
            t = st_[0]
            try:
                next(t[0])
                t[2] += 1
            except StopIteration:
                st_.remove(t)

    def run_gen(g):
        for _ in g:
            pass

    def score_tile_g(qt, scbuf, skey, qiT, kiT, wi, rl, itc, every=2):
        L = (qt + 1) * 128
        step = 0
        for ih in range(8):
            ich, ir0 = ih // 2, (ih % 2) * 64
            for kc in range((L + 511) // 512):
                n = min(512, L - kc * 512)
                b = nb()
                mm(psb[b][:, 0:n], qiT[ir0:ir0 + 64, ich, qt * 128:(qt + 1) * 128], kiT[ir0:ir0 + 64, kc * 512:kc * 512 + n], True, True,
                   r=[('qi', ich, qt // 4), ('ki', kc)], w=[PS(b)])
                i2 = itc[0] % 2
                itc[0] += 1
                rt = rl[i2]
                P.add('act', lambda e, rt=rt, b=b, n=n: e.activation(out=rt[:, 0:n], in_=psb[b][:, 0:n], func=AF.Relu), w=[PS(b), ('rl', i2)])
                sc = scbuf[:, kc * 512:kc * 512 + n]
                wcol = wi[:, qt, ih:ih + 1]
                if ih == 0:
                    P.add('dve', lambda e, sc=sc, rt=rt, n=n, wcol=wcol: e.tensor_scalar(out=sc, in0=rt[:, 0:n], scalar1=wcol, scalar2=None, op0=ALU.mult),
                          r=[('rl', i2), ('wi', qt)], w=[(skey, kc)])
                else:
                    P.add('dve', lambda e, sc=sc, rt=rt, n=n, wcol=wcol: e.scalar_tensor_tensor(out=sc, in0=rt[:, 0:n], scalar=wcol, in1=sc, op0=ALU.mult, op1=ALU.add),
                          r=[('rl', i2), ('wi', qt)], w=[(skey, kc)])
                step += 1
                if step % every == 0:
                    yield
        yield

    def score_steps(qt, every=2):
        L = (qt + 1) * 128
        return (8 * ((L + 511) // 512)) // every + 1

    def load_strips(h0):
        for j in range(4):
            load_const_bf16(stripb[:, j, :], strips_d[h0 + j], 'strip', [128, 640])

    for l in layers:
        lam_init = 0.8 - 0.6 * math.exp(-0.3 * l)
        rmsnorm_to_h(l, 0)
        if dbg and 'h' in dbg:
            pass
        for c in range(8):
            dma(xpark[:, c * S:(c + 1) * S], xT[:, c, :], r=[('x', c, q) for q in range(4)])
        P.do_fence()

        o = [A0]

        def aalloc(shape, dt, o=o):
            n = int(np.prod(shape[1:])) * (4 if dt == F32 else 2)
            n = (n + 31) // 32 * 32
            v = view(o[0], shape, dt)
            o[0] += n
            assert o[0] <= B0
            return v

        qT = aalloc([128, 2, S], BF16)
        kT = aalloc([128, 2, S], BF16)
        Vt = aalloc([128, 16, 256], BF16)
        Eb = [aalloc([128, 512], F32) for _ in range(2)]
        Ab = [aalloc([128, 512], BF16) for _ in range(2)]
        projT(l, 0, 256, lambda j, qc: [(qT[:, j, qs(qc)], [('q', j, qc)], slice(0, 128))], scale=0.125)
        projT(l, 256, 256, lambda j, qc: [(kT[:, j, qs(qc)], [('k', j, qc)], slice(0, 128))])
        projTok(l, 512, 256, lambda tt, ps: (Vt[:, tt, :], [('V', tt)], ps[:, 0:256]))
        x_qiT = aalloc([128, 4, S], BF16)
        x_kiT = aalloc([128, S], BF16)
        x_wi = aalloc([128, 16, 8], F32)
        x_rl = [aalloc([128, 512], F32) for _ in range(2)]
        o2 = [D0]

        def dalloc(shape, dt, o2=o2):
            n = int(np.prod(shape[1:])) * (4 if dt == F32 else 2)
            n = (n + 31) // 32 * 32
            v = view(o2[0], shape, dt)
            o2[0] += n
            assert o2[0] <= E0
            return v
        x_scb = [dalloc([128, S], F32) for _ in range(2)]
        x_junk = [dalloc([128, S], BF16) for _ in range(2)]
        NIT = 16
        mids = [dalloc([128, NIT + 2], F32) for _ in range(2)]
        hcols = [dalloc([128, NIT + 2], F32) for _ in range(2)]
        cntb = [dalloc([128, NIT + 2], F32) for _ in range(2)]
        rmm = [dalloc([128, 8], F32) for _ in range(2)]
        x_cmaskq = dalloc([128, 128], F32)
        dma(x_cmaskq, cmaskq_d, w=['cmaskq'])
        projT(l, 2304, 512, lambda j, qc: [(x_qiT[:, j, qs(qc)], [('qi', j, qc)], slice(0, 128))])
        projT(l, 2816, 64, lambda j, qc: [(x_kiT[0:64, qs(qc)], [('ki', qc)], slice(0, 64)), (x_kiT[64:128, qs(qc)], [('ki', qc)], slice(0, 64))])
        projTok(l, 2880, 8, lambda tt, ps: (x_wi[:, tt, :], [('wi', tt)], ps[:, 0:8]))

        def gen_index():
            itc = [0]
            P.add('dve', lambda e: e.memset(thrAll[:, 0:2], -1e29), w=['thrAll'])
            yield
            for pr in range(1, 8):
                tiles = [2 * pr, 2 * pr + 1]
                for s_, qt in enumerate(tiles):
                    L = (qt + 1) * 128
                    scbuf = x_scb[s_]
                    yield from score_tile_g(qt, scbuf, ('score', s_), x_qiT, x_kiT, x_wi, x_rl, itc)
                    allsc = [(('score', s_), kc) for kc in range(4)]
                    P.add('dve', lambda e, s_=s_, L=L, scbuf=scbuf: e.tensor_reduce(out=rmm[s_][:, 0:1], in_=scbuf[:, 0:L], axis=AX.X, op=ALU.min), r=allsc, w=[('rmm', s_)])
                    P.add('dve', lambda e, s_=s_, L=L, scbuf=scbuf: e.tensor_reduce(out=rmm[s_][:, 1:2], in_=scbuf[:, 0:L], axis=AX.X, op=ALU.max), r=allsc, w=[('rmm', s_)])
                    dsl = scbuf[:, qt * 128:(qt + 1) * 128]
                    P.add('dve', lambda e, dsl=dsl: e.tensor_tensor(out=dsl, in0=dsl, in1=x_cmaskq, op=ALU.add), r=['cmaskq'], w=allsc)
                for s_ in range(2):
                    P.add('dve', lambda e, s_=s_: e.tensor_tensor(out=rmm[s_][:, 2:3], in0=rmm[s_][:, 1:2], in1=rmm[s_][:, 0:1], op=ALU.subtract), w=[('rmm', s_)])
                    P.add('dve', lambda e, s_=s_: e.tensor_scalar(out=hcols[s_][:, 0:NIT], in0=pow2tab[:, 0:NIT], scalar1=rmm[s_][:, 2:3], scalar2=None, op0=ALU.mult),
                          r=[('rmm', s_), 'pow2tab'], w=[('hc', s_)])
                    P.add('dve', lambda e, s_=s_: e.tensor_tensor(out=mids[s_][:, 0:1], in0=rmm[s_][:, 0:1], in1=hcols[s_][:, 0:1], op=ALU.add),
                          r=[('rmm', s_), ('hc', s_)], w=[('mid', s_)])
                yield
                for i_ in range(NIT):
                    for s_, qt in enumerate(tiles):
                        L = (qt + 1) * 128
                        scr = [(('score', s_), kc) for kc in range(4)]
                        if s_ == 0:
                            P.add('dve', lambda e, s_=s_, L=L, i_=i_: e.tensor_scalar(out=x_junk[s_][:, 0:L], in0=x_scb[s_][:, 0:L], scalar1=mids[s_][:, i_:i_ + 1], scalar2=0.0,
                                                                                   op0=ALU.is_ge, op1=ALU.add, accum_out=cntb[s_][:, i_:i_ + 1]),
                                  r=scr + [('mid', s_)], w=[('junk', s_), ('cnt', s_)])
                        else:
                            P.add('pool', lambda e, s_=s_, i_=i_: e.tensor_scalar(out=rmm[s_][:, 4:5], in0=mids[s_][:, i_:i_ + 1], scalar1=-1.0, scalar2=None, op0=ALU.mult),
                                  r=[('mid', s_)], w=[('nmid', s_)])
                            P.add('act', lambda e, s_=s_, L=L, i_=i_: e.activation(out=x_junk[s_][:, 0:L], in_=x_scb[s_][:, 0:L], func=AF.Sign, bias=rmm[s_][:, 4:5], scale=1.0,
                                                                                accum_out=cntb[s_][:, i_:i_ + 1]),
                                  r=scr + [('nmid', s_)], w=[('junk', s_), ('cnt', s_)])
                    for s_, qt in enumerate(tiles):
                        L = (qt + 1) * 128
                        cth = 255.5 if s_ == 0 else (510.5 - L)
                        sub_ = 0.5 if i_ < NIT - 1 else 1.0
                        P.add('pool', lambda e, s_=s_, i_=i_, cth=cth, sub_=sub_: e.tensor_scalar(out=rmm[s_][:, 3:4], in0=cntb[s_][:, i_:i_ + 1], scalar1=cth, scalar2=sub_,
                                                                                       op0=ALU.is_ge, op1=ALU.subtract), r=[('cnt', s_)], w=[('tmpb', s_)])
                    for s_ in range(2):
                        P.add('pool', lambda e, s_=s_, i_=i_: e.tensor_scalar(out=mids[s_][:, i_ + 1:i_ + 2], in0=rmm[s_][:, 3:4], scalar1=hcols[s_][:, i_:i_ + 1], scalar2=mids[s_][:, i_:i_ + 1],
                                                                           op0=ALU.mult, op1=ALU.add),
                              r=[('tmpb', s_), ('hc', s_)], w=[('mid', s_)])
                    yield
                for s_, qt in enumerate(tiles):
                    P.add('dve', lambda e, s_=s_, qt=qt: e.tensor_copy(out=thrAll[:, qt:qt + 1], in_=mids[s_][:, NIT:NIT + 1]), r=[('mid', s_)], w=['thrAll'])
                yield

        def gen_sb():
            for h in range(4):
                ch, r0 = h // 2, (h % 2) * 64
                for qc in range(4):
                    for j_ in range(4):
                        P.add('dve', lambda e, j_=j_: e.tensor_scalar(out=Cb[:, j_ * 128:(j_ + 1) * 128], in0=onesF, scalar1=0.0, scalar2=None, op0=ALU.mult), r=['onesF'], w=['C'])
                    last = 4 * qc + 3
                    accb = nacc()
                    order = list(range(last, -1, -1))
                    n_ = len(order)
                    inf = {}

                    def geo(k, qc=qc, ch=ch, r0=r0):
                        kb = order[k]
                        ks = kb * 128
                        c0 = max(0, ks - 512 * qc)
                        diag = kb >= 4 * qc
                        lk = kT[r0:r0 + 64, ch, ks:ks + 128]
                        rq = qT[r0:r0 + 64, ch, qs(qc, c0)]
                        rk = [('k', ch, kb // 4), ('q', ch, qc)]
                        return kb, c0, diag, lk, rq, rk

                    def sA1(k):
                        kb, c0, diag, lk, rq, rk = geo(k)
                        b1 = nb()
                        inf[k] = b1
                        mm(psb[b1][:, c0:512], lk, rq, True, not diag, r=rk, w=[PS(b1)], sgc=True)
                        if diag:
                            mm(psb[b1][:, c0:c0 + 128], identb, sbmaskb, False, True, r=['identb', 'sbmaskb'], w=[PS(b1)], sgc=True)

                    def sB1(k):
                        kb, c0, diag, lk, rq, rk = geo(k)
                        b1 = inf[k]
                        i2 = k % 2
                        E, SPt = Eb[i2], SPb[i2]
                        P.add('act', lambda e, E=E, b1=b1, c0=c0: e.activation(out=E[:, c0:512], in_=psb[b1][:, c0:512], func=AF.Exp),
                              w=[PS(b1), ('E', i2)])
                        P.add('act', lambda e, E=E, SPt=SPt, c0=c0: e.activation(out=SPt[:, c0:512], in_=E[:, c0:512], func=AF.Ln, bias=1.0, scale=1.0),
                              r=[('E', i2)], w=[('SP', i2)])

                    def sA2(k):
                        kb, c0, diag, lk, rq, rk = geo(k)
                        i2 = k % 2
                        SPt = SPb[i2]
                        b2 = nb()
                        inf[('b2', k)] = b2
                        mm(psb[b2][:, c0:512], lk, rq, True, False, r=rk, w=[PS(b2)], sgc=True)
                        if diag:
                            mm(psb[b2][:, c0:c0 + 128], identb, sbmaskb, False, False, r=['identb', 'sbmaskb'], w=[PS(b2)], sgc=True)
                        mm(psb[b2][:, c0:512], negTr, SPt[:, c0:512], False, k == 0, r=[('SP', i2), 'negTr'], w=[PS(b2)], sgc=True)
                        if k != 0:
                            mm(psb[b2][:, c0:512], negOnesr, Cb[:, c0:512], False, True, r=['C', 'negOnesr'], w=[PS(b2)], sgc=True)

                    def sB2(k):
                        kb, c0, diag, lk, rq, rk = geo(k)
                        i2 = k % 2
                        At = Ab[i2]
                        b2 = inf[('b2', k)]
                        P.add('act', lambda e, At=At, b2=b2, c0=c0: e.activation(out=At[:, c0:512], in_=psb[b2][:, c0:512], func=AF.Exp),
                              w=[PS(b2), ('A', i2)])

                    def sC(k):
                        kb, c0, diag, lk, rq, rk = geo(k)
                        i2 = k % 2
                        SPt = SPb[i2]
                        if k != n_ - 1:
                            P.add('dve', lambda e, SPt=SPt, c0=c0: e.tensor_tensor(out=Cb[:, c0:512], in0=Cb[:, c0:512].bitcast(F32), in1=SPt[:, c0:512].bitcast(F32), op=ALU.add),
                                  r=[('SP', i2)], w=['C'])

                    def sA3(k, h=h):
                        kb, c0, diag, lk, rq, rk = geo(k)
                        i2 = k % 2
                        At = Ab[i2]
                        mm(psb[accb][0:64, c0:512], Vt[:, kb, h * 64:(h + 1) * 64], At[:, c0:512], k == 0, k == n_ - 1,
                           r=[('A', i2), ('V', kb)], w=[PS(accb)], sgc=True)

                    sA1(0)
                    sB1(0)
                    for k in range(n_):
                        if k + 1 < n_:
                            sA1(k + 1)
                            sB1(k + 1)
                        sA2(k)
                        sB2(k)
                        sC(k)
                        if k >= 1:
                            sA3(k - 1)
                        flush_one()
                        yield
                    sA3(n_ - 1)
                    reg_fin(accb, lambda r0=r0, ch=ch, qc=qc, accb=accb: evac(oT[r0:r0 + 64, ch, qs(qc)], psb[accb][0:64, :], [('o', ch, qc)], accb))

        n_index = sum(score_steps(2 * pr) + score_steps(2 * pr + 1) + NIT + 2 for pr in range(1, 8)) + 1
        interleave([(gen_sb(), 160), (gen_index(), n_index)])
        flush_pending()
        P.do_fence()
        if dbg and 'stop_sb' in dbg:
            break

        o[0] = A0
        q1T = aalloc([128, S], BF16)
        q2T = aalloc([128, S], BF16)
        k1T = aalloc([128, S], BF16)
        k2T = aalloc([128, S], BF16)
        Vaug = aalloc([128, 16, 4, 66], BF16)
        qmb = [aalloc([128, 512], BF16) for _ in range(2)]
        Pb = [aalloc([128, 512], BF16) for _ in range(3)]
        rrec = aalloc([128, 512], F32)
        bcs = aalloc([128, 512], F32)
        t1 = aalloc([128, 512], F32)
        t2 = aalloc([128, 512], F32)
        od = aalloc([128, 512], F32)
        sq64 = aalloc([128, 512], F32)
        rs64 = aalloc([128, 512], F32)
        ln64 = aalloc([128, 512], F32)
        lamv = aalloc([128, 4 * 32], F32)
        smallf = aalloc([128, 64], F32)
        load_strips(0)
        dma(lamv, lamv_d[:, l * 128:(l + 1) * 128], w=['lamv'])
        P.add('dve', lambda e: e.tensor_tensor(out=smallf[:, 0:32], in0=lamv[:, 0:32], in1=lamv[:, 32:64], op=ALU.mult), r=['lamv'], w=['sm0'])
        P.add('dve', lambda e: e.reduce_sum(out=smallf[:, 32:33], in_=smallf[:, 0:32], axis=AX.X), r=['sm0'], w=['sm1'])
        P.add('dve', lambda e: e.tensor_tensor(out=smallf[:, 0:32], in0=lamv[:, 64:96], in1=lamv[:, 96:128], op=ALU.mult), r=['lamv', 'sm1'], w=['sm0'])
        P.add('dve', lambda e: e.reduce_sum(out=smallf[:, 33:34], in_=smallf[:, 0:32], axis=AX.X), r=['sm0'], w=['sm1'])
        P.add('act', lambda e: e.activation(out=smallf[:, 34:36], in_=smallf[:, 32:34], func=AF.Exp), r=['sm1'], w=['sm2'])
        P.add('dve', lambda e: e.tensor_tensor(out=smallf[:, 36:37], in0=smallf[:, 35:36], in1=smallf[:, 34:35], op=ALU.subtract), r=['sm2'], w=['sm3'])
        P.add('dve', lambda e, lam_init=lam_init: e.tensor_scalar(out=smallf[:, 37:38], in0=smallf[:, 36:37], scalar1=-lam_init, scalar2=None, op0=ALU.add), r=['sm3'], w=['neglam'])
        neglam = smallf[:, 37:38]
        sc32 = 32 ** -0.5
        projT(l, 768, 128, lambda j, qc: [(q1T[:, qs(qc)], [('q1', qc)], slice(0, 128))], scale=sc32)
        projT(l, 896, 128, lambda j, qc: [(q2T[:, qs(qc)], [('q2', qc)], slice(0, 128))], scale=sc32)
        projT(l, 1024, 128, lambda j, qc: [(k1T[:, qs(qc)], [('k1', qc)], slice(0, 128))])
        projT(l, 1152, 128, lambda j, qc: [(k2T[:, qs(qc)], [('k2', qc)], slice(0, 128))])
        P.add('dve', lambda e: e.memset(Vaug[:, :, :, 64:66], 1.0), w=[('V', t) for t in range(16)])
        projTok(l, 1280, 256, lambda tt, ps: (Vaug[:, tt, :, 0:64], [('V', tt)], ps[:, 0:256].rearrange("p (h d) -> p h d", h=4)))
        lnc = math.log(1.0 - lam_init)
        for h in range(4):
            och, r0 = 2 + h // 2, (h % 2) * 64
            for qc in range(4):
                accs = []
                for which, (qq, kk, qn, kn) in enumerate([(q1T, k1T, 'q1', 'k1'), (q2T, k2T, 'q2', 'k2')]):
                    qm = qmb[which]
                    P.add('dve', lambda e, qm=qm, qq=qq, qc=qc, h=h: e.tensor_scalar(out=qm, in0=qq[:, qs(qc)], scalar1=headmask[:, h:h + 1], scalar2=None, op0=ALU.mult),
                          r=[(qn, qc), 'headmask'], w=[('qm', which)])
                    accb = attn_soft_qc(qc, lambda kb, kk=kk: kk[:, kb * 128:(kb + 1) * 128], lambda qc_, c0, qm=qm: qm[:, c0:512],
                                        [(kn, q) for q in range(4)], [('qm', which)], stripb[:, h, :], c31[:, h:h + 1],
                                        lambda kb, h=h: Vaug[:, kb, h, 0:65], None, Pb, vkeyfn=lambda kb: ('V', kb))
                    for st_ in soft_norm_stages(accb, rrec, bcs):
                        reg_fin(accb, st_)

                    def fin_p3(accb=accb, which=which, bcs=bcs):
                        tt_ = t1 if which == 0 else t2
                        P.add('dve', lambda e, tt_=tt_, accb=accb, bcs=bcs: e.tensor_tensor(out=tt_[0:64, :], in0=psb[accb][0:64, :], in1=bcs[0:64, :], op=ALU.mult),
                              r=['bcs'], w=[PS(accb), ('t', which)])
                    reg_fin(accb, fin_p3)

                def fin_u1():
                    P.add('dve', lambda e: e.scalar_tensor_tensor(out=od[0:64, :], in0=t2[0:64, :], scalar=neglam[0:64, :], in1=t1[0:64, :], op0=ALU.mult, op1=ALU.add),
                          r=[('t', 0), ('t', 1), 'neglam'], w=['od'])
                    P.add('act', lambda e: e.activation(out=sq64[0:64, :], in_=od[0:64, :], func=AF.Square), r=['od'], w=['sq64'])

                def fin_u2():
                    bb = nb()
                    mm(psb[bb][0:64, :], onesF[0:64, 0:64], sq64[0:64, :], True, True, r=['sq64', 'onesF'], w=[PS(bb)])
                    P.add('act', lambda e, bb=bb: e.activation(out=ln64[0:64, :], in_=psb[bb][0:64, :], func=AF.Ln, scale=1.0 / 64, bias=EPS), w=[PS(bb), 'ln64'])

                def fin_u3(och=och, r0=r0, qc=qc, l=l, lnc=lnc):
                    P.add('act', lambda e, lnc=lnc: e.activation(out=rs64[0:64, :], in_=ln64[0:64, :], func=AF.Exp, scale=-0.5, bias=lnc), r=['ln64'], w=['rs64'])
                    P.add('dve', lambda e, och=och, r0=r0, qc=qc, l=l: e.scalar_tensor_tensor(out=oT[r0:r0 + 64, och, qs(qc)], in0=od[0:64, :], scalar=subg[0:64, l:l + 1], in1=rs64[0:64, :], op0=ALU.mult, op1=ALU.mult),
                          r=['od', 'rs64', 'subg'], w=[('o', och, qc)])
                pending.extend([fin_u1, fin_u2, fin_u3])
        flush_pending()
        P.do_fence()

        o[0] = A0
        o2[0] = D0
        dq = aalloc([128, 2, S], BF16)
        dk_ = aalloc([128, 2, S], BF16)
        Vaug = aalloc([128, 16, 4, 66], BF16)
        kiT = aalloc([128, S], BF16)
        qiT = aalloc([128, 4, S], BF16)
        score = aalloc([128, S], F32)
        wi = aalloc([128, 16, 8], F32)
        rl = [aalloc([128, 512], F32) for _ in range(2)]
        cmaskq = aalloc([128, 128], F32)
        dma(cmaskq, cmaskq_d, w=['cmaskq'])
        Pb = [aalloc([128, 512], BF16) for _ in range(3)]
        bcs = aalloc([128, 512], F32)
        rrec = bcs
        maskTs = [dalloc([128, 12, 512], BF16), dalloc([128, 16, 512], BF16)]
        nmb = dalloc([128, S], BF16)
        load_strips(4)
        projT(l, 1536, 256, lambda j, qc: [(dq[:, j, qs(qc)], [('q', j, qc)], slice(0, 128))], scale=0.125)
        projT(l, 1792, 256, lambda j, qc: [(dk_[:, j, qs(qc)], [('k', j, qc)], slice(0, 128))])
        P.add('dve', lambda e, Vaug=Vaug: e.memset(Vaug[:, :, :, 64:66], 1.0), w=[('V', t) for t in range(16)])
        projTok(l, 2048, 256, lambda tt, ps: (Vaug[:, tt, :, 0:64], [('V', tt)], ps[:, 0:256].rearrange("p (h d) -> p h d", h=4)))
        projT(l, 2304, 512, lambda j, qc: [(qiT[:, j, qs(qc)], [('qi', j, qc)], slice(0, 128))])
        projT(l, 2816, 64, lambda j, qc: [(kiT[0:64, qs(qc)], [('ki', qc)], slice(0, 64)), (kiT[64:128, qs(qc)], [('ki', qc)], slice(0, 64))])
        projTok(l, 2880, 8, lambda tt, ps: (wi[:, tt, :], [('wi', tt)], ps[:, 0:8]))
        itc2 = [0]

        def gen_mask(qc):
            mT_ = maskTs[qc % 2]
            mkey = ('maskT', qc % 2)
            for qt in range(4 * qc, 4 * qc + 4):
                L = (qt + 1) * 128
                yield from score_tile_g(qt, score, ('score', 0), qiT, kiT, wi, rl, itc2)
                allsc = [(('score', 0), kc) for kc in range(4)]
                dsl = score[:, qt * 128:(qt + 1) * 128]
                P.add('dve', lambda e, dsl=dsl: e.tensor_tensor(out=dsl, in0=dsl, in1=cmaskq, op=ALU.add), r=['cmaskq'], w=allsc)
                P.add('dve', lambda e, L=L, qt=qt: e.tensor_scalar(out=nmb[:, 0:L], in0=score[:, 0:L], scalar1=thrAll[:, qt:qt + 1], scalar2=NEG, op0=ALU.is_lt, op1=ALU.mult),
                      r=allsc + ['thrAll'], w=['nm'])
                for kb0 in range(0, qt + 1, 4):
                    nkb = min(4, qt + 1 - kb0)
                    for j in range(nkb):
                        P.add('pe', lambda e, j=j, kb0=kb0: e.transpose(out=psT[:, j * 128:(j + 1) * 128], in_=nmb[:, (kb0 + j) * 128:(kb0 + j + 1) * 128], identity=identb),
                              r=['nm', 'identb'], w=['psT'])
                    qo = (qt % 4) * 128
                    evac(mT_[:, kb0:kb0 + nkb, qo:qo + 128], psT[:, 0:nkb * 128].rearrange("p (a b) -> p a b", a=nkb), [mkey], 'T')
                    yield

        def mask_steps(qc):
            return sum(score_steps(qt) + (qt + 4) // 4 for qt in range(4 * qc, 4 * qc + 4))

        def gen_attn(qc):
            mT_ = maskTs[qc % 2]
            mkey = ('maskT', qc % 2)
            for h in range(4):
                och, r0 = 4 + h // 2, (h % 2) * 64
                ch = h // 2
                holder = []
                yield from attn_soft_qc_g(qc, lambda kb, ch=ch, r0=r0: dk_[r0:r0 + 64, ch, kb * 128:(kb + 1) * 128],
                                          lambda qc_, c0, ch=ch, r0=r0: dq[r0:r0 + 64, ch, qs(qc_, c0)],
                                          [('k', ch, q) for q in range(4)], [('q', ch, qc)], stripb[:, h, :], c31[:, 4 + h:5 + h],
                                          lambda kb, h=h: Vaug[:, kb, h, 0:65], None, Pb, vkeyfn=lambda kb: ('V', kb),
                                          extra_fn=lambda b1, qc_, kb, c0, mT_=mT_, mkey=mkey: [(psb[b1][:, c0:512], identb, mT_[:, kb, c0:512], ['identb', mkey])],
                                          holder=holder)
                accb = holder[0]

                for st_ in soft_norm_stages(accb, rrec, bcs):
                    reg_fin(accb, st_)

                def fin_sm(accb=accb, och=och, r0=r0, qc=qc, bcs=bcs):
                    P.add('dve', lambda e, accb=accb, och=och, r0=r0, qc=qc, bcs=bcs: e.tensor_tensor(out=oT[r0:r0 + 64, och, qs(qc)], in0=psb[accb][0:64, :], in1=bcs[0:64, :], op=ALU.mult),
                          r=['bcs'], w=[PS(accb), ('o', och, qc)])
                reg_fin(accb, fin_sm)

        run_gen(gen_mask(0))
        for qc in range(4):
            items = [(gen_attn(qc), 4 * (4 * qc + 4))]
            if qc < 3:
                items.append((gen_mask(qc + 1), mask_steps(qc + 1)))
            interleave(items)
        flush_pending()
        P.do_fence()

        o[0] = A0
        o2[0] = D0
        mq = aalloc([128, 2, S], BF16)
        mk_ = aalloc([128, 2, S], BF16)
        Vaug = aalloc([128, 16, 4, 66], BF16)
        ksf = aalloc([128, 2, 8], F32)
        kshi = aalloc([128, 2, 8], BF16)
        kslo = aalloc([128, 2, 8], BF16)
        gm = aalloc([128, 32], F32)
        t8 = aalloc([128, 4, 8], F32)
        thr4 = aalloc([128, 4], F32)
        negm = aalloc([128, 32], BF16)
        negmT = aalloc([32, S], BF16)
        selb = dalloc([32, 32, 128], BF16)
        pastmask = dalloc([128, 8 * 32], F32)
        dma(pastmask, pastmask_d, w=['pastmask'])
        Pb = [aalloc([128, 512], BF16) for _ in range(3)]
        rrec = aalloc([128, 512], F32)
        bcs = aalloc([128, 512], F32)
        load_strips(8)
        load_const_bf16(selb[:, 0:16, :], sel_d[:, 0:2048].rearrange("p (a b) -> p a b", a=16), 'selb', [32, 16, 128])
        load_const_bf16(selb[:, 16:32, :], sel_d[:, 2048:4096].rearrange("p (a b) -> p a b", a=16), 'selb', [32, 16, 128])
        projT(l, 2888, 256, lambda j, qc: [(mq[:, j, qs(qc)], [('q', j, qc)], slice(0, 128))], scale=0.125)
        projT(l, 3144, 256, lambda j, qc: [(mk_[:, j, qs(qc)], [('k', j, qc)], slice(0, 128))])
        P.add('dve', lambda e: e.memset(Vaug[:, :, :, 64:66], 1.0), w=[('V', t) for t in range(16)])
        projTok(l, 3400, 256, lambda tt, ps: (Vaug[:, tt, :, 0:64], [('V', tt)], ps[:, 0:256].rearrange("p (h d) -> p h d", h=4)))
        for ch in range(2):
            P.add('dve', lambda e, ch=ch: e.reduce_sum(out=ksf[:, ch, :], in_=mk_[:, ch, :].rearrange("p (n k) -> p n k", n=8), axis=AX.X),
                  r=[('k', ch, q) for q in range(4)], w=['ksf'])
        P.add('dve', lambda e: e.tensor_copy(out=kshi, in_=ksf), r=['ksf'], w=['kshi'])
        P.add('dve', lambda e: e.tensor_tensor(out=kslo, in0=ksf, in1=kshi, op=ALU.subtract), r=['ksf', 'kshi'], w=['kslo'])
        def gen_gates(gq):
            for qt in range(4 * gq, 4 * gq + 4):
                own = qt // 2
                bpar = [nb(), nb()]
                for h in range(4):
                    ch, r0 = h // 2, (h % 2) * 64
                    b = bpar[h % 2]
                    lq = mq[r0:r0 + 64, ch, qt * 128:(qt + 1) * 128]
                    mm(psb[b][:, h * 8:(h + 1) * 8], lq, kshi[r0:r0 + 64, ch, :], True, False, r=[('q', ch, qt // 4), 'kshi'], w=[PS(b)], sgc=True)
                    mm(psb[b][:, h * 8:(h + 1) * 8], lq, kslo[r0:r0 + 64, ch, :], False, True, r=[('q', ch, qt // 4), 'kslo'], w=[PS(b)], sgc=True)
                for h in range(4):
                    b = bpar[h % 2]
                    P.add('dve', lambda e, b=b, own=own, h=h: e.tensor_tensor(out=gm[:, h * 8:(h + 1) * 8], in0=psb[b][:, h * 8:(h + 1) * 8], in1=pastmask[:, own * 32 + h * 8:own * 32 + (h + 1) * 8], op=ALU.add),
                          r=['pastmask'], w=[PS(b), 'gm'])
                for h in range(4):
                    P.add('dve', lambda e, h=h: e.max(out=t8[:, h, :], in_=gm[:, h * 8:(h + 1) * 8]), r=['gm'], w=['t8'])
                P.add('dve', lambda e: e.tensor_scalar(out=thr4, in0=t8[:, :, 2], scalar1=-1e29, scalar2=None, op0=ALU.max), r=['t8'], w=['thr4'])
                for h in range(4):
                    P.add('dve', lambda e, h=h: e.tensor_scalar(out=negm[:, h * 8:(h + 1) * 8], in0=gm[:, h * 8:(h + 1) * 8], scalar1=thr4[:, h:h + 1], scalar2=NEG, op0=ALU.is_lt, op1=ALU.mult),
                          r=['gm', 'thr4'], w=['negm'])
                P.add('pe', lambda e: e.transpose(out=psT[0:32, 0:128], in_=negm, identity=identb), r=['negm', 'identb'], w=['psT'])
                evac(negmT[0:32, qt * 128:(qt + 1) * 128], psT[0:32, 0:128], [('negmT', qt // 4)], 'T')
                yield
        def gen_attn_mb(qc):
            for h in range(4):
                och, r0 = 6 + h // 2, (h % 2) * 64
                ch = h // 2

                def mb_extra(b1, qc_, kb, c0, h=h):
                    nbk = kb // 2
                    if nbk < 2 * qc_:
                        return [(psb[b1][:, c0:512], selb[0:32, h * 8 + nbk, :], negmT[0:32, qs(qc_, c0)], ['selb', ('negmT', qc_)])]
                    if nbk == 2 * qc_:
                        return [(psb[b1][:, 256:512], selb[0:32, h * 8 + nbk, :], negmT[0:32, qs(qc_, 256)], ['selb', ('negmT', qc_)])]
                    return []
                holder = []
                yield from attn_soft_qc_g(qc, lambda kb, ch=ch, r0=r0: mk_[r0:r0 + 64, ch, kb * 128:(kb + 1) * 128],
                                          lambda qc_, c0, ch=ch, r0=r0: mq[r0:r0 + 64, ch, qs(qc_, c0)],
                                          [('k', ch, q) for q in range(4)], [('q', ch, qc)], stripb[:, h, :], c31[:, 8 + h:9 + h],
                                          lambda kb, h=h: Vaug[:, kb, h, 0:65], None, Pb, vkeyfn=lambda kb: ('V', kb), extra_fn=mb_extra, holder=holder)
                accb = holder[0]
                for st_ in soft_norm_stages(accb, rrec, bcs):
                    reg_fin(accb, st_)

                def fin_sm(accb=accb, och=och, r0=r0, qc=qc, bcs=bcs):
                    P.add('dve', lambda e, accb=accb, och=och, r0=r0, qc=qc, bcs=bcs: e.tensor_tensor(out=oT[r0:r0 + 64, och, qs(qc)], in0=psb[accb][0:64, :], in1=bcs[0:64, :], op=ALU.mult),
                              r=['bcs'], w=[PS(accb), ('o', och, qc)])
                reg_fin(accb, fin_sm)

        run_gen(gen_gates(0))
        for qc in range(4):
            items = [(gen_attn_mb(qc), 4 * (4 * qc + 4))]
            if qc < 3:
                items.append((gen_gates(qc + 1), 4))
            interleave(items)
        flush_pending()
        P.do_fence()
        if dbg and 'stop_br' in dbg:
            break

        o[0] = A0
        macc = aalloc([128, 2, 4, 512], F32)
        sgb = [aalloc([128, 512], F32) for _ in range(2)]
        tb = [aalloc([128, 512], F32) for _ in range(2)]
        assert o[0] <= A0 + 3 * 8192
        for c in range(3, 8):
            dma(xT[:, c, :], xpark[:, c * S:(c + 1) * S], w=[('x', c, q) for q in range(4)])
        kk_ = 0
        for jp in range(4):
            for i in range(4):
                wg, wgk = load_w(w_in[l, :, 3656 + i * 1024 + jp * 256: 3656 + i * 1024 + (jp + 1) * 256], 8, 256)
                wb, wbk = load_w(w_br[l, i, :, jp * 256:(jp + 1) * 256], 2, 256)
                for jj in range(2):
                    j = jp * 2 + jj
                    for qc in range(4):
                        bg = nb()
                        for c in range(8):
                            mm(psb[bg][:, :], wg[:, c, jj * 128:(jj + 1) * 128], hT[:, c, qs(qc)], c == 0, c == 7, r=[wgk, ('h', c, qc)], w=[PS(bg)], nofence=True)
                        by = nb()
                        for c2 in range(2):
                            mm(psb[by][:, :], wb[:, c2, jj * 128:(jj + 1) * 128], oT[:, 2 * i + c2, qs(qc)], c2 == 0, c2 == 1, r=[wbk, ('o', 2 * i + c2, qc)], w=[PS(by)])
                        k2 = kk_ % 2
                        kk_ += 1
                        sg, tt_ = sgb[k2], tb[k2]
                        P.add('act', lambda e, sg=sg, bg=bg: e.activation(out=sg, in_=psb[bg][:, :], func=AF.Sigmoid), w=[PS(bg), ('sg', k2)])
                        mslc = macc[:, jj, qc, :]
                        if i == 0:
                            P.add('dve', lambda e, sg=sg, by=by, mslc=mslc: e.tensor_tensor(out=mslc, in0=sg, in1=psb[by][:, :], op=ALU.mult),
                                  r=[('sg', k2)], w=[PS(by), ('macc', jj, qc)])
                        else:
                            P.add('dve', lambda e, sg=sg, by=by, tt_=tt_: e.tensor_tensor(out=tt_, in0=sg, in1=psb[by][:, :], op=ALU.mult),
                                  r=[('sg', k2)], w=[PS(by), ('mt', k2)])
                            if i < 3:
                                P.add('pool' if kk_ % 3 == 0 else 'dve', lambda e, mslc=mslc, tt_=tt_: e.tensor_tensor(out=mslc, in0=mslc, in1=tt_, op=ALU.add),
                                      r=[('mt', k2)], w=[('macc', jj, qc)])
                            else:
                                dst = mT[:, j, qs(qc)]
                                P.add('pool' if kk_ % 3 == 0 else 'dve', lambda e, mslc=mslc, tt_=tt_, dst=dst: e.tensor_tensor(out=dst, in0=mslc, in1=tt_, op=ALU.add),
                                      r=[('mt', k2), ('macc', jj, qc)], w=[('m', j, qc)])
        P.do_fence()

        for c in range(0, 3):
            dma(xT[:, c, :], xpark[:, c * S:(c + 1) * S], w=[('x', c, q) for q in range(4)])
        wout_bf = view(C0, [128, 8, 1024], BF16)
        ptmp = [view(C0 + 16384 + 2048 * i_, [128, 512], F32) for i_ in range(2)]
        yTb = [view(B0 + 16384 * i_, [128, 8, 512], F32) for i_ in range(2)]
        for t in range(4):
            s_ = cnt['wst'] % 2
            cnt['wst'] += 1
            stg = wst[s_]
            dma(stg, w_out[l, :, t * 256:(t + 1) * 256].rearrange("(c p) n -> p c n", p=128), w=[('wst', s_)])
            dstw = wout_bf[:, :, t * 256:(t + 1) * 256]
            P.add('act', lambda e, dstw=dstw, stg=stg: e.copy(out=dstw, in_=stg), r=[('wst', s_)], w=[('wout', t)])

        def post_norm_residual(gt, y, ykey, qc, l=l):
            bs = nb()
            for j in range(8):
                sq = sqb[j % 2]
                ys = y[:, j, :]
                P.add('act', lambda e, sq=sq, ys=ys: e.activation(out=sq, in_=ys, func=AF.Square), r=[ykey(j)], w=[('sq', j % 2)])
                mm(psb[bs][:, :], onesF, sq, j == 0, j == 7, r=[('sq', j % 2), 'onesF'], w=[PS(bs)])
            P.add('act', lambda e, bs=bs: e.activation(out=lnt, in_=psb[bs][:, :], func=AF.Ln, scale=1.0 / D, bias=EPS), w=[PS(bs), ('sq', 0)])
            P.add('act', lambda e: e.activation(out=rstd, in_=lnt, func=AF.Exp, scale=-0.5), r=[('sq', 0)], w=['rstd'])
            for j in range(8):
                pt = ptmp[j % 2]
                ys = y[:, j, :]
                g = gcol(gt, l, j)
                xs = xT[:, j, qs(qc)]
                P.add('dve', lambda e, pt=pt, ys=ys, g=g: e.scalar_tensor_tensor(out=pt, in0=ys, scalar=g, in1=rstd, op0=ALU.mult, op1=ALU.mult),
                      r=[ykey(j), 'rstd', 'gains'], w=[('pt', j % 2)])
                P.add('pool' if j % 4 == 3 else 'dve', lambda e, pt=pt, xs=xs: e.tensor_tensor(out=xs, in0=xs, in1=pt, op=ALU.add), r=[('pt', j % 2)], w=[('x', j, qc)])

        for qc in range(4):
            y = yTb[qc % 2]
            for j in range(8):
                b = nb()
                for c in range(8):
                    mm(psb[b][:, :], wout_bf[:, c, j * 128:(j + 1) * 128], mT[:, c, qs(qc)], c == 0, c == 7, r=[('wout', j // 2), ('m', c, qc)], w=[PS(b)])
                evac(y[:, j, :], psb[b][:, :], [('y', qc % 2, j)], b)
            post_norm_residual(1, y, lambda j, qc=qc: ('y', qc % 2, j), qc)
        P.do_fence()
        if dbg and 'stop_mix' in dbg:
            break

        rmsnorm_to_h(l, 2)
        uT = view(C0, [128, 22, 1024], BF16)
        yF = view(C0 + 45056, [128, 8, 512], F32)
        ptmp = [view(C0 + 61440 + 2048 * i_, [128, 512], F32) for i_ in range(2)]
        kk_ = 0
        for th in range(2):
            for fp in range(11):
                wg, wgk = load_w(w_f1[l, :, fp * 256:(fp + 1) * 256], 8, 256)
                wu, wuk = load_w(w_f1[l, :, DFF + fp * 256: DFF + (fp + 1) * 256], 8, 256)
                for ff in range(2):
                    f = fp * 2 + ff
                    for q2 in range(2):
                        qc = th * 2 + q2
                        bg = nb()
                        for c in range(8):
                            mm(psb[bg][:, :], wg[:, c, ff * 128:(ff + 1) * 128], hT[:, c, qs(qc)], c == 0, c == 7, r=[wgk, ('h', c, qc)], w=[PS(bg)], nofence=True)
                        bu = nb()
                        for c in range(8):
                            mm(psb[bu][:, :], wu[:, c, ff * 128:(ff + 1) * 128], hT[:, c, qs(qc)], c == 0, c == 7, r=[wuk, ('h', c, qc)], w=[PS(bu)], nofence=True)
                        k2 = kk_ % 2
                        kk_ += 1
                        sg = sqb[k2]
                        P.add('act', lambda e, sg=sg, bg=bg: e.activation(out=sg, in_=psb[bg][:, :], func=AF.Silu), w=[PS(bg), ('sq', k2)])
                        us = uT[:, f, q2 * 512:(q2 + 1) * 512]
                        P.add('dve', lambda e, sg=sg, bu=bu, us=us: e.tensor_tensor(out=us, in0=sg, in1=psb[bu][:, :], op=ALU.mult),
                              r=[('sq', k2)], w=[PS(bu), ('u', f, q2)])
            for q2 in range(2):
                qc = th * 2 + q2
                for jp in range(4):
                    banks = [nb(), nb()]
                    for kg in range(3):
                        nk = 8 if kg < 2 else 6
                        w2, w2k = load_w(w_f2[l, kg * 1024: kg * 1024 + nk * 128, jp * 256:(jp + 1) * 256], nk, 256, eng='dve')
                        for jj in range(2):
                            for fk in range(nk):
                                f = kg * 8 + fk
                                mm(psb[banks[jj]][:, :], w2[:, fk, jj * 128:(jj + 1) * 128], uT[:, f, q2 * 512:(q2 + 1) * 512], f == 0, f == 21,
                                   r=[w2k, ('u', f, q2)], w=[PS(banks[jj])], sgc=True)
                    for jj in range(2):
                        evac(yF[:, jp * 2 + jj, :], psb[banks[jj]][:, :], [('yf', jp * 2 + jj)], banks[jj])
                post_norm_residual(3, yF, lambda j: ('yf', j), qc)
        P.do_fence()
    if dbg and 'thr' in dbg:
        dma(dbg_out['thr'][0:128, 0:16], thrAll, r=['thrAll'])
    if dbg and 'o' in dbg:
        tmp = view(A0, [128, 8, S], F32)
        for c in range(8):
            P.add('dve', lambda e, c=c: e.tensor_copy(out=tmp[:, c, :], in_=oT[:, c, :]), r=[('o', c, q) for q in range(4)], w=[('tmp', c)])
            dma(dbg_out['o'][c * 128:(c + 1) * 128, :], tmp[:, c, :], r=[('tmp', c)])
    elif dbg and 'x' in dbg:
        for c in range(8):
            dma(dbg_out['x'][c * 128:(c + 1) * 128, :], xT[:, c, :], r=[('x', c, q) for q in range(4)])
    else:
        for c in range(8):
            dma(outT_d[c * 128:(c + 1) * 128, :], xT[:, c, :], r=[('x', c, q) for q in range(4)])
    P.emit(st)
    st.close()
    return nc, P


def host_consts(inputs):
    rb = np.asarray(inputs['rel_bias'], np.float32)
    c = {}
    g = np.stack([np.asarray(inputs[k], np.float32) for k in ['g_pre_mix', 'g_post_mix', 'g_pre_ffn', 'g_post_ffn']])
    g = g.reshape(4, DEPTH, 8, 128).transpose(3, 0, 1, 2).reshape(128, 4 * DEPTH * 8)
    c['gains'] = np.ascontiguousarray(g)
    kl = np.arange(128)[:, None]
    r = np.arange(640)[None, :]
    dist = r - kl
    bucket = _t5_bucket_np(dist)
    strips = rb[bucket]
    strips = np.where((dist < 0)[:, :, None], np.float32(NEG), strips)
    c['strips'] = np.ascontiguousarray(strips.transpose(2, 0, 1).astype(np.float32))
    c['c31'] = np.ascontiguousarray(np.broadcast_to(rb[31][None, :], (128, 12)).astype(np.float32))
    lv = np.stack([np.asarray(inputs[k], np.float32) for k in ['lambda_q1', 'lambda_k1', 'lambda_q2', 'lambda_k2']], axis=1)
    c['lamv'] = np.ascontiguousarray(np.broadcast_to(lv.reshape(1, -1), (128, DEPTH * 4 * 32)).astype(np.float32))
    sg = np.asarray(inputs['diff_subln_g'], np.float32)
    c['subg'] = np.ascontiguousarray(np.concatenate([sg.T, sg.T], axis=0))
    c['ident'] = np.eye(128, dtype=np.float32)
    j = np.arange(128)[:, None]
    k = np.arange(128)[None, :]
    c['negT'] = np.where(j >= k, -1.0, 0.0).astype(np.float32)
    c['sbmask'] = np.where(k <= j, NEG, 0.0).astype(np.float32)
    c['cmaskq'] = np.where(k > j, -1e30, 0.0).astype(np.float32)
    pm = np.zeros((128, 8, 4, 8), np.float32)
    for own in range(8):
        for nbk in range(8):
            if not (nbk < own):
                pm[:, own, :, nbk] = -1e30
    c['pastmask'] = pm.reshape(128, 256)
    sel = np.zeros((32, 32, 128), np.float32)
    for i in range(32):
        sel[i, i, :] = 1.0
    c['sel'] = sel.reshape(32, 32 * 128)
    hm = np.zeros((128, 4), np.float32)
    for p in range(128):
        hm[p, p // 32] = 1.0
    c['headmask'] = hm
    c['pow2tab'] = np.ascontiguousarray(np.broadcast_to((0.5 ** np.arange(1, 25, dtype=np.float64)).astype(np.float32)[None, :], (128, 24)))
    return c


_CACHE = {}


def kernel(**inputs):
    x = np.asarray(inputs['x'], np.float32)
    consts = host_consts(inputs)
    w_br = np.ascontiguousarray(np.stack([np.asarray(inputs[k], np.float32) for k in ['w_br_sb', 'w_br_diff', 'w_br_dsa', 'w_br_moba']], axis=1))
    shared = dict(consts)
    shared['w_in'] = np.ascontiguousarray(np.asarray(inputs['w_in'], np.float32))
    shared['w_br'] = w_br
    shared['w_out'] = np.ascontiguousarray(np.asarray(inputs['w_out'], np.float32))
    shared['w_ffn_in'] = np.ascontiguousarray(np.asarray(inputs['w_ffn_in'], np.float32))
    shared['w_ffn_out'] = np.ascontiguousarray(np.asarray(inputs['w_ffn_out'], np.float32))
    if 'nc' not in _CACHE:
        _CACHE['nc'] = build_program(list(range(DEPTH)))[0]
    nc = _CACHE['nc']
    in_maps = []
    for b in range(8):
        m = dict(shared)
        m['xT'] = np.ascontiguousarray(x[b].T)
        in_maps.append(m)
    res = run_bass_kernel_spmd(nc, in_maps, core_ids=list(range(8)))
    out = np.stack([np.ascontiguousarray(r['outT'].T) for r in res.results], axis=0)
    return out.astype(np.float32)
```

```python
import math
from contextlib import ExitStack
import numpy as np
import concourse.bass as bass
import concourse.mybir as mybir
from concourse.bass_utils import run_bass_kernel_spmd

F32 = mybir.dt.float32
F32R = mybir.dt.float32r
BF16 = mybir.dt.bfloat16
ALU = mybir.AluOpType
AF = mybir.ActivationFunctionType
AX = mybir.AxisListType
NS = 8
ENGS = ['pe', 'act', 'dve', 'pool', 'sp']

D = 1024
S = 2048
DEPTH = 4
NIN = 7752
DFF = 2816
EPS = 1e-6
NEG = -30000.0


class Prog:
    def __init__(self, nc):
        self.nc = nc
        self.ops = []
        self.lastw = {}
        self.readers = {}
        self.fence = {}

    def add(self, eng, fn, r=(), w=(), nofence=False):
        i = len(self.ops)
        deps = set()
        for k in r:
            if k in self.lastw:
                deps.add(self.lastw[k])
        for k in w:
            if k in self.lastw:
                deps.add(self.lastw[k])
            deps.update(self.readers.get(k, ()))
        if eng in self.fence and not nofence:
            deps.update(self.fence.pop(eng))
        red = {}
        out = set()
        for d in deps:
            e = self.ops[d][0]
            if e == 'sp':
                out.add(d)
            else:
                red[e] = max(red.get(e, -1), d)
        out.update(red.values())
        for k in r:
            self.readers.setdefault(k, []).append(i)
        for k in w:
            self.lastw[k] = i
            self.readers[k] = []
        self.ops.append((eng, fn, out))
        return i

    def do_fence(self):
        last = {}
        for i, (e, _, _) in enumerate(self.ops):
            if e == 'sp':
                last.setdefault(e, []).append(i)
                last[e] = last[e][-NS:]
            else:
                last[e] = [i]
        allidx = set(i for v in last.values() for i in v)
        for e in ENGS:
            self.fence[e] = set(allidx)

    def emit(self, stack):
        nc = self.nc
        ops = self.ops
        need = [False] * len(ops)
        for (e, fn, deps) in ops:
            for d in deps:
                if ops[d][0] == 'pe' and e == 'pe':
                    continue
                need[d] = True
        for i, (e, _, _) in enumerate(ops):
            if e == 'sp':
                need[i] = True
        sig = {}
        cnt = {}
        ndma = 0
        extra = {}
        for i, (e, fn, deps) in enumerate(ops):
            if not need[i]:
                continue
            if e == 'sp':
                s = ('sp', ndma % NS)
                ndma += 1
                c = cnt.get(s, 0)
                if c > 0:
                    extra[i] = (s, c)
                cnt[s] = c + 16
                sig[i] = (s, c + 16)
            else:
                s = (e, 0)
                cnt[s] = cnt.get(s, 0) + 1
                sig[i] = (s, cnt[s])
        sems = {}
        for s in cnt:
            sems[s] = stack.enter_context(nc.semaphore("sem_%s_%d" % s))
        idxs = {e: [] for e in ENGS}
        for i, (e, _, _) in enumerate(ops):
            idxs[e].append(i)

        def mk(e):
            def body(eng):
                waited = {}
                for i in idxs[e]:
                    _, fn, deps = ops[i]
                    ws = {}
                    for d in deps:
                        if ops[d][0] == 'pe' and e == 'pe':
                            continue
                        s, c = sig[d]
                        if waited.get(s, 0) < c:
                            ws[s] = max(ws.get(s, 0), c)
                    if i in extra:
                        s, c = extra[i]
                        if waited.get(s, 0) < c:
                            ws[s] = max(ws.get(s, 0), c)
                    for s, c in ws.items():
                        eng.wait_ge(sems[s], c)
                        waited[s] = c
                    ins = fn(eng)
                    if i in sig:
                        ins.then_inc(sems[sig[i][0]], 16 if e == 'sp' else 1)
                if e == 'sp':
                    for s, c in cnt.items():
                        if s[0] == 'sp' and waited.get(s, 0) < c:
                            eng.wait_ge(sems[s], c)
            return body

        with nc.Block() as block:
            block.tensor(mk('pe'))
            block.scalar(mk('act'))
            block.vector(mk('dve'))
            block.gpsimd(mk('pool'))
            block.sync(mk('sp'))


def _t5_bucket_np(d):
    d = np.maximum(d, 0)
    lr = np.log(np.maximum(d, 1).astype(np.float32) / np.float32(16)) / np.float32(math.log(128 / 16))
    large = np.minimum(16 + (lr * 16).astype(np.int32), 31)
    return np.where(d < 16, d, large)


def build_program(layers, dbg=None):
    nc = bass.Bass("TRN2", target_bir_lowering=False)
    dr = {}

    def din(name, shape):
        dr[name] = nc.dram_tensor(name, list(shape), F32, kind="ExternalInput").ap()
        return dr[name]

    xT_d = din("xT", [D, S])
    w_in = din("w_in", [DEPTH, D, NIN])
    w_br = din("w_br", [DEPTH, 4, 256, D])
    w_out = din("w_out", [DEPTH, D, D])
    w_f1 = din("w_ffn_in", [DEPTH, D, 2 * DFF])
    w_f2 = din("w_ffn_out", [DEPTH, DFF, D])
    gains_d = din("gains", [128, 4 * DEPTH * 8])
    strips_d = din("strips", [12, 128, 640])
    c31_d = din("c31", [128, 12])
    lamv_d = din("lamv", [128, DEPTH * 4 * 32])
    subg_d = din("subg", [128, DEPTH])
    ident_d = din("ident", [128, 128])
    negT_d = din("negT", [128, 128])
    sbmask_d = din("sbmask", [128, 128])
    cmaskq_d = din("cmaskq", [128, 128])
    pastmask_d = din("pastmask", [128, 8 * 32])
    sel_d = din("sel", [32, 32 * 128])
    headmask_d = din("headmask", [128, 4])
    pow2_d = din("pow2tab", [128, 24])
    outT_d = nc.dram_tensor("outT", [D, S], F32, kind="ExternalOutput").ap()
    xpark = nc.dram_tensor("xpark", [128, 8 * S], F32, kind="Internal").ap()
    dbg_out = {}
    if dbg:
        for n in dbg:
            dbg_out[n] = nc.dram_tensor("dbg_" + n, [D, S], F32, kind="ExternalOutput").ap()

    st = ExitStack()
    NW = 51384
    big = st.enter_context(nc.sbuf_tensor("SB", [128, NW], F32))
    r_sp = [st.enter_context(nc.sbuf_tensor("r_sp%d" % i, [128, 512], F32R)) for i in range(2)]
    r_c = st.enter_context(nc.sbuf_tensor("r_c", [128, 512], F32R))
    r_negT = st.enter_context(nc.sbuf_tensor("r_negT", [128, 128], F32R))
    r_negOnes = st.enter_context(nc.sbuf_tensor("r_negOnes", [128, 128], F32R))
    thr_t = st.enter_context(nc.sbuf_tensor("thrAll", [128, 16], F32))
    thrAll = thr_t[:, :]
    psb = [st.enter_context(nc.psum_tensor("ps%d" % i, [128, 512], F32)) for i in range(7)]
    psT = st.enter_context(nc.psum_tensor("psT", [128, 1024], BF16))

    def view(off, shape, dt):
        assert off % 4 == 0
        n = int(np.prod(shape[1:]))
        if dt == F32:
            assert off // 4 + n <= NW, (off, shape)
            ap = big[:shape[0], off // 4: off // 4 + n]
        else:
            assert n % 2 == 0 and off // 4 + n // 2 <= NW, (off, shape)
            ap = big[:shape[0], off // 4: off // 4 + n // 2].bitcast(BF16)
        if len(shape) == 3:
            ap = ap.rearrange("p (a b) -> p a b", a=shape[1])
        elif len(shape) == 4:
            ap = ap.rearrange("p (a b c) -> p a b c", a=shape[1], b=shape[2])
        return ap

    A0, B0, C0, D0, E0 = 0, 65536, 98304, 131072, 163840
    xT = view(A0, [128, 8, S], F32)
    hT = view(B0, [128, 8, S], BF16)
    oT = view(C0, [128, 8, S], BF16)
    mT = view(D0, [128, 8, S], BF16)
    eo = [E0]

    def ealloc(shape, dt):
        n = int(np.prod(shape[1:])) * (4 if dt == F32 else 2)
        n = (n + 31) // 32 * 32
        v = view(eo[0], shape, dt)
        eo[0] += n
        return v

    wst = [ealloc([128, 8, 256], F32) for _ in range(2)]
    wbf = [ealloc([128, 8, 256], BF16) for _ in range(3)]
    identb = ealloc([128, 128], BF16)
    onesF = ealloc([128, 128], F32)
    negTr = r_negT[:, :]
    negOnesr = r_negOnes[:, :]
    sbmaskb = ealloc([128, 128], BF16)
    gains = ealloc([128, 4 * DEPTH * 8], F32)
    c31 = ealloc([128, 12], F32)
    subg = ealloc([128, DEPTH], F32)
    headmask = ealloc([128, 4], F32)
    pow2tab = ealloc([128, 24], F32)
    stripb = ealloc([128, 4, 640], BF16)
    rstd = ealloc([128, 512], F32)
    sqb = [ealloc([128, 512], F32) for _ in range(2)]
    lnt = sqb[0]
    SPb = [r_sp[0][:, :], r_sp[1][:, :]]
    Cb = r_c[:, :]
    assert eo[0] <= NW * 4, eo[0]

    P = Prog(nc)
    cnt = {'ps': 0, 'acc': 0, 'wst': 0, 'wbf': 0, 'ev': 0}
    pending = []
    acc_pending = {5: 0, 6: 0}

    def flush_pending():
        while pending:
            pending.pop(0)()

    def flush_one():
        if pending:
            pending.pop(0)()

    def reg_fin(accb, fn):
        acc_pending[accb] += 1

        def w_():
            fn()
            acc_pending[accb] -= 1
        pending.append(w_)

    live = set()

    def nb():
        for _ in range(5):
            cnt['ps'] += 1
            b = cnt['ps'] % 5
            if b not in live:
                live.add(b)
                return b
        raise AssertionError("no free PSUM bank")

    _orig_add = P.add

    def _add2(eng, fn, r=(), w=(), nofence=False):
        if eng != 'pe':
            for k in w:
                if isinstance(k, tuple) and len(k) == 2 and k[0] == 'ps' and k[1] in live:
                    live.discard(k[1])
        return _orig_add(eng, fn, r=r, w=w, nofence=nofence)
    P.add = _add2

    def nacc():
        cnt['acc'] += 1
        b = 5 + cnt['acc'] % 2
        if acc_pending[b] > 0:
            flush_pending()
        return b

    def PS(b):
        return 'psT' if b == 'T' else ('ps', b)

    def qs(qc, c0=0):
        return slice(512 * qc + c0, 512 * (qc + 1))

    def gcol(gt, l, c):
        i = (gt * DEPTH + l) * 8 + c
        return gains[:, i:i + 1]

    def dma(out, in_, r=(), w=(), nofence=False):
        P.add('sp', lambda e: e.dma_start(out=out, in_=in_), r=r, w=w, nofence=nofence)

    def mm(out, lhsT, rhs, start, stop, r=(), w=(), sgc=False, nofence=False):
        P.add('pe', lambda e: e.matmul(out, lhsT=lhsT, rhs=rhs, start=start, stop=stop, skip_group_check=sgc), r=r, w=w, nofence=nofence)

    def evac(dst, src, dk, b, scale=None, eng=None):
        cnt['ev'] += 1
        if eng is None:
            eng = 'act' if cnt['ev'] % 2 == 0 else 'dve'
        if eng == 'act':
            if scale is None:
                P.add('act', lambda e: e.copy(out=dst, in_=src), w=[PS(b)] + list(dk))
            else:
                P.add('act', lambda e: e.mul(out=dst, in_=src, mul=scale), w=[PS(b)] + list(dk))
        else:
            if scale is None:
                P.add('dve', lambda e: e.tensor_copy(out=dst, in_=src), w=[PS(b)] + list(dk))
            else:
                P.add('dve', lambda e: e.tensor_scalar(out=dst, in0=src, scalar1=scale, scalar2=None, op0=ALU.mult), w=[PS(b)] + list(dk))

    def load_w(src, nk, ncols, eng='act'):
        s = cnt['wst'] % 2
        cnt['wst'] += 1
        b = cnt['wbf'] % 3
        cnt['wbf'] += 1
        stg = wst[s][:, 0:nk, 0:ncols]
        dma(stg, src.rearrange("(c p) n -> p c n", p=128), w=[('wst', s)], nofence=True)
        dst = wbf[b][:, 0:nk, 0:ncols]
        if eng == 'act':
            P.add('act', lambda e: e.copy(out=dst, in_=stg), r=[('wst', s)], w=[('wbf', b)], nofence=True)
        else:
            P.add(eng, lambda e: e.tensor_copy(out=dst, in_=stg), r=[('wst', s)], w=[('wbf', b)], nofence=True)
        return wbf[b], ('wbf', b)

    def load_const_f32(dst, src, key):
        dma(dst, src, w=[key])

    def load_const_bf16(dst, src, key, shape):
        s = cnt['wst'] % 2
        cnt['wst'] += 1
        n = int(np.prod(shape[1:]))
        stg = wst[s].rearrange("p a b -> p (a b)")[:shape[0], 0:n]
        if len(shape) == 3:
            stg = stg.rearrange("p (a b) -> p a b", a=shape[1])
        dma(stg, src, w=[('wst', s)])
        P.add('dve', lambda e: e.tensor_copy(out=dst, in_=stg), r=[('wst', s)], w=[key])

    load_const_bf16(identb, ident_d, 'identb', [128, 128])
    load_const_bf16(sbmaskb, sbmask_d, 'sbmaskb', [128, 128])
    load_const_f32(gains, gains_d, 'gains')
    load_const_f32(c31, c31_d, 'c31')
    load_const_f32(subg, subg_d, 'subg')
    load_const_f32(headmask, headmask_d, 'headmask')
    load_const_f32(pow2tab, pow2_d, 'pow2tab')
    P.add('dve', lambda e: e.memset(onesF, 1.0), w=['onesF'])
    _s = cnt['wst'] % 2
    cnt['wst'] += 1
    _stg = wst[_s].rearrange("p a b -> p (a b)")[:, 0:128]
    dma(_stg, negT_d, w=[('wst', _s)])
    P.add('dve', lambda e: e.tensor_copy(out=negTr, in_=_stg), r=[('wst', _s)], w=['negTr'])
    P.add('dve', lambda e: e.tensor_scalar(out=negOnesr, in0=onesF, scalar1=-1.0, scalar2=None, op0=ALU.mult), r=['onesF'], w=['negOnesr'])
    for c in range(8):
        dma(xT[:, c, :], xT_d[c * 128:(c + 1) * 128, :], w=[('x', c, q) for q in range(4)])

    def rmsnorm_to_h(l, gt):
        for qc in range(4):
            b = nb()
            for c in range(8):
                sq = sqb[c % 2]
                xs = xT[:, c, qs(qc)]
                P.add('act', lambda e, sq=sq, xs=xs: e.activation(out=sq, in_=xs, func=AF.Square), r=[('x', c, qc)], w=[('sq', c % 2)])
                mm(psb[b][:, :], onesF, sq, c == 0, c == 7, r=[('sq', c % 2), 'onesF'], w=[PS(b)])
            P.add('act', lambda e, b=b: e.activation(out=lnt, in_=psb[b][:, :], func=AF.Ln, scale=1.0 / D, bias=EPS), w=[PS(b), ('sq', 0)])
            P.add('act', lambda e: e.activation(out=rstd, in_=lnt, func=AF.Exp, scale=-0.5), r=[('sq', 0)], w=['rstd'])
            for c in range(8):
                xs = xT[:, c, qs(qc)]
                hs = hT[:, c, qs(qc)]
                g = gcol(gt, l, c)
                P.add('dve', lambda e, xs=xs, hs=hs, g=g: e.scalar_tensor_tensor(out=hs, in0=xs, scalar=g, in1=rstd, op0=ALU.mult, op1=ALU.mult),
                      r=[('x', c, qc), 'rstd', 'gains'], w=[('h', c, qc)])

    def projT(l, col0, ncols_total, dst_fn, scale=None):
        for t0 in range(0, ncols_total, 256):
            ncl = min(256, ncols_total - t0)
            w, wk = load_w(w_in[l, :, col0 + t0: col0 + t0 + ncl], 8, ncl)
            for jj in range(0, ncl, 128):
                m = min(128, ncl - jj)
                for qc in range(4):
                    b = nb()
                    for c in range(8):
                        mm(psb[b][0:m, :], w[:, c, jj:jj + m], hT[:, c, qs(qc)], c == 0, c == 7, r=[wk, ('h', c, qc)], w=[PS(b)], nofence=True)
                    for (dst, dk, rows) in dst_fn((t0 + jj) // 128, qc):
                        evac(dst, psb[b][rows, :], dk, b, scale)

    def projTok(l, col0, ncols, dst_fn):
        w, wk = load_w(w_in[l, :, col0: col0 + ncols], 8, ncols)
        for tt in range(16):
            b = nb()
            for c in range(8):
                mm(psb[b][:, 0:ncols], hT[:, c, tt * 128:(tt + 1) * 128], w[:, c, 0:ncols], c == 0, c == 7, r=[wk, ('h', c, tt // 4)], w=[PS(b)], nofence=True)
            dst, dk, src = dst_fn(tt, psb[b])
            evac(dst, src, dk, b)

    def attn_soft_qc(*a, **k):
        holder = []
        for _ in attn_soft_qc_g(*a, holder=holder, **k):
            pass
        return holder[0]

    def attn_soft_qc_g(qc, kfn, qfn, kkeys, qkeys, strip, c31col, vfn, vkey, Pbuf, extra_fn=None, extra_keys=(), vkeyfn=None, holder=None):
        accb = nacc()
        holder.append(accb)
        last = 4 * qc + 3
        info = {}

        def stA(kb):
            ks = kb * 128
            c0 = max(0, ks - 512 * qc)
            n = 512 - c0
            near = kb >= 4 * qc - 1
            b1 = nb()
            mms = [(psb[b1][:, c0:512], kfn(kb), qfn(qc, c0), list(kkeys) + list(qkeys))]
            if near:
                r0 = 512 * qc + c0 - ks
                mms.append((psb[b1][:, c0:512], identb, strip[:, r0:r0 + n], ['identb', 'strip']))
            if extra_fn is not None:
                mms += extra_fn(b1, qc, kb, c0)
            for i, (o_, lt, rh, rk) in enumerate(mms):
                mm(o_, lt, rh, i == 0, i == len(mms) - 1, r=rk + list(extra_keys), w=[PS(b1)], sgc=True)
            info[kb] = (b1, c0, near)

        def stB(kb):
            b1, c0, near = info[kb]
            pi = kb % len(Pbuf)
            Pt = Pbuf[pi]
            if near:
                P.add('act', lambda e, Pt=Pt, b1=b1, c0=c0: e.activation(out=Pt[:, c0:512], in_=psb[b1][:, c0:512], func=AF.Exp),
                      w=[PS(b1), ('P', pi)])
            else:
                P.add('act', lambda e, Pt=Pt, b1=b1, c0=c0: e.activation(out=Pt[:, c0:512], in_=psb[b1][:, c0:512], func=AF.Exp, bias=c31col),
                      r=['c31'], w=[PS(b1), ('P', pi)])

        def stC(kb):
            b1, c0, near = info[kb]
            pi = kb % len(Pbuf)
            Pt = Pbuf[pi]
            mm(psb[accb][0:65, c0:512], vfn(kb), Pt[:, c0:512], kb == 0, kb == last, r=[('P', pi), vkeyfn(kb)], w=[PS(accb)], sgc=True)

        stA(0)
        if last >= 1:
            stA(1)
        for kb in range(0, last + 1):
            if kb + 2 <= last:
                stA(kb + 2)
            stB(kb)
            if kb >= 1:
                stC(kb - 1)
            flush_one()
            yield
        stC(last)

    def soft_norm_stages(accb, rrec, bcs):
        def s1():
            P.add('dve', lambda e: e.reciprocal(out=rrec[64:65, :], in_=psb[accb][64:65, :]), w=[PS(accb), 'rrec'])

        def s2():
            bb = nb()
            mm(psb[bb][0:64, :], onesF[64:65, 0:64], rrec[64:65, :], True, True, r=['rrec', 'onesF'], w=[PS(bb)])
            P.add('act', lambda e: e.copy(out=bcs[0:64, :], in_=psb[bb][0:64, :]), w=[PS(bb), 'bcs'])
        return [s1, (lambda: None), (lambda: None), s2]

    def interleave(items):
        st_ = [[g, max(1, est), 0] for g, est in items]
        while st_:
            st_.sort(key=lambda t: t[2] / t[1])
            t = st_[0]
            try:
                next(t[0])
                t[2] += 1
            except StopIteration:
                st_.remove(t)

    def run_gen(g):
        for _ in g:
            pass

    def score_tile_g(qt, scbuf, skey, qiT, kiT, wi, rl, itc, every=2):
        L = (qt + 1) * 128
        step = 0
        for ih in range(8):
            ich, ir0 = ih // 2, (ih % 2) * 64
            for kc in range((L + 511) // 512):
                n = min(512, L - kc * 512)
                b = nb()
                mm(psb[b][:, 0:n], qiT[ir0:ir0 + 64, ich, qt * 128:(qt + 1) * 128], kiT[ir0:ir0 + 64, kc * 512:kc * 512 + n], True, True,
                   r=[('qi', ich, qt // 4), ('ki', kc)], w=[PS(b)])
                i2 = itc[0] % 2
                itc[0] += 1
                rt = rl[i2]
                P.add('act', lambda e, rt=rt, b=b, n=n: e.activation(out=rt[:, 0:n], in_=psb[b][:, 0:n], func=AF.Relu), w=[PS(b), ('rl', i2)])
                sc = scbuf[:, kc * 512:kc * 512 + n]
                wcol = wi[:, qt, ih:ih + 1]
                if ih == 0:
                    P.add('dve', lambda e, sc=sc, rt=rt, n=n, wcol=wcol: e.tensor_scalar(out=sc, in0=rt[:, 0:n], scalar1=wcol, scalar2=None, op0=ALU.mult),
                          r=[('rl', i2), ('wi', qt)], w=[(skey, kc)])
                else:
                    P.add('dve', lambda e, sc=sc, rt=rt, n=n, wcol=wcol: e.scalar_tensor_tensor(out=sc, in0=rt[:, 0:n], scalar=wcol, in1=sc, op0=ALU.mult, op1=ALU.add),
                          r=[('rl', i2), ('wi', qt)], w=[(skey, kc)])
                step += 1
                if step % every == 0:
                    yield
        yield

    def score_steps(qt, every=2):
        L = (qt + 1) * 128
        return (8 * ((L + 511) // 512)) // every + 1

    def load_strips(h0):
        for j in range(4):
            load_const_bf16(stripb[:, j, :], strips_d[h0 + j], 'strip', [128, 640])

    for l in layers:
        lam_init = 0.8 - 0.6 * math.exp(-0.3 * l)
        rmsnorm_to_h(l, 0)
        if dbg and 'h' in dbg:
            pass
        for c in range(8):
            dma(xpark[:, c * S:(c + 1) * S], xT[:, c, :], r=[('x', c, q) for q in range(4)])
        P.do_fence()

        o = [A0]

        def aalloc(shape, dt, o=o):
            n = int(np.prod(shape[1:])) * (4 if dt == F32 else 2)
            n = (n + 31) // 32 * 32
            v = view(o[0], shape, dt)
            o[0] += n
            assert o[0] <= B0
            return v

        qT = aalloc([128, 2, S], BF16)
        kT = aalloc([128, 2, S], BF16)
        Vt = aalloc([128, 16, 256], BF16)
        Eb = [aalloc([128, 512], F32) for _ in range(2)]
        Ab = [aalloc([128, 512], BF16) for _ in range(2)]
        projT(l, 0, 256, lambda j, qc: [(qT[:, j, qs(qc)], [('q', j, qc)], slice(0, 128))], scale=0.125)
        projT(l, 256, 256, lambda j, qc: [(kT[:, j, qs(qc)], [('k', j, qc)], slice(0, 128))])
        projTok(l, 512, 256, lambda tt, ps: (Vt[:, tt, :], [('V', tt)], ps[:, 0:256]))
        x_qiT = aalloc([128, 4, S], BF16)
        x_kiT = aalloc([128, S], BF16)
        x_wi = aalloc([128, 16, 8], F32)
        x_rl = [aalloc([128, 512], F32) for _ in range(2)]
        o2 = [D0]

        def dalloc(shape, dt, o2=o2):
            n = int(np.prod(shape[1:])) * (4 if dt == F32 else 2)
            n = (n + 31) // 32 * 32
            v = view(o2[0], shape, dt)
            o2[0] += n
            assert o2[0] <= E0
            return v
        x_scb = [dalloc([128, S], F32) for _ in range(2)]
        x_junk = [dalloc([128, S], BF16) for _ in range(2)]
        NIT = 16
        mids = [dalloc([128, NIT + 2], F32) for _ in range(2)]
        hcols = [dalloc([128, NIT + 2], F32) for _ in range(2)]
        cntb = [dalloc([128, NIT + 2], F32) for _ in range(2)]
        rmm = [dalloc([128, 8], F32) for _ in range(2)]
        x_cmaskq = dalloc([128, 128], F32)
        dma(x_cmaskq, cmaskq_d, w=['cmaskq'])
        projT(l, 2304, 512, lambda j, qc: [(x_qiT[:, j, qs(qc)], [('qi', j, qc)], slice(0, 128))])
        projT(l, 2816, 64, lambda j, qc: [(x_kiT[0:64, qs(qc)], [('ki', qc)], slice(0, 64)), (x_kiT[64:128, qs(qc)], [('ki', qc)], slice(0, 64))])
        projTok(l, 2880, 8, lambda tt, ps: (x_wi[:, tt, :], [('wi', tt)], ps[:, 0:8]))

        def gen_index():
            itc = [0]
            P.add('dve', lambda e: e.memset(thrAll[:, 0:2], -1e29), w=['thrAll'])
            yield
            for pr in range(1, 8):
                tiles = [2 * pr, 2 * pr + 1]
                for s_, qt in enumerate(tiles):
                    L = (qt + 1) * 128
                    scbuf = x_scb[s_]
                    yield from score_tile_g(qt, scbuf, ('score', s_), x_qiT, x_kiT, x_wi, x_rl, itc)
                    allsc = [(('score', s_), kc) for kc in range(4)]
                    P.add('dve', lambda e, s_=s_, L=L, scbuf=scbuf: e.tensor_reduce(out=rmm[s_][:, 0:1], in_=scbuf[:, 0:L], axis=AX.X, op=ALU.min), r=allsc, w=[('rmm', s_)])
                    P.add('dve', lambda e, s_=s_, L=L, scbuf=scbuf: e.tensor_reduce(out=rmm[s_][:, 1:2], in_=scbuf[:, 0:L], axis=AX.X, op=ALU.max), r=allsc, w=[('rmm', s_)])
                    dsl = scbuf[:, qt * 128:(qt + 1) * 128]
                    P.add('dve', lambda e, dsl=dsl: e.tensor_tensor(out=dsl, in0=dsl, in1=x_cmaskq, op=ALU.add), r=['cmaskq'], w=allsc)
                for s_ in range(2):
                    P.add('dve', lambda e, s_=s_: e.tensor_tensor(out=rmm[s_][:, 2:3], in0=rmm[s_][:, 1:2], in1=rmm[s_][:, 0:1], op=ALU.subtract), w=[('rmm', s_)])
                    P.add('dve', lambda e, s_=s_: e.tensor_scalar(out=hcols[s_][:, 0:NIT], in0=pow2tab[:, 0:NIT], scalar1=rmm[s_][:, 2:3], scalar2=None, op0=ALU.mult),
                          r=[('rmm', s_), 'pow2tab'], w=[('hc', s_)])
                    P.add('dve', lambda e, s_=s_: e.tensor_tensor(out=mids[s_][:, 0:1], in0=rmm[s_][:, 0:1], in1=hcols[s_][:, 0:1], op=ALU.add),
                          r=[('rmm', s_), ('hc', s_)], w=[('mid', s_)])
                yield
                for i_ in range(NIT):
                    for s_, qt in enumerate(tiles):
                        L = (qt + 1) * 128
                        scr = [(('score', s_), kc) for kc in range(4)]
                        if s_ == 0:
                            P.add('dve', lambda e, s_=s_, L=L, i_=i_: e.tensor_scalar(out=x_junk[s_][:, 0:L], in0=x_scb[s_][:, 0:L], scalar1=mids[s_][:, i_:i_ + 1], scalar2=0.0,
                                                                                   op0=ALU.is_ge, op1=ALU.add, accum_out=cntb[s_][:, i_:i_ + 1]),
                                  r=scr + [('mid', s_)], w=[('junk', s_), ('cnt', s_)])
                        else:
                            P.add('pool', lambda e, s_=s_, i_=i_: e.tensor_scalar(out=rmm[s_][:, 4:5], in0=mids[s_][:, i_:i_ + 1], scalar1=-1.0, scalar2=None, op0=ALU.mult),
                                  r=[('mid', s_)], w=[('nmid', s_)])
                            P.add('act', lambda e, s_=s_, L=L, i_=i_: e.activation(out=x_junk[s_][:, 0:L], in_=x_scb[s_][:, 0:L], func=AF.Sign, bias=rmm[s_][:, 4:5], scale=1.0,
                                                                                accum_out=cntb[s_][:, i_:i_ + 1]),
                                  r=scr + [('nmid', s_)], w=[('junk', s_), ('cnt', s_)])
                    for s_, qt in enumerate(tiles):
                        L = (qt + 1) * 128
                        cth = 255.5 if s_ == 0 else (510.5 - L)
                        sub_ = 0.5 if i_ < NIT - 1 else 1.0
                        P.add('pool', lambda e, s_=s_, i_=i_, cth=cth, sub_=sub_: e.tensor_scalar(out=rmm[s_][:, 3:4], in0=cntb[s_][:, i_:i_ + 1], scalar1=cth, scalar2=sub_,
                                                                                       op0=ALU.is_ge, op1=ALU.subtract), r=[('cnt', s_)], w=[('tmpb', s_)])
                    for s_ in range(2):
                        P.add('pool', lambda e, s_=s_, i_=i_: e.tensor_scalar(out=mids[s_][:, i_ + 1:i_ + 2], in0=rmm[s_][:, 3:4], scalar1=hcols[s_][:, i_:i_ + 1], scalar2=mids[s_][:, i_:i_ + 1],
                                                                           op0=ALU.mult, op1=ALU.add),
                              r=[('tmpb', s_), ('hc', s_)], w=[('mid', s_)])
                    yield
                for s_, qt in enumerate(tiles):
                    P.add('dve', lambda e, s_=s_, qt=qt: e.tensor_copy(out=thrAll[:, qt:qt + 1], in_=mids[s_][:, NIT:NIT + 1]), r=[('mid', s_)], w=['thrAll'])
                yield

        def gen_sb():
            for h in range(4):
                ch, r0 = h // 2, (h % 2) * 64
                for qc in range(4):
                    for j_ in range(4):
                        P.add('dve', lambda e, j_=j_: e.tensor_scalar(out=Cb[:, j_ * 128:(j_ + 1) * 128], in0=onesF, scalar1=0.0, scalar2=None, op0=ALU.mult), r=['onesF'], w=['C'])
                    last = 4 * qc + 3
                    accb = nacc()
                    order = list(range(last, -1, -1))
                    n_ = len(order)
                    inf = {}

                    def geo(k, qc=qc, ch=ch, r0=r0):
                        kb = order[k]
                        ks = kb * 128
                        c0 = max(0, ks - 512 * qc)
                        diag = kb >= 4 * qc
                        lk = kT[r0:r0 + 64, ch, ks:ks + 128]
                        rq = qT[r0:r0 + 64, ch, qs(qc, c0)]
                        rk = [('k', ch, kb // 4), ('q', ch, qc)]
                        return kb, c0, diag, lk, rq, rk

                    def sA1(k):
                        kb, c0, diag, lk, rq, rk = geo(k)
                        b1 = nb()
                        inf[k] = b1
                        mm(psb[b1][:, c0:512], lk, rq, True, not diag, r=rk, w=[PS(b1)], sgc=True)
                        if diag:
                            mm(psb[b1][:, c0:c0 + 128], identb, sbmaskb, False, True, r=['identb', 'sbmaskb'], w=[PS(b1)], sgc=True)

                    def sB1(k):
                        kb, c0, diag, lk, rq, rk = geo(k)
                        b1 = inf[k]
                        i2 = k % 2
                        E, SPt = Eb[i2], SPb[i2]
                        P.add('act', lambda e, E=E, b1=b1, c0=c0: e.activation(out=E[:, c0:512], in_=psb[b1][:, c0:512], func=AF.Exp),
                              w=[PS(b1), ('E', i2)])
                        P.add('act', lambda e, E=E, SPt=SPt, c0=c0: e.activation(out=SPt[:, c0:512], in_=E[:, c0:512], func=AF.Ln, bias=1.0, scale=1.0),
                              r=[('E', i2)], w=[('SP', i2)])

                    def sA2(k):
                        kb, c0, diag, lk, rq, rk = geo(k)
                        i2 = k % 2
                        SPt = SPb[i2]
                        b2 = nb()
                        inf[('b2', k)] = b2
                        mm(psb[b2][:, c0:512], lk, rq, True, False, r=rk, w=[PS(b2)], sgc=True)
                        if diag:
                            mm(psb[b2][:, c0:c0 + 128], identb, sbmaskb, False, False, r=['identb', 'sbmaskb'], w=[PS(b2)], sgc=True)
                        mm(psb[b2][:, c0:512], negTr, SPt[:, c0:512], False, k == 0, r=[('SP', i2), 'negTr'], w=[PS(b2)], sgc=True)
                        if k != 0:
                            mm(psb[b2][:, c0:512], negOnesr, Cb[:, c0:512], False, True, r=['C', 'negOnesr'], w=[PS(b2)], sgc=True)

                    def sB2(k):
                        kb, c0, diag, lk, rq, rk = geo(k)
                        i2 = k % 2
                        At = Ab[i2]
                        b2 = inf[('b2', k)]
                        P.add('act', lambda e, At=At, b2=b2, c0=c0: e.activation(out=At[:, c0:512], in_=psb[b2][:, c0:512], func=AF.Exp),
                              w=[PS(b2), ('A', i2)])

                    def sC(k):
                        kb, c0, diag, lk, rq, rk = geo(k)
                        i2 = k % 2
                        SPt = SPb[i2]
                        if k != n_ - 1:
                            P.add('dve', lambda e, SPt=SPt, c0=c0: e.tensor_tensor(out=Cb[:, c0:512], in0=Cb[:, c0:512].bitcast(F32), in1=SPt[:, c0:512].bitcast(F32), op=ALU.add),
                                  r=[('SP', i2)], w=['C'])

                    def sA3(k, h=h):
                        kb, c0, diag, lk, rq, rk = geo(k)
                        i2 = k % 2
                        At = Ab[i2]
                        mm(psb[accb][0:64, c0:512], Vt[:, kb, h * 64:(h + 1) * 64], At[:, c0:512], k == 0, k == n_ - 1,
                           r=[('A', i2), ('V', kb)], w=[PS(accb)], sgc=True)

                    sA1(0)
                    sB1(0)
                    for k in range(n_):
                        if k + 1 < n_:
                            sA1(k + 1)
                            sB1(k + 1)
                        sA2(k)
                        sB2(k)
                        sC(k)
                        if k >= 1:
                            sA3(k - 1)
                        flush_one()
                        yield
                    sA3(n_ - 1)
                    reg_fin(accb, lambda r0=r0, ch=ch, qc=qc, accb=accb: evac(oT[r0:r0 + 64, ch, qs(qc)], psb[accb][0:64, :], [('o', ch, qc)], accb))

        n_index = sum(score_steps(2 * pr) + score_steps(2 * pr + 1) + NIT + 2 for pr in range(1, 8)) + 1
        interleave([(gen_sb(), 160), (gen_index(), n_index)])
        flush_pending()
        P.do_fence()
        if dbg and 'stop_sb' in dbg:
            break

        o[0] = A0
        q1T = aalloc([128, S], BF16)
        q2T = aalloc([128, S], BF16)
        k1T = aalloc([128, S], BF16)
        k2T = aalloc([128, S], BF16)
        Vaug = aalloc([128, 16, 4, 66], BF16)
        qmb = [aalloc([128, 512], BF16) for _ in range(2)]
        Pb = [aalloc([128, 512], BF16) for _ in range(3)]
        rrec = aalloc([128, 512], F32)
        bcs = aalloc([128, 512], F32)
        t1 = aalloc([128, 512], F32)
        t2 = aalloc([128, 512], F32)
        od = aalloc([128, 512], F32)
        sq64 = aalloc([128, 512], F32)
        rs64 = aalloc([128, 512], F32)
        ln64 = aalloc([128, 512], F32)
        lamv = aalloc([128, 4 * 32], F32)
        smallf = aalloc([128, 64], F32)
        load_strips(0)
        dma(lamv, lamv_d[:, l * 128:(l + 1) * 128], w=['lamv'])
        P.add('dve', lambda e: e.tensor_tensor(out=smallf[:, 0:32], in0=lamv[:, 0:32], in1=lamv[:, 32:64], op=ALU.mult), r=['lamv'], w=['sm0'])
        P.add('dve', lambda e: e.reduce_sum(out=smallf[:, 32:33], in_=smallf[:, 0:32], axis=AX.X), r=['sm0'], w=['sm1'])
        P.add('dve', lambda e: e.tensor_tensor(out=smallf[:, 0:32], in0=lamv[:, 64:96], in1=lamv[:, 96:128], op=ALU.mult), r=['lamv', 'sm1'], w=['sm0'])
        P.add('dve', lambda e: e.reduce_sum(out=smallf[:, 33:34], in_=smallf[:, 0:32], axis=AX.X), r=['sm0'], w=['sm1'])
        P.add('act', lambda e: e.activation(out=smallf[:, 34:36], in_=smallf[:, 32:34], func=AF.Exp), r=['sm1'], w=['sm2'])
        P.add('dve', lambda e: e.tensor_tensor(out=smallf[:, 36:37], in0=smallf[:, 35:36], in1=smallf[:, 34:35], op=ALU.subtract), r=['sm2'], w=['sm3'])
        P.add('dve', lambda e, lam_init=lam_init: e.tensor_scalar(out=smallf[:, 37:38], in0=smallf[:, 36:37], scalar1=-lam_init, scalar2=None, op0=ALU.add), r=['sm3'], w=['neglam'])
        neglam = smallf[:, 37:38]
        sc32 = 32 ** -0.5
        projT(l, 768, 128, lambda j, qc: [(q1T[:, qs(qc)], [('q1', qc)], slice(0, 128))], scale=sc32)
        projT(l, 896, 128, lambda j, qc: [(q2T[:, qs(qc)], [('q2', qc)], slice(0, 128))], scale=sc32)
        projT(l, 1024, 128, lambda j, qc: [(k1T[:, qs(qc)], [('k1', qc)], slice(0, 128))])
        projT(l, 1152, 128, lambda j, qc: [(k2T[:, qs(qc)], [('k2', qc)], slice(0, 128))])
        P.add('dve', lambda e: e.memset(Vaug[:, :, :, 64:66], 1.0), w=[('V', t) for t in range(16)])
        projTok(l, 1280, 256, lambda tt, ps: (Vaug[:, tt, :, 0:64], [('V', tt)], ps[:, 0:256].rearrange("p (h d) -> p h d", h=4)))
        lnc = math.log(1.0 - lam_init)
        for h in range(4):
            och, r0 = 2 + h // 2, (h % 2) * 64
            for qc in range(4):
                accs = []
                for which, (qq, kk, qn, kn) in enumerate([(q1T, k1T, 'q1', 'k1'), (q2T, k2T, 'q2', 'k2')]):
                    qm = qmb[which]
                    P.add('dve', lambda e, qm=qm, qq=qq, qc=qc, h=h: e.tensor_scalar(out=qm, in0=qq[:, qs(qc)], scalar1=headmask[:, h:h + 1], scalar2=None, op0=ALU.mult),
                          r=[(qn, qc), 'headmask'], w=[('qm', which)])
                    accb = attn_soft_qc(qc, lambda kb, kk=kk: kk[:, kb * 128:(kb + 1) * 128], lambda qc_, c0, qm=qm: qm[:, c0:512],
                                        [(kn, q) for q in range(4)], [('qm', which)], stripb[:, h, :], c31[:, h:h + 1],
                                        lambda kb, h=h: Vaug[:, kb, h, 0:65], None, Pb, vkeyfn=lambda kb: ('V', kb))
                    for st_ in soft_norm_stages(accb, rrec, bcs):
                        reg_fin(accb, st_)

                    def fin_p3(accb=accb, which=which, bcs=bcs):
                        tt_ = t1 if which == 0 else t2
                        P.add('dve', lambda e, tt_=tt_, accb=accb, bcs=bcs: e.tensor_tensor(out=tt_[0:64, :], in0=psb[accb][0:64, :], in1=bcs[0:64, :], op=ALU.mult),
                              r=['bcs'], w=[PS(accb), ('t', which)])
                    reg_fin(accb, fin_p3)

                def fin_u1():
                    P.add('dve', lambda e: e.scalar_tensor_tensor(out=od[0:64, :], in0=t2[0:64, :], scalar=neglam[0:64, :], in1=t1[0:64, :], op0=ALU.mult, op1=ALU.add),
                          r=[('t', 0), ('t', 1), 'neglam'], w=['od'])
                    P.add('act', lambda e: e.activation(out=sq64[0:64, :], in_=od[0:64, :], func=AF.Square), r=['od'], w=['sq64'])

                def fin_u2():
                    bb = nb()
                    mm(psb[bb][0:64, :], onesF[0:64, 0:64], sq64[0:64, :], True, True, r=['sq64', 'onesF'], w=[PS(bb)])
                    P.add('act', lambda e, bb=bb: e.activation(out=ln64[0:64, :], in_=psb[bb][0:64, :], func=AF.Ln, scale=1.0 / 64, bias=EPS), w=[PS(bb), 'ln64'])

                def fin_u3(och=och, r0=r0, qc=qc, l=l, lnc=lnc):
                    P.add('act', lambda e, lnc=lnc: e.activation(out=rs64[0:64, :], in_=ln64[0:64, :], func=AF.Exp, scale=-0.5, bias=lnc), r=['ln64'], w=['rs64'])
                    P.add('dve', lambda e, och=och, r0=r0, qc=qc, l=l: e.scalar_tensor_tensor(out=oT[r0:r0 + 64, och, qs(qc)], in0=od[0:64, :], scalar=subg[0:64, l:l + 1], in1=rs64[0:64, :], op0=ALU.mult, op1=ALU.mult),
                          r=['od', 'rs64', 'subg'], w=[('o', och, qc)])
                pending.extend([fin_u1, fin_u2, fin_u3])
        flush_pending()
        P.do_fence()

        o[0] = A0
        o2[0] = D0
        dq = aalloc([128, 2, S], BF16)
        dk_ = aalloc([128, 2, S], BF16)
        Vaug = aalloc([128, 16, 4, 66], BF16)
        kiT = aalloc([128, S], BF16)
        qiT = aalloc([128, 4, S], BF16)
        score = aalloc([128, S], F32)
        wi = aalloc([128, 16, 8], F32)
        rl = [aalloc([128, 512], F32) for _ in range(2)]
        cmaskq = aalloc([128, 128], F32)
        dma(cmaskq, cmaskq_d, w=['cmaskq'])
        Pb = [aalloc([128, 512], BF16) for _ in range(3)]
        bcs = aalloc([128, 512], F32)
        rrec = bcs
        maskTs = [dalloc([128, 12, 512], BF16), dalloc([128, 16, 512], BF16)]
        nmb = dalloc([128, S], BF16)
        load_strips(4)
        projT(l, 1536, 256, lambda j, qc: [(dq[:, j, qs(qc)], [('q', j, qc)], slice(0, 128))], scale=0.125)
        projT(l, 1792, 256, lambda j, qc: [(dk_[:, j, qs(qc)], [('k', j, qc)], slice(0, 128))])
        P.add('dve', lambda e, Vaug=Vaug: e.memset(Vaug[:, :, :, 64:66], 1.0), w=[('V', t) for t in range(16)])
        projTok(l, 2048, 256, lambda tt, ps: (Vaug[:, tt, :, 0:64], [('V', tt)], ps[:, 0:256].rearrange("p (h d) -> p h d", h=4)))
        projT(l, 2304, 512, lambda j, qc: [(qiT[:, j, qs(qc)], [('qi', j, qc)], slice(0, 128))])
        projT(l, 2816, 64, lambda j, qc: [(kiT[0:64, qs(qc)], [('ki', qc)], slice(0, 64)), (kiT[64:128, qs(qc)], [('ki', qc)], slice(0, 64))])
        projTok(l, 2880, 8, lambda tt, ps: (wi[:, tt, :], [('wi', tt)], ps[:, 0:8]))
        itc2 = [0]

        def gen_mask(qc):
            mT_ = maskTs[qc % 2]
            mkey = ('maskT', qc % 2)
            for qt in range(4 * qc, 4 * qc + 4):
                L = (qt + 1) * 128
                yield from score_tile_g(qt, score, ('score', 0), qiT, kiT, wi, rl, itc2)
                allsc = [(('score', 0), kc) for kc in range(4)]
                dsl = score[:, qt * 128:(qt + 1) * 128]
                P.add('dve', lambda e, dsl=dsl: e.tensor_tensor(out=dsl, in0=dsl, in1=cmaskq, op=ALU.add), r=['cmaskq'], w=allsc)
                P.add('dve', lambda e, L=L, qt=qt: e.tensor_scalar(out=nmb[:, 0:L], in0=score[:, 0:L], scalar1=thrAll[:, qt:qt + 1], scalar2=NEG, op0=ALU.is_lt, op1=ALU.mult),
                      r=allsc + ['thrAll'], w=['nm'])
                for kb0 in range(0, qt + 1, 4):
                    nkb = min(4, qt + 1 - kb0)
                    for j in range(nkb):
                        P.add('pe', lambda e, j=j, kb0=kb0: e.transpose(out=psT[:, j * 128:(j + 1) * 128], in_=nmb[:, (kb0 + j) * 128:(kb0 + j + 1) * 128], identity=identb),
                              r=['nm', 'identb'], w=['psT'])
                    qo = (qt % 4) * 128
                    evac(mT_[:, kb0:kb0 + nkb, qo:qo + 128], psT[:, 0:nkb * 128].rearrange("p (a b) -> p a b", a=nkb), [mkey], 'T')
                    yield

        def mask_steps(qc):
            return sum(score_steps(qt) + (qt + 4) // 4 for qt in range(4 * qc, 4 * qc + 4))

        def gen_attn(qc):
            mT_ = maskTs[qc % 2]
            mkey = ('maskT', qc % 2)
            for h in range(4):
                och, r0 = 4 + h // 2, (h % 2) * 64
                ch = h // 2
                holder = []
                yield from attn_soft_qc_g(qc, lambda kb, ch=ch, r0=r0: dk_[r0:r0 + 64, ch, kb * 128:(kb + 1) * 128],
                                          lambda qc_, c0, ch=ch, r0=r0: dq[r0:r0 + 64, ch, qs(qc_, c0)],
                                          [('k', ch, q) for q in range(4)], [('q', ch, qc)], stripb[:, h, :], c31[:, 4 + h:5 + h],
                                          lambda kb, h=h: Vaug[:, kb, h, 0:65], None, Pb, vkeyfn=lambda kb: ('V', kb),
                                          extra_fn=lambda b1, qc_, kb, c0, mT_=mT_, mkey=mkey: [(psb[b1][:, c0:512], identb, mT_[:, kb, c0:512], ['identb', mkey])],
                                          holder=holder)
                accb = holder[0]

                for st_ in soft_norm_stages(accb, rrec, bcs):
                    reg_fin(accb, st_)

                def fin_sm(accb=accb, och=och, r0=r0, qc=qc, bcs=bcs):
                    P.add('dve', lambda e, accb=accb, och=och, r0=r0, qc=qc, bcs=bcs: e.tensor_tensor(out=oT[r0:r0 + 64, och, qs(qc)], in0=psb[accb][0:64, :], in1=bcs[0:64, :], op=ALU.mult),
                          r=['bcs'], w=[PS(accb), ('o', och, qc)])
                reg_fin(accb, fin_sm)

        run_gen(gen_mask(0))
        for qc in range(4):
            items = [(gen_attn(qc), 4 * (4 * qc + 4))]
            if qc < 3:
                items.append((gen_mask(qc + 1), mask_steps(qc + 1)))
            interleave(items)
        flush_pending()
        P.do_fence()

        o[0] = A0
        o2[0] = D0
        mq = aalloc([128, 2, S], BF16)
        mk_ = aalloc([128, 2, S], BF16)
        Vaug = aalloc([128, 16, 4, 66], BF16)
        ksf = aalloc([128, 2, 8], F32)
        kshi = aalloc([128, 2, 8], BF16)
        kslo = aalloc([128, 2, 8], BF16)
        gm = aalloc([128, 32], F32)
        t8 = aalloc([128, 4, 8], F32)
        thr4 = aalloc([128, 4], F32)
        negm = aalloc([128, 32], BF16)
        negmT = aalloc([32, S], BF16)
        selb = dalloc([32, 32, 128], BF16)
        pastmask = dalloc([128, 8 * 32], F32)
        dma(pastmask, pastmask_d, w=['pastmask'])
        Pb = [aalloc([128, 512], BF16) for _ in range(3)]
        rrec = aalloc([128, 512], F32)
        bcs = aalloc([128, 512], F32)
        load_strips(8)
        load_const_bf16(selb[:, 0:16, :], sel_d[:, 0:2048].rearrange("p (a b) -> p a b", a=16), 'selb', [32, 16, 128])
        load_const_bf16(selb[:, 16:32, :], sel_d[:, 2048:4096].rearrange("p (a b) -> p a b", a=16), 'selb', [32, 16, 128])
        projT(l, 2888, 256, lambda j, qc: [(mq[:, j, qs(qc)], [('q', j, qc)], slice(0, 128))], scale=0.125)
        projT(l, 3144, 256, lambda j, qc: [(mk_[:, j, qs(qc)], [('k', j, qc)], slice(0, 128))])
        P.add('dve', lambda e: e.memset(Vaug[:, :, :, 64:66], 1.0), w=[('V', t) for t in range(16)])
        projTok(l, 3400, 256, lambda tt, ps: (Vaug[:, tt, :, 0:64], [('V', tt)], ps[:, 0:256].rearrange("p (h d) -> p h d", h=4)))
        for ch in range(2):
            P.add('dve', lambda e, ch=ch: e.reduce_sum(out=ksf[:, ch, :], in_=mk_[:, ch, :].rearrange("p (n k) -> p n k", n=8), axis=AX.X),
                  r=[('k', ch, q) for q in range(4)], w=['ksf'])
        P.add('dve', lambda e: e.tensor_copy(out=kshi, in_=ksf), r=['ksf'], w=['kshi'])
        P.add('dve', lambda e: e.tensor_tensor(out=kslo, in0=ksf, in1=kshi, op=ALU.subtract), r=['ksf', 'kshi'], w=['kslo'])
        def gen_gates(gq):
            for qt in range(4 * gq, 4 * gq + 4):
                own = qt // 2
                bpar = [nb(), nb()]
                for h in range(4):
                    ch, r0 = h // 2, (h % 2) * 64
                    b = bpar[h % 2]
                    lq = mq[r0:r0 + 64, ch, qt * 128:(qt + 1) * 128]
                    mm(psb[b][:, h * 8:(h + 1) * 8], lq, kshi[r0:r0 + 64, ch, :], True, False, r=[('q', ch, qt // 4), 'kshi'], w=[PS(b)], sgc=True)
                    mm(psb[b][:, h * 8:(h + 1) * 8], lq, kslo[r0:r0 + 64, ch, :], False, True, r=[('q', ch, qt // 4), 'kslo'], w=[PS(b)], sgc=True)
                for h in range(4):
                    b = bpar[h % 2]
                    P.add('dve', lambda e, b=b, own=own, h=h: e.tensor_tensor(out=gm[:, h * 8:(h + 1) * 8], in0=psb[b][:, h * 8:(h + 1) * 8], in1=pastmask[:, own * 32 + h * 8:own * 32 + (h + 1) * 8], op=ALU.add),
                          r=['pastmask'], w=[PS(b), 'gm'])
                for h in range(4):
                    P.add('dve', lambda e, h=h: e.max(out=t8[:, h, :], in_=gm[:, h * 8:(h + 1) * 8]), r=['gm'], w=['t8'])
                P.add('dve', lambda e: e.tensor_scalar(out=thr4, in0=t8[:, :, 2], scalar1=-1e29, scalar2=None, op0=ALU.max), r=['t8'], w=['thr4'])
                for h in range(4):
                    P.add('dve', lambda e, h=h: e.tensor_scalar(out=negm[:, h * 8:(h + 1) * 8], in0=gm[:, h * 8:(h + 1) * 8], scalar1=thr4[:, h:h + 1], scalar2=NEG, op0=ALU.is_lt, op1=ALU.mult),
                          r=['gm', 'thr4'], w=['negm'])
                P.add('pe', lambda e: e.transpose(out=psT[0:32, 0:128], in_=negm, identity=identb), r=['negm', 'identb'], w=['psT'])
                evac(negmT[0:32, qt * 128:(qt + 1) * 128], psT[0:32, 0:128], [('negmT', qt // 4)], 'T')
                yield
        def gen_attn_mb(qc):
            for h in range(4):
                och, r0 = 6 + h // 2, (h % 2) * 64
                ch = h // 2

                def mb_extra(b1, qc_, kb, c0, h=h):
                    nbk = kb // 2
                    if nbk < 2 * qc_:
                        return [(psb[b1][:, c0:512], selb[0:32, h * 8 + nbk, :], negmT[0:32, qs(qc_, c0)], ['selb', ('negmT', qc_)])]
                    if nbk == 2 * qc_:
                        return [(psb[b1][:, 256:512], selb[0:32, h * 8 + nbk, :], negmT[0:32, qs(qc_, 256)], ['selb', ('negmT', qc_)])]
                    return []
                holder = []
                yield from attn_soft_qc_g(qc, lambda kb, ch=ch, r0=r0: mk_[r0:r0 + 64, ch, kb * 128:(kb + 1) * 128],
                                          lambda qc_, c0, ch=ch, r0=r0: mq[r0:r0 + 64, ch, qs(qc_, c0)],
                                          [('k', ch, q) for q in range(4)], [('q', ch, qc)], stripb[:, h, :], c31[:, 8 + h:9 + h],
                                          lambda kb, h=h: Vaug[:, kb, h, 0:65], None, Pb, vkeyfn=lambda kb: ('V', kb), extra_fn=mb_extra, holder=holder)
                accb = holder[0]
                for st_ in soft_norm_stages(accb, rrec, bcs):
                    reg_fin(accb, st_)

                def fin_sm(accb=accb, och=och, r0=r0, qc=qc, bcs=bcs):
                    P.add('dve', lambda e, accb=accb, och=och, r0=r0, qc=qc, bcs=bcs: e.tensor_tensor(out=oT[r0:r0 + 64, och, qs(qc)], in0=psb[accb][0:64, :], in1=bcs[0:64, :], op=ALU.mult),
                              r=['bcs'], w=[PS(accb), ('o', och, qc)])
                reg_fin(accb, fin_sm)

        run_gen(gen_gates(0))
        for qc in range(4):
            items = [(gen_attn_mb(qc), 4 * (4 * qc + 4))]
            if qc < 3:
                items.append((gen_gates(qc + 1), 4))
            interleave(items)
        flush_pending()
        P.do_fence()
        if dbg and 'stop_br' in dbg:
            break

        o[0] = A0
        macc = aalloc([128, 2, 4, 512], F32)
        sgb = [aalloc([128, 512], F32) for _ in range(2)]
        tb = [aalloc([128, 512], F32) for _ in range(2)]
        assert o[0] <= A0 + 3 * 8192
        for c in range(3, 8):
            dma(xT[:, c, :], xpark[:, c * S:(c + 1) * S], w=[('x', c, q) for q in range(4)])
        kk_ = 0
        for jp in range(4):
            for i in range(4):
                wg, wgk = load_w(w_in[l, :, 3656 + i * 1024 + jp * 256: 3656 + i * 1024 + (jp + 1) * 256], 8, 256)
                wb, wbk = load_w(w_br[l, i, :, jp * 256:(jp + 1) * 256], 2, 256)
                for jj in range(2):
                    j = jp * 2 + jj
                    for qc in range(4):
                        bg = nb()
                        for c in range(8):
                            mm(psb[bg][:, :], wg[:, c, jj * 128:(jj + 1) * 128], hT[:, c, qs(qc)], c == 0, c == 7, r=[wgk, ('h', c, qc)], w=[PS(bg)], nofence=True)
                        by = nb()
                        for c2 in range(2):
                            mm(psb[by][:, :], wb[:, c2, jj * 128:(jj + 1) * 128], oT[:, 2 * i + c2, qs(qc)], c2 == 0, c2 == 1, r=[wbk, ('o', 2 * i + c2, qc)], w=[PS(by)])
                        k2 = kk_ % 2
                        kk_ += 1
                        sg, tt_ = sgb[k2], tb[k2]
                        P.add('act', lambda e, sg=sg, bg=bg: e.activation(out=sg, in_=psb[bg][:, :], func=AF.Sigmoid), w=[PS(bg), ('sg', k2)])
                        mslc = macc[:, jj, qc, :]
                        if i == 0:
                            P.add('dve', lambda e, sg=sg, by=by, mslc=mslc: e.tensor_tensor(out=mslc, in0=sg, in1=psb[by][:, :], op=ALU.mult),
                                  r=[('sg', k2)], w=[PS(by), ('macc', jj, qc)])
                        else:
                            P.add('dve', lambda e, sg=sg, by=by, tt_=tt_: e.tensor_tensor(out=tt_, in0=sg, in1=psb[by][:, :], op=ALU.mult),
                                  r=[('sg', k2)], w=[PS(by), ('mt', k2)])
                            if i < 3:
                                P.add('pool' if kk_ % 3 == 0 else 'dve', lambda e, mslc=mslc, tt_=tt_: e.tensor_tensor(out=mslc, in0=mslc, in1=tt_, op=ALU.add),
                                      r=[('mt', k2)], w=[('macc', jj, qc)])
                            else:
                                dst = mT[:, j, qs(qc)]
                                P.add('pool' if kk_ % 3 == 0 else 'dve', lambda e, mslc=mslc, tt_=tt_, dst=dst: e.tensor_tensor(out=dst, in0=mslc, in1=tt_, op=ALU.add),
                                      r=[('mt', k2), ('macc', jj, qc)], w=[('m', j, qc)])
        P.do_fence()

        for c in range(0, 3):
            dma(xT[:, c, :], xpark[:, c * S:(c + 1) * S], w=[('x', c, q) for q in range(4)])
        wout_bf = view(C0, [128, 8, 1024], BF16)
        ptmp = [view(C0 + 16384 + 2048 * i_, [128, 512], F32) for i_ in range(2)]
        yTb = [view(B0 + 16384 * i_, [128, 8, 512], F32) for i_ in range(2)]
        for t in range(4):
            s_ = cnt['wst'] % 2
            cnt['wst'] += 1
            stg = wst[s_]
            dma(stg, w_out[l, :, t * 256:(t + 1) * 256].rearrange("(c p) n -> p c n", p=128), w=[('wst', s_)])
            dstw = wout_bf[:, :, t * 256:(t + 1) * 256]
            P.add('act', lambda e, dstw=dstw, stg=stg: e.copy(out=dstw, in_=stg), r=[('wst', s_)], w=[('wout', t)])

        def _kl(k):
            return list(k) if isinstance(k, list) else [k]

        def post_norm_residual(gt, y, ykey, qc, l=l):
            bs = nb()
            for j in range(8):
                sq = sqb[j % 2]
                ys = y[:, j, :]
                P.add('act', lambda e, sq=sq, ys=ys: e.activation(out=sq, in_=ys, func=AF.Square), r=_kl(ykey(j)), w=[('sq', j % 2)])
                mm(psb[bs][:, :], onesF, sq, j == 0, j == 7, r=[('sq', j % 2), 'onesF'], w=[PS(bs)])
            P.add('act', lambda e, bs=bs: e.activation(out=lnt, in_=psb[bs][:, :], func=AF.Ln, scale=1.0 / D, bias=EPS), w=[PS(bs), ('sq', 0)])
            P.add('act', lambda e: e.activation(out=rstd, in_=lnt, func=AF.Exp, scale=-0.5), r=[('sq', 0)], w=['rstd'])
            for j in range(8):
                pt = ptmp[j % 2]
                ys = y[:, j, :]
                g = gcol(gt, l, j)
                xs = xT[:, j, qs(qc)]
                P.add('dve', lambda e, pt=pt, ys=ys, g=g: e.scalar_tensor_tensor(out=pt, in0=ys, scalar=g, in1=rstd, op0=ALU.mult, op1=ALU.mult),
                      r=_kl(ykey(j)) + ['rstd', 'gains'], w=[('pt', j % 2)])
                P.add('pool' if j % 4 == 3 else 'dve', lambda e, pt=pt, xs=xs: e.tensor_tensor(out=xs, in0=xs, in1=pt, op=ALU.add), r=[('pt', j % 2)], w=[('x', j, qc)])

        for qc in range(4):
            y = yTb[qc % 2]
            for j in range(8):
                b = nb()
                for c in range(8):
                    mm(psb[b][:, :], wout_bf[:, c, j * 128:(j + 1) * 128], mT[:, c, qs(qc)], c == 0, c == 7, r=[('wout', j // 2), ('m', c, qc)], w=[PS(b)])
                evac(y[:, j, :], psb[b][:, :], [('y', qc % 2, j)], b)
            post_norm_residual(1, y, lambda j, qc=qc: ('y', qc % 2, j), qc)
        P.do_fence()
        if dbg and 'stop_mix' in dbg:
            break

        rmsnorm_to_h(l, 2)
        uT = view(C0, [128, 22, 1024], BF16)
        yF = view(C0 + 45056, [128, 8, 512], F32)
        ptmp = [view(C0 + 61440 + 2048 * i_, [128, 512], F32) for i_ in range(2)]
        kk_ = 0
        for th in range(2):
            for fp in range(11):
                wg, wgk = load_w(w_f1[l, :, fp * 256:(fp + 1) * 256], 8, 256)
                wu, wuk = load_w(w_f1[l, :, DFF + fp * 256: DFF + (fp + 1) * 256], 8, 256)
                for ff in range(2):
                    f = fp * 2 + ff
                    for q2 in range(2):
                        qc = th * 2 + q2
                        bg = nb()
                        for c in range(8):
                            mm(psb[bg][:, :], wg[:, c, ff * 128:(ff + 1) * 128], hT[:, c, qs(qc)], c == 0, c == 7, r=[wgk, ('h', c, qc)], w=[PS(bg)], nofence=True)
                        bu = nb()
                        for c in range(8):
                            mm(psb[bu][:, :], wu[:, c, ff * 128:(ff + 1) * 128], hT[:, c, qs(qc)], c == 0, c == 7, r=[wuk, ('h', c, qc)], w=[PS(bu)], nofence=True)
                        k2 = kk_ % 2
                        kk_ += 1
                        sg = sqb[k2]
                        P.add('act', lambda e, sg=sg, bg=bg: e.activation(out=sg, in_=psb[bg][:, :], func=AF.Silu), w=[PS(bg), ('sq', k2)])
                        us = uT[:, f, q2 * 512:(q2 + 1) * 512]
                        P.add('dve', lambda e, sg=sg, bu=bu, us=us: e.tensor_tensor(out=us, in0=sg, in1=psb[bu][:, :], op=ALU.mult),
                              r=[('sq', k2)], w=[PS(bu), ('u', f, q2)])
            yFs = [yF, view(B0, [128, 8, 1024], F32)[:, :, th * 512:(th + 1) * 512]]

            def yfk(q2, j, th=th):
                if q2 == 0:
                    return [('yf', 0, j)]
                return [('yf', 1, j), ('h', j, 2 * th), ('h', j, 2 * th + 1)]
            for jp in range(4):
                banks = [[nb(), nb()], [nb(), nb()]]
                for kg in range(3):
                    nk = 8 if kg < 2 else 6
                    w2, w2k = load_w(w_f2[l, kg * 1024: kg * 1024 + nk * 128, jp * 256:(jp + 1) * 256], nk, 256, eng='dve')
                    for q2 in range(2):
                        for jj in range(2):
                            for fk in range(nk):
                                f = kg * 8 + fk
                                mm(psb[banks[q2][jj]][:, :], w2[:, fk, jj * 128:(jj + 1) * 128], uT[:, f, q2 * 512:(q2 + 1) * 512], f == 0, f == 21,
                                   r=[w2k, ('u', f, q2)], w=[PS(banks[q2][jj])], sgc=True)
                for q2 in range(2):
                    for jj in range(2):
                        evac(yFs[q2][:, jp * 2 + jj, :], psb[banks[q2][jj]][:, :], yfk(q2, jp * 2 + jj), banks[q2][jj])
            for q2 in range(2):
                post_norm_residual(3, yFs[q2], lambda j, q2=q2: yfk(q2, j), th * 2 + q2)
        P.do_fence()
    if dbg and 'thr' in dbg:
        dma(dbg_out['thr'][0:128, 0:16], thrAll, r=['thrAll'])
    if dbg and 'o' in dbg:
        tmp = view(A0, [128, 8, S], F32)
        for c in range(8):
            P.add('dve', lambda e, c=c: e.tensor_copy(out=tmp[:, c, :], in_=oT[:, c, :]), r=[('o', c, q) for q in range(4)], w=[('tmp', c)])
            dma(dbg_out['o'][c * 128:(c + 1) * 128, :], tmp[:, c, :], r=[('tmp', c)])
    elif dbg and 'x' in dbg:
        for c in range(8):
            dma(dbg_out['x'][c * 128:(c + 1) * 128, :], xT[:, c, :], r=[('x', c, q) for q in range(4)])
    else:
        for c in range(8):
            dma(outT_d[c * 128:(c + 1) * 128, :], xT[:, c, :], r=[('x', c, q) for q in range(4)])
    P.emit(st)
    st.close()
    return nc, P


def host_consts(inputs):
    rb = np.asarray(inputs['rel_bias'], np.float32)
    c = {}
    g = np.stack([np.asarray(inputs[k], np.float32) for k in ['g_pre_mix', 'g_post_mix', 'g_pre_ffn', 'g_post_ffn']])
    g = g.reshape(4, DEPTH, 8, 128).transpose(3, 0, 1, 2).reshape(128, 4 * DEPTH * 8)
    c['gains'] = np.ascontiguousarray(g)
    kl = np.arange(128)[:, None]
    r = np.arange(640)[None, :]
    dist = r - kl
    bucket = _t5_bucket_np(dist)
    strips = rb[bucket]
    strips = np.where((dist < 0)[:, :, None], np.float32(NEG), strips)
    c['strips'] = np.ascontiguousarray(strips.transpose(2, 0, 1).astype(np.float32))
    c['c31'] = np.ascontiguousarray(np.broadcast_to(rb[31][None, :], (128, 12)).astype(np.float32))
    lv = np.stack([np.asarray(inputs[k], np.float32) for k in ['lambda_q1', 'lambda_k1', 'lambda_q2', 'lambda_k2']], axis=1)
    c['lamv'] = np.ascontiguousarray(np.broadcast_to(lv.reshape(1, -1), (128, DEPTH * 4 * 32)).astype(np.float32))
    sg = np.asarray(inputs['diff_subln_g'], np.float32)
    c['subg'] = np.ascontiguousarray(np.concatenate([sg.T, sg.T], axis=0))
    c['ident'] = np.eye(128, dtype=np.float32)
    j = np.arange(128)[:, None]
    k = np.arange(128)[None, :]
    c['negT'] = np.where(j >= k, -1.0, 0.0).astype(np.float32)
    c['sbmask'] = np.where(k <= j, NEG, 0.0).astype(np.float32)
    c['cmaskq'] = np.where(k > j, -1e30, 0.0).astype(np.float32)
    pm = np.zeros((128, 8, 4, 8), np.float32)
    for own in range(8):
        for nbk in range(8):
            if not (nbk < own):
                pm[:, own, :, nbk] = -1e30
    c['pastmask'] = pm.reshape(128, 256)
    sel = np.zeros((32, 32, 128), np.float32)
    for i in range(32):
        sel[i, i, :] = 1.0
    c['sel'] = sel.reshape(32, 32 * 128)
    hm = np.zeros((128, 4), np.float32)
    for p in range(128):
        hm[p, p // 32] = 1.0
    c['headmask'] = hm
    c['pow2tab'] = np.ascontiguousarray(np.broadcast_to((0.5 ** np.arange(1, 25, dtype=np.float64)).astype(np.float32)[None, :], (128, 24)))
    return c


_CACHE = {}


def kernel(**inputs):
    x = np.asarray(inputs['x'], np.float32)
    consts = host_consts(inputs)
    w_br = np.ascontiguousarray(np.stack([np.asarray(inputs[k], np.float32) for k in ['w_br_sb', 'w_br_diff', 'w_br_dsa', 'w_br_moba']], axis=1))
    shared = dict(consts)
    shared['w_in'] = np.ascontiguousarray(np.asarray(inputs['w_in'], np.float32))
    shared['w_br'] = w_br
    shared['w_out'] = np.ascontiguousarray(np.asarray(inputs['w_out'], np.float32))
    shared['w_ffn_in'] = np.ascontiguousarray(np.asarray(inputs['w_ffn_in'], np.float32))
    shared['w_ffn_out'] = np.ascontiguousarray(np.asarray(inputs['w_ffn_out'], np.float32))
    if 'nc' not in _CACHE:
        _CACHE['nc'] = build_program(list(range(DEPTH)))[0]
    nc = _CACHE['nc']
    in_maps = []
    for b in range(8):
        m = dict(shared)
        m['xT'] = np.ascontiguousarray(x[b].T)
        in_maps.append(m)
    res = run_bass_kernel_spmd(nc, in_maps, core_ids=list(range(8)))
    out = np.stack([np.ascontiguousarray(r['outT'].T) for r in res.results], axis=0)
    return out.astype(np.float32)
```
